# Optimizing a Trainium2 kernel written in Bass

```python
import math
import jax, jax.numpy as jnp
from jax import lax
import numpy as np

D_MODEL = 1024
BATCH = 2
SEQ = 8192
DEPTH = 1

N_HEADS_ATTN = 8
HEAD_DIM = 64
D_ATTN = N_HEADS_ATTN * HEAD_DIM
Q_BLOCK = 128
D_SSM = D_MODEL // 2
SSM_GROUP = 16
N_SSM_GROUPS = D_SSM // SSM_GROUP
SSM_STATE = 64
DT_MIN = 1e-3
DT_MAX = 1e-1
N_EXPERT_GROUPS = 4
EXPERTS_PER_GROUP = 8
N_EXPERTS = N_EXPERT_GROUPS * EXPERTS_PER_GROUP
TOP_K = 2
D_EXPERT = D_MODEL // 4
D_IN_PROJ = 3 * D_ATTN + N_HEADS_ATTN + D_SSM + 2 * D_MODEL
EPS = 1e-6
NEG_INF = -1e30

kernel_name = "hybrid_fox_s5_hier_moe_block"


def _rmsnorm(x, g):
    xf = x.astype(jnp.float32)
    y = xf * lax.rsqrt(jnp.mean(xf * xf, axis=-1, keepdims=True) + EPS)
    return (y * g.astype(jnp.float32)).astype(x.dtype)


def _forgetting_attention(q, k, v, f_logit):
    b, s, h, d = q.shape
    n_blk = s // Q_BLOCK
    log_f = jax.nn.log_sigmoid(f_logit.astype(jnp.float32))
    cum = jnp.cumsum(log_f, axis=1).transpose(0, 2, 1)
    qf = q.astype(jnp.float32).transpose(0, 2, 1, 3) * (d ** -0.5)
    kf = k.astype(jnp.float32).transpose(0, 2, 1, 3)
    vf = v.astype(jnp.float32).transpose(0, 2, 1, 3)
    q_blk = qf.reshape(b, h, n_blk, Q_BLOCK, d).transpose(2, 0, 1, 3, 4)
    c_blk = cum.reshape(b, h, n_blk, Q_BLOCK).transpose(2, 0, 1, 3)
    pos = jnp.arange(s, dtype=jnp.int32)
    p_blk = pos.reshape(n_blk, Q_BLOCK)

    def one_block(args):
        qb, cb, pb = args
        logits = jnp.einsum('bhqd,bhkd->bhqk', qb, kf) + cb[..., :, None] - cum[:, :, None, :]
        causal = pos[None, :] <= pb[:, None]
        logits = jnp.where(causal, logits, NEG_INF)
        p = jax.nn.softmax(logits, axis=-1)
        return jnp.einsum('bhqk,bhkd->bhqd', p, vf)

    o = lax.map(one_block, (q_blk, c_blk, p_blk))
    return o.transpose(1, 0, 3, 2, 4).reshape(b, s, h, d).astype(v.dtype)


def _linear_combine(a, b):
    a1, b1 = a
    a2, b2 = b
    return a2 * a1, a2 * b1 + b2


def _s5(u, A_re, A_im, log_dt, B_re, B_im, C_re, C_im, D_skip, w_glu):
    f32 = jnp.float32
    b, s, _ = u.shape
    uf = u.astype(f32).reshape(b, s, N_SSM_GROUPS, SSM_GROUP)
    A = lax.complex(A_re.astype(f32), A_im.astype(f32))
    dt = jnp.exp(log_dt.astype(f32))[:, None]
    A_bar = jnp.exp(A * dt)
    Bm = lax.complex(B_re.astype(f32), B_im.astype(f32))
    B_bar = ((A_bar - 1.0) / A)[..., None] * Bm
    Bu = jnp.einsum('bsgc,gpc->bsgp', uf.astype(jnp.complex64), B_bar)
    A_seq = jnp.broadcast_to(A_bar, Bu.shape)
    _, state = lax.associative_scan(_linear_combine, (A_seq, Bu), axis=1)
    Cm = lax.complex(C_re.astype(f32), C_im.astype(f32))
    y = jnp.real(jnp.einsum('gcp,bsgp->bsgc', Cm, state))
    y = y + D_skip.astype(f32).reshape(N_SSM_GROUPS, SSM_GROUP) * uf
    y = jax.nn.gelu(y.reshape(b, s, D_SSM))
    y = y * jax.nn.sigmoid(y @ w_glu.astype(f32))
    return y.astype(u.dtype)


def _hier_moe(x, w_rg, b_rg, w_re, b_re, w_gate, w_up, w_down):
    f32 = jnp.float32
    b, s, d = x.shape
    t = b * s
    xt = x.reshape(t, d)
    g_prob = jax.nn.softmax((xt @ w_rg).astype(f32) + b_rg.astype(f32), axis=-1)
    g_top, g_sel = lax.top_k(g_prob, 1)
    e_logits = ((xt @ w_re).astype(f32) + b_re.astype(f32)).reshape(t, N_EXPERT_GROUPS, EXPERTS_PER_GROUP)
    idx = jnp.broadcast_to(g_sel[:, :, None], (t, 1, EXPERTS_PER_GROUP))
    e_in = jnp.take_along_axis(e_logits, idx, axis=1)[:, 0]
    e_prob = jax.nn.softmax(e_in, axis=-1)
    e_top, e_idx = lax.top_k(e_prob, TOP_K)
    w = g_top * e_top / jnp.sum(e_top, axis=-1, keepdims=True)
    expert_id = g_sel * EXPERTS_PER_GROUP + e_idx
    combine = jnp.sum(jax.nn.one_hot(expert_id, N_EXPERTS, dtype=f32) * w[..., None], axis=1).astype(x.dtype)
    y = jnp.zeros_like(xt)
    for e in range(N_EXPERTS):
        h = jax.nn.silu(xt @ w_gate[e]) * (xt @ w_up[e])
        y = y + combine[:, e:e + 1] * (h @ w_down[e])
    return y.reshape(b, s, d)


def setup_inputs(seed: int = 0) -> dict:
    key = jax.random.key(seed)
    ks = jax.random.split(key, 32)
    f32 = jnp.float32
    nrm = lambda k, shape, scale: (jax.random.normal(k, shape, f32) * scale)
    L = DEPTH
    x = jax.random.normal(ks[0], (BATCH, SEQ, D_MODEL), f32)
    norm_mix_g = 1.0 + nrm(ks[1], (L, D_MODEL), 0.02)
    w_in = nrm(ks[2], (L, D_MODEL, D_IN_PROJ), D_MODEL ** -0.5)
    b_forget = jnp.broadcast_to(jnp.linspace(1.0, 6.0, N_HEADS_ATTN, dtype=f32), (L, N_HEADS_ATTN)) + nrm(ks[3], (L, N_HEADS_ATTN), 0.1)
    q_norm_g = 1.0 + nrm(ks[4], (L, HEAD_DIM), 0.02)
    k_norm_g = 1.0 + nrm(ks[5], (L, HEAD_DIM), 0.02)
    ssm_A_re = -0.5 + nrm(ks[6], (L, N_SSM_GROUPS, SSM_STATE), 0.01)
    ssm_A_im = jnp.pi * jnp.arange(SSM_STATE, dtype=f32)[None, None, :] + nrm(ks[7], (L, N_SSM_GROUPS, SSM_STATE), 0.01)
    ssm_log_dt = jax.random.uniform(ks[8], (L, N_SSM_GROUPS), f32, math.log(DT_MIN), math.log(DT_MAX))
    ssm_B_re = nrm(ks[9], (L, N_SSM_GROUPS, SSM_STATE, SSM_GROUP), (2 * SSM_GROUP) ** -0.5)
    ssm_B_im = nrm(ks[10], (L, N_SSM_GROUPS, SSM_STATE, SSM_GROUP), (2 * SSM_GROUP) ** -0.5)
    ssm_C_re = nrm(ks[11], (L, N_SSM_GROUPS, SSM_GROUP, SSM_STATE), (2 * SSM_STATE) ** -0.5)
    ssm_C_im = nrm(ks[12], (L, N_SSM_GROUPS, SSM_GROUP, SSM_STATE), (2 * SSM_STATE) ** -0.5)
    ssm_D = nrm(ks[13], (L, D_SSM), 1.0)
    w_glu = nrm(ks[14], (L, D_SSM, D_SSM), D_SSM ** -0.5)
    w_proj_attn = nrm(ks[15], (L, D_ATTN, D_MODEL), D_ATTN ** -0.5)
    w_proj_ssm = nrm(ks[16], (L, D_SSM, D_MODEL), D_SSM ** -0.5)
    w_out = nrm(ks[17], (L, D_MODEL, D_MODEL), D_MODEL ** -0.5)
    norm_ffn_g = 1.0 + nrm(ks[18], (L, D_MODEL), 0.02)
    w_router_group = nrm(ks[19], (L, D_MODEL, N_EXPERT_GROUPS), D_MODEL ** -0.5)
    b_router_group = nrm(ks[20], (L, N_EXPERT_GROUPS), 0.01)
    w_router_expert = nrm(ks[21], (L, D_MODEL, N_EXPERTS), D_MODEL ** -0.5)
    b_router_expert = nrm(ks[22], (L, N_EXPERTS), 0.01)
    w_expert_gate = nrm(ks[23], (L, N_EXPERTS, D_MODEL, D_EXPERT), D_MODEL ** -0.5)
    w_expert_up = nrm(ks[24], (L, N_EXPERTS, D_MODEL, D_EXPERT), D_MODEL ** -0.5)
    w_expert_down = nrm(ks[25], (L, N_EXPERTS, D_EXPERT, D_MODEL), D_EXPERT ** -0.5)
    return {"x": x, "norm_mix_g": norm_mix_g, "w_in": w_in, "b_forget": b_forget,
            "q_norm_g": q_norm_g, "k_norm_g": k_norm_g,
            "ssm_A_re": ssm_A_re, "ssm_A_im": ssm_A_im, "ssm_log_dt": ssm_log_dt,
            "ssm_B_re": ssm_B_re, "ssm_B_im": ssm_B_im, "ssm_C_re": ssm_C_re, "ssm_C_im": ssm_C_im,
            "ssm_D": ssm_D, "w_glu": w_glu, "w_proj_attn": w_proj_attn, "w_proj_ssm": w_proj_ssm,
            "w_out": w_out, "norm_ffn_g": norm_ffn_g,
            "w_router_group": w_router_group, "b_router_group": b_router_group,
            "w_router_expert": w_router_expert, "b_router_expert": b_router_expert,
            "w_expert_gate": w_expert_gate, "w_expert_up": w_expert_up, "w_expert_down": w_expert_down}


def reference(x, norm_mix_g, w_in, b_forget, q_norm_g, k_norm_g,
              ssm_A_re, ssm_A_im, ssm_log_dt, ssm_B_re, ssm_B_im, ssm_C_re, ssm_C_im,
              ssm_D, w_glu, w_proj_attn, w_proj_ssm, w_out, norm_ffn_g,
              w_router_group, b_router_group, w_router_expert, b_router_expert,
              w_expert_gate, w_expert_up, w_expert_down):
    b, s, _ = x.shape
    splits = np.cumsum([D_ATTN, D_ATTN, D_ATTN, N_HEADS_ATTN, D_SSM, D_MODEL]).tolist()
    for l in range(DEPTH):
        h = _rmsnorm(x, norm_mix_g[l])
        z = h @ w_in[l]
        q, k, v, f, u_ssm, g_attn, g_ssm = jnp.split(z, splits, axis=-1)
        q = _rmsnorm(q.reshape(b, s, N_HEADS_ATTN, HEAD_DIM), q_norm_g[l])
        k = _rmsnorm(k.reshape(b, s, N_HEADS_ATTN, HEAD_DIM), k_norm_g[l])
        v = v.reshape(b, s, N_HEADS_ATTN, HEAD_DIM)
        y_attn = _forgetting_attention(q, k, v, f + b_forget[l]).reshape(b, s, D_ATTN)
        y_ssm = _s5(u_ssm, ssm_A_re[l], ssm_A_im[l], ssm_log_dt[l], ssm_B_re[l], ssm_B_im[l],
                    ssm_C_re[l], ssm_C_im[l], ssm_D[l], w_glu[l])
        mixed = (jax.nn.sigmoid(g_attn) * (y_attn @ w_proj_attn[l])
                 + jax.nn.sigmoid(g_ssm) * (y_ssm @ w_proj_ssm[l]))
        x = x + mixed @ w_out[l]
        x = x + _hier_moe(_rmsnorm(x, norm_ffn_g[l]), w_router_group[l], b_router_group[l],
                          w_router_expert[l], b_router_expert[l],
                          w_expert_gate[l], w_expert_up[l], w_expert_down[l])
    return x
```

```python
import contextlib
import numpy as np
import ml_dtypes
import concourse.bass as bass
import concourse.mybir as mybir
from concourse.bass_utils import run_bass_kernel_spmd

F32 = mybir.dt.float32
BF16 = mybir.dt.bfloat16
AF = mybir.ActivationFunctionType
OP = mybir.AluOpType
AX = mybir.AxisListType

NCORES = 8
D = 1024
S = 8192
NTOK = 16384
TB = 2048
EPS = 1e-6
CH = 3000
NDS = 3
CHBIG = 30000
MAGIC = 12582912.0
INV2PI = 0.15915494309189535
C1 = 6.28125
C2 = 0.0019353071795864769
PI = 3.141592653589793

STREAMS = {'pe': ('tensor', False), 'dve': ('vector', False), 'act': ('scalar', False),
           'pool': ('gpsimd', False), 'gq': ('gpsimd', True), 'sq': ('sync', True), 'vq': ('scalar', True)}


def _norm(k):
    if isinstance(k, tuple):
        return k[0], k[1:]
    return k, None


class Prog:
    def __init__(self, nc, sem_stack=None, shared=None):
        self.nc = nc
        self.ops = []
        self.sem_stack = sem_stack
        self.shared = shared if shared is not None else {"sems": None, "base": {}}

    def add(self, st, fn, r=(), w=()):
        self.ops.append((st, fn, tuple(r), tuple(w)))

    def emit(self, barrier=False):
        nc = self.nc
        ops = self.ops
        base = self.shared["base"]
        cnt = {}
        idx = []
        for (st, fn, r, w) in ops:
            idx.append(base.get(st, 0) + cnt.get(st, 0))
            cnt[st] = cnt.get(st, 0) + 1
        tot = {st: base.get(st, 0) + cnt[st] for st in cnt}
        writers = {}
        readers = {}
        deps = []

        def conf(a, b):
            return a is None or b is None or a == b
        for i, (st, fn, r, w) in enumerate(ops):
            d = set()
            for k in r:
                name, sub = _norm(k)
                for (s2, o) in writers.get(name, ()):
                    if conf(sub, s2):
                        d.add(o)
            for k in w:
                name, sub = _norm(k)
                for (s2, o) in writers.get(name, ()):
                    if conf(sub, s2):
                        d.add(o)
                for (s2, o) in readers.get(name, ()):
                    if conf(sub, s2):
                        d.add(o)
            for k in w:
                name, sub = _norm(k)
                writers[name] = [(s2, o) for (s2, o) in writers.get(name, []) if not (sub is None or s2 == sub)]
                writers[name].append((sub, i))
                readers[name] = [(s2, o) for (s2, o) in readers.get(name, []) if not conf(sub, s2)]
            for k in r:
                name, sub = _norm(k)
                readers.setdefault(name, []).append((sub, i))
            d.discard(i)
            need = {}
            needd = set()
            for o in d:
                s2 = ops[o][0]
                if STREAMS[s2][1]:
                    needd.add((s2, idx[o]))
                elif need.get(s2, -1) < idx[o]:
                    need[s2] = idx[o]
            deps.append((need, needd))

        with contextlib.ExitStack() as st_:
            if self.shared["sems"] is None:
                sstack = self.sem_stack if self.sem_stack is not None else st_
                sems = {}
                for s in STREAMS:
                    if self.sem_stack is None and s not in cnt:
                        continue
                    if STREAMS[s][1]:
                        sems[s] = [sstack.enter_context(nc.semaphore("d_%s_%d" % (s, i))) for i in range(NDS)]
                    else:
                        nchunk = 1 if self.sem_stack is not None else (cnt[s] // CH + 1)
                        sems[s] = [sstack.enter_context(nc.semaphore("s_%s_%d" % (s, i))) for i in range(nchunk)]
                if self.sem_stack is not None:
                    self.shared["sems"] = sems
            else:
                sems = self.shared["sems"]
            chdiv = CHBIG if self.sem_stack is not None else CH
            block = st_.enter_context(nc.Block())

            def section(ename, eng):
                waited = {}
                waitedd = {}

                def wait_c(s2, j):
                    if waited.get(s2, -1) >= j:
                        return
                    eng.wait_ge(sems[s2][j // chdiv], (j % chdiv) + 1)
                    waited[s2] = j

                def wait_d(s2, j):
                    key = (s2, j % NDS)
                    if waitedd.get(key, -1) >= j:
                        return
                    eng.wait_ge(sems[s2][j % NDS], (j // NDS + 1) * 16)
                    waitedd[key] = j
                for i, (st, fn, r, w) in enumerate(ops):
                    if STREAMS[st][0] != ename:
                        continue
                    need, needd = deps[i]
                    for s2, j in need.items():
                        if st == 'pe' and s2 == 'pe':
                            continue
                        wait_c(s2, j)
                    for (s2, j) in sorted(needd):
                        wait_d(s2, j)
                    j = idx[i]
                    if STREAMS[st][1]:
                        if j >= NDS:
                            wait_d(st, j - NDS)
                        inst = fn(eng)
                        inst.then_inc(sems[st][j % NDS], 16)
                    else:
                        inst = fn(eng)
                        inst.then_inc(sems[st][j // chdiv], 1)
                if ename == 'sync' or barrier:
                    for s2 in cnt:
                        if STREAMS[s2][1]:
                            for j in range(max(0, tot[s2] - NDS), tot[s2]):
                                wait_d(s2, j)
                        else:
                            wait_c(s2, tot[s2] - 1)

            @block.tensor
            def _(eng):
                section('tensor', eng)

            @block.vector
            def _(eng):
                section('vector', eng)

            @block.scalar
            def _(eng):
                section('scalar', eng)

            @block.gpsimd
            def _(eng):
                section('gpsimd', eng)

            @block.sync
            def _(eng):
                section('sync', eng)
        for st in cnt:
            base[st] = tot[st]
        self.ops = []


def _helpers(P):
    def mk(st):
        def f(fn, r=(), w=()):
            P.add(st, fn, r, w)
        return f
    return mk('dve'), mk('act'), mk('pe'), mk('gq'), mk('sq'), mk('pool'), mk('vq')


def build_a(nc=None, snd=None, sem_stack=None, shared=None):
    fused = nc is not None
    if nc is None:
        nc = bass.Bass("TRN2", target_bir_lowering=False)
    dt = nc.dram_tensor
    xT = dt("xT", [D, NTOK], F32, kind="ExternalInput").ap()
    wq = dt("wq", [D, 64], F32, kind="ExternalInput").ap()
    wk = dt("wk", [D, 65], F32, kind="ExternalInput").ap()
    wvu = dt("wvu", [D, 128], F32, kind="ExternalInput").ap()
    g1 = dt("g1", [128, 8], F32, kind="ExternalInput").ap()
    nbf = dt("nbf", [65, 2], F32, kind="ExternalInput").ap()
    gqk = dt("gqk", [64, 2], F32, kind="ExternalInput").ap()
    are = dt("are", [128, 2], F32, kind="ExternalInput").ap()
    aim = dt("aim", [128, 2], F32, kind="ExternalInput").ap()
    ldt = dt("ldt", [128, 2], F32, kind="ExternalInput").ap()
    brp = dt("brp", [64, 2, 128], F32, kind="ExternalInput").ap()
    bip = dt("bip", [64, 2, 128], F32, kind="ExternalInput").ap()
    crp = dt("crp", [128, 2, 128], F32, kind="ExternalInput").ap()
    cip = dt("cip", [128, 2, 128], F32, kind="ExternalInput").ap()
    dsk = dt("dsk", [64, 2], F32, kind="ExternalInput").ap()
    onesb = dt("onesb", [128, 128], F32, kind="ExternalInput").ap()
    identb = dt("identb", [128, 128], F32, kind="ExternalInput").ap()
    iota1 = dt("iota1", [128, 512], F32, kind="ExternalInput").ap()
    maskd = dt("maskd", [128, 128], F32, kind="ExternalInput").ap()
    if not fused:
        yaT = dt("yaT", [64, NTOK], BF16, kind="ExternalOutput").ap()
        ysT = dt("ysT", [64, NTOK], BF16, kind="ExternalOutput").ap()

    P = Prog(nc, sem_stack=sem_stack, shared=shared)
    V, A, T, G, SY, PL, VQ = _helpers(P)
    with contextlib.ExitStack() as st:
        def sb(name, shape, dtype):
            return st.enter_context(nc.sbuf_tensor(name, shape, dtype))

        def pp(name, shape, dtype=F32):
            return st.enter_context(nc.psum_tensor(name, shape, dtype))
        WQ = sb("WQ", [128, 8, 64], BF16)
        WK = sb("WK", [128, 8, 65], BF16)
        WVU = sb("WVU", [128, 8, 128], BF16)
        G1 = sb("G1", [128, 8], F32)
        NBF = sb("NBF", [65, 2], F32)
        GQK = sb("GQK", [64, 2], F32)
        ARE = sb("ARE", [128, 2], F32)
        AIM = sb("AIM", [128, 2], F32)
        LDT = sb("LDT", [128, 2], F32)
        BRP = sb("BRP", [128, 2, 128], BF16)
        BIP = sb("BIP", [128, 2, 128], BF16)
        CRP = sb("CRP", [128, 2, 128], BF16)
        CIP = sb("CIP", [128, 2, 128], BF16)
        CRN = sb("CRN", [128, 2, 128], BF16)
        CIN = sb("CIN", [128, 2, 128], BF16)
        DSK = sb("DSK", [128, 2], F32)
        ONB = sb("ONB", [128, 128], BF16)
        ONF = sb("ONF", [128, 128], F32)
        IDB = sb("IDB", [128, 128], BF16)
        IOT = sb("IOT", [128, 512], F32)
        MSK = sb("MSK", [128, 128], BF16)
        sm = {n: sb("sm_" + n, [128, 2], F32) for n in
              ["dt", "th", "rho", "k", "r", "ar", "sn", "cs", "lr", "li", "nr", "den", "t1", "t2", "cr", "ci",
               "cL", "sL", "CR", "CI", "ta", "tb"]}
        TR = sb("TR", [128, 2, 512], F32)
        TI = sb("TI", [128, 2, 512], F32)
        TC = sb("TC", [128, 2, 512], F32)
        TS = sb("TS", [128, 2, 512], F32)
        RB = sb("RB", [128, 2, 512], F32)
        QA = sb("QA", [128, S], BF16)
        KA = sb("KA", [128, S], BF16)
        VTM = sb("VTM", [128, 64, 128], BF16)
        UT = sb("UT", [128, S], BF16)
        FR = [sb("FR%d" % i, [65, 512], F32) for i in range(2)]
        AR = sb("AR", [65, 512], F32)
        AH = sb("AH", [65, 512], BF16)
        AL = sb("AL", [65, 512], BF16)
        FCOL = sb("FCOL", [128, 64], F32)
        FQ0 = sb("FQ0", [128, 16], F32)
        CB = sb("CB", [128, 16, 64], F32)
        YA = [sb("YA%d" % i, [64, 512], F32 if fused else BF16) for i in range(2)]
        YS = [sb("YS%d" % i, [128, 512], F32 if fused else BF16) for i in range(2)]
        XSQ = sb("XSQ", [128, 8, 512], BF16)
        XBF = [sb("XBF%d" % i, [128, 8, 512], BF16) for i in range(2)]
        RSTD = [sb("RSTD%d" % i, [128, 512], F32) for i in range(2)]
        RS = RSTD
        XST = [sb("XST%d" % i, [128, 512], F32) for i in range(2)]
        QF = [[sb("QF%d_%d" % (i, j), [64, 512], F32) for j in range(2)] for i in range(2)]
        SQ1 = [sb("SQ1%d" % i, [64, 512], BF16) for i in range(2)]
        RQ2 = [sb("RQ2%d" % i, [64, 512], F32) for i in range(2)]
        RQ = RQ2
        VB = [sb("VB%d" % i, [64, 512], BF16) for i in range(2)]
        FF = [sb("FF%d" % i, [65, 512], F32) for i in range(2)]
        FE = sb("FE", [65, 512], F32)
        ONR = sb("ONR", [65, 512], F32)
        PT = [sb("PT%d" % i, [128, 512], BF16) for i in range(2)]
        OS = [sb("OS%d" % i, [65, 512], F32) for i in range(2)]
        f1 = sb("f1", [128, 512], F32)
        f2 = sb("f2", [128, 512], F32)
        f3 = sb("f3", [128, 512], F32)
        f4 = sb("f4", [128, 512], F32)
        PH, PK, PA2 = f1, f2, f3
        WRl = [sb("WRr%d" % i, [128, 512], F32) for i in range(2)]
        WIl = [sb("WIi%d" % i, [128, 512], F32) for i in range(2)]
        bb = [[sb("b%d_%d" % (i, j), [128, 512], BF16) for i in range(4)] for j in range(2)]
        B = [pp("B%d" % i, [128, 512]) for i in range(7)]
        PTB = pp("PTB", [128, 4, 64], BF16)

        def ldw(dst, src, key):
            G(lambda e: e.dma_start(out=dst, in_=src), w=[key])
        ldw(WQ[:], wq.rearrange("(k p) n -> p k n", p=128), "WQ")
        ldw(WK[:], wk.rearrange("(k p) n -> p k n", p=128), "WK")
        ldw(WVU[:], wvu.rearrange("(k p) n -> p k n", p=128), "WVU")
        for (dst, src, key) in [(G1[:], g1, "G1"), (NBF[:], nbf, "NBF"), (GQK[:], gqk, "GQK"), (ARE[:], are, "ARE"),
                                (AIM[:], aim, "AIM"), (LDT[:], ldt, "LDT"), (BRP[64:128], brp, "BRP"),
                                (BIP[64:128], bip, "BIP"), (CRP[:], crp, "CRP"), (CIP[:], cip, "CIP"),
                                (DSK[64:128], dsk, "DSK"), (ONB[:], onesb, "ONB"), (ONF[:], onesb, "ONF"),
                                (IDB[:], identb, "IDB"), (IOT[:], iota1, "IOT"), (MSK[:], maskd, "MSK")]:
            ldw(dst, src, key)

        def xload(gb):
            T0 = gb * 512
            p = gb % 2
            for k in range(8):
                si = (gb * 8 + k) % 4
                if k < 4:
                    G(lambda e, k=k, T0=T0, p=p: e.dma_start(out=XBF[p][:, k, :],
                                                              in_=xT[k * 128:(k + 1) * 128, T0:T0 + 512]),
                      w=[("XBF", p, k)])
                else:
                    si = k % 2
                    SY(lambda e, k=k, T0=T0, si=si: e.dma_start(out=XST[si][:],
                                                                in_=xT[k * 128:(k + 1) * 128, T0:T0 + 512]),
                       w=[("XST", si)])
                    A(lambda e, k=k, p=p, si=si: e.activation(out=XBF[p][:, k, :], in_=XST[si][:], func=AF.Copy),
                      r=[("XST", si)], w=[("XBF", p, k)])
        xload(0)
        for (Wt, key) in ((WQ, "WQ"), (WK, "WK"), (WVU, "WVU")):
            for k in range(8):
                V(lambda e, Wt=Wt, k=k: e.tensor_scalar(out=Wt[:, k, :], in0=Wt[:, k, :], scalar1=G1[:, k:k + 1],
                                                        scalar2=None, op0=OP.mult), r=["G1", key], w=[key])
        V(lambda e: e.tensor_scalar(out=GQK[:, 0:1], in0=GQK[:, 0:1], scalar1=0.125, scalar2=None, op0=OP.mult),
          r=["GQK"], w=["GQK"])
        V(lambda e: e.tensor_scalar(out=NBF[:], in0=NBF[:], scalar1=-1.0, scalar2=None, op0=OP.mult), r=["NBF"],
          w=["NBF"])
        V(lambda e: e.memset(ONR[:], 1.0), w=["ONR"])
        V(lambda e: e.memset(QA[:], 0.0), w=["QA"])
        V(lambda e: e.memset(KA[:], 0.0), w=["KA"])
        V(lambda e: e.memset(KA[64:66, :], 1.0), r=["KA"], w=["KA"])
        V(lambda e: e.memset(VTM[:], 1.0), w=["VTM"])
        V(lambda e: e.tensor_scalar(out=CRN[:], in0=CRP[:], scalar1=-1.0, scalar2=None, op0=OP.mult), r=["CRP"],
          w=["CRN"])
        V(lambda e: e.tensor_scalar(out=CIN[:], in0=CIP[:], scalar1=-1.0, scalar2=None, op0=OP.mult), r=["CIP"],
          w=["CIN"])

        def VS(fn):
            V(fn, r=["S5T", "ARE", "AIM", "LDT", "IOT"], w=["S5T"])

        def AS(fn):
            A(fn, r=["S5T", "LDT"], w=["S5T"])

        def rred(out, in_, k_t):
            VS(lambda e: e.tensor_scalar(out=k_t, in0=in_, scalar1=INV2PI, scalar2=MAGIC, op0=OP.mult, op1=OP.add))
            VS(lambda e: e.tensor_scalar(out=k_t, in0=k_t, scalar1=MAGIC, scalar2=None, op0=OP.subtract))
            VS(lambda e: e.scalar_tensor_tensor(out=out, in0=k_t, scalar=-C1, in1=in_, op0=OP.mult, op1=OP.add))
            VS(lambda e: e.scalar_tensor_tensor(out=out, in0=k_t, scalar=-C2, in1=out, op0=OP.mult, op1=OP.add))
            VS(lambda e: e.tensor_scalar(out=out, in0=out, scalar1=PI, scalar2=-PI, op0=OP.min, op1=OP.max))

        def sincos(sn, cs, r, tmp):
            AS(lambda e: e.activation(out=sn, in_=r, func=AF.Sin))
            VS(lambda e: e.tensor_scalar(out=tmp, in0=r, scalar1=-1.0, scalar2=None, op0=OP.mult))
            VS(lambda e: e.tensor_tensor(out=tmp, in0=tmp, in1=r, op=OP.max))
            VS(lambda e: e.tensor_scalar(out=tmp, in0=tmp, scalar1=-1.0, scalar2=PI / 2, op0=OP.mult, op1=OP.add))
            AS(lambda e: e.activation(out=cs, in_=tmp, func=AF.Sin))
        s_ = {k: v[:] for k, v in sm.items()}
        AS(lambda e: e.activation(out=s_["dt"], in_=LDT[:], func=AF.Exp))
        VS(lambda e: e.tensor_tensor(out=s_["th"], in0=AIM[:], in1=s_["dt"], op=OP.mult))
        VS(lambda e: e.tensor_tensor(out=s_["t1"], in0=ARE[:], in1=s_["dt"], op=OP.mult))
        AS(lambda e: e.activation(out=s_["rho"], in_=s_["t1"], func=AF.Exp))
        rred(s_["r"], s_["th"], s_["k"])
        sincos(s_["sn"], s_["cs"], s_["r"], s_["ar"])
        VS(lambda e: e.tensor_tensor(out=s_["lr"], in0=s_["rho"], in1=s_["cs"], op=OP.mult))
        VS(lambda e: e.tensor_tensor(out=s_["li"], in0=s_["rho"], in1=s_["sn"], op=OP.mult))
        VS(lambda e: e.tensor_scalar(out=s_["nr"], in0=s_["lr"], scalar1=-1.0, scalar2=None, op0=OP.add))
        VS(lambda e: e.tensor_tensor(out=s_["t1"], in0=ARE[:], in1=ARE[:], op=OP.mult))
        VS(lambda e: e.tensor_tensor(out=s_["t2"], in0=AIM[:], in1=AIM[:], op=OP.mult))
        VS(lambda e: e.tensor_tensor(out=s_["den"], in0=s_["t1"], in1=s_["t2"], op=OP.add))
        VS(lambda e: e.reciprocal(out=s_["den"], in_=s_["den"]))
        VS(lambda e: e.tensor_tensor(out=s_["t1"], in0=s_["nr"], in1=ARE[:], op=OP.mult))
        VS(lambda e: e.tensor_tensor(out=s_["t2"], in0=s_["li"], in1=AIM[:], op=OP.mult))
        VS(lambda e: e.tensor_tensor(out=s_["t1"], in0=s_["t1"], in1=s_["t2"], op=OP.add))
        VS(lambda e: e.tensor_tensor(out=s_["cr"], in0=s_["t1"], in1=s_["den"], op=OP.mult))
        VS(lambda e: e.tensor_tensor(out=s_["t1"], in0=s_["li"], in1=ARE[:], op=OP.mult))
        VS(lambda e: e.tensor_tensor(out=s_["t2"], in0=s_["nr"], in1=AIM[:], op=OP.mult))
        VS(lambda e: e.tensor_tensor(out=s_["t1"], in0=s_["t1"], in1=s_["t2"], op=OP.subtract))
        VS(lambda e: e.tensor_tensor(out=s_["ci"], in0=s_["t1"], in1=s_["den"], op=OP.mult))
        for gh in range(2):
            VS(lambda e, gh=gh: e.tensor_scalar(out=PH[:], in0=IOT[:], scalar1=sm["th"][:, gh:gh + 1], scalar2=None,
                                                op0=OP.mult))
            rred(PA2[:], PH[:], PK[:])
            sincos(TS[:, gh, :], TC[:, gh, :], PA2[:], PK[:])
            VS(lambda e, gh=gh: e.tensor_scalar(out=PH[:], in0=TS[:, gh, :], scalar1=sm["ci"][:, gh:gh + 1],
                                                scalar2=None, op0=OP.mult))
            VS(lambda e, gh=gh: e.scalar_tensor_tensor(out=TR[:, gh, :], in0=TC[:, gh, :],
                                                       scalar=sm["cr"][:, gh:gh + 1], in1=PH[:], op0=OP.mult,
                                                       op1=OP.add))
            VS(lambda e, gh=gh: e.tensor_scalar(out=PH[:], in0=TS[:, gh, :], scalar1=sm["cr"][:, gh:gh + 1],
                                                scalar2=None, op0=OP.mult))
            VS(lambda e, gh=gh: e.scalar_tensor_tensor(out=TI[:, gh, :], in0=TC[:, gh, :],
                                                       scalar=sm["ci"][:, gh:gh + 1], in1=PH[:], op0=OP.mult,
                                                       op1=OP.subtract))
            VS(lambda e, gh=gh: e.tensor_scalar(out=RB[:, gh, :], in0=IOT[:], scalar1=0.0,
                                                scalar2=sm["rho"][:, gh:gh + 1], op0=OP.mult, op1=OP.add))
            VS(lambda e, gh=gh: e.tensor_copy(out=sm["cL"][:, gh:gh + 1], in_=TC[:, gh, 511:512]))
            VS(lambda e, gh=gh: e.tensor_copy(out=sm["sL"][:, gh:gh + 1], in_=TS[:, gh, 511:512]))
        S5K = ["S5T"]
        V(lambda e: e.memset(f4[0:1, 0:2], 0.0), r=["S5T"], w=["f1", "f2", "f3", "f4"])

        def inproj_s1(s, b):
            gb = s * 16 + b
            p = gb % 2
            t0 = b * 512
            sl = slice(t0, t0 + 512)
            xk = ("XBF", p)
            if gb + 1 < 32:
                xload(gb + 1)
            for k in range(8):
                V(lambda e, k=k, p=p: e.tensor_tensor(out=XSQ[:, k, :], in0=XBF[p][:, k, :], in1=XBF[p][:, k, :],
                                                      op=OP.mult), r=[("XBF", p, k)], w=[("XSQ", k)])
            for k in range(8):
                T(lambda e, k=k, p=p: e.matmul(B[p][:], lhsT=ONB[:], rhs=XSQ[:, k, :], start=(k == 0), stop=(k == 7)),
                  r=[("XSQ", k), "ONB"], w=[("B", p)])
            A(lambda e, p=p: e.activation(out=RS[p][:], in_=B[p][:], func=AF.Ln, bias=EPS, scale=1.0 / D),
              r=[("B", p)], w=[("RSTD", p)])
            A(lambda e, p=p: e.activation(out=RSTD[p][:], in_=RS[p][:], func=AF.Exp, scale=-0.5), r=[("RSTD", p)],
              w=[("RSTD", p)])
            rk = ("RSTD", p)
            for qi, (Wt, wkey, M) in enumerate(((WQ, "WQ", 64), (WK, "WK", 65))):
                pb = 2 + qi
                for k in range(8):
                    T(lambda e, k=k, Wt=Wt, pb=pb, p=p, M=M: e.matmul(B[pb][0:M, :], lhsT=Wt[:, k, :],
                                                                       rhs=XBF[p][:, k, :], start=(k == 0),
                                                                       stop=(k == 7)),
                      r=[wkey, ("XBF", p, k)], w=[("B", pb)])
                V(lambda e, pb=pb, qi=qi, p=p: e.tensor_tensor(out=QF[qi][p][:], in0=B[pb][0:64, :],
                                                               in1=RSTD[p][0:64, :], op=OP.mult),
                  r=[("B", pb), rk], w=[("QF", qi, p)])
                if qi == 1:
                    V(lambda e, p=p: e.tensor_tensor(out=FF[p][64:65, :], in0=B[3][64:65, :],
                                                     in1=RSTD[p][64:65, :], op=OP.mult), r=[("B", 3), rk],
                      w=[("FF", p)])
            for k in range(8):
                T(lambda e, k=k, p=p: e.matmul(B[6][:], lhsT=WVU[:, k, :], rhs=XBF[p][:, k, :], start=(k == 0),
                                               stop=(k == 7)), r=["WVU", ("XBF", p, k)], w=[("B", 6)])
            V(lambda e, p=p: e.tensor_tensor(out=VB[p][:], in0=B[6][0:64, :], in1=RSTD[p][0:64, :], op=OP.mult),
              r=[("B", 6), rk], w=[("VB", p)])
            V(lambda e, sl=sl, p=p: e.tensor_tensor(out=UT[64:128, sl], in0=B[6][64:128, :], in1=RSTD[p][64:128, :],
                                                    op=OP.mult), r=[("B", 6), rk], w=[("UT", b)])

        def inproj_s2(s, b):
            gb = s * 16 + b
            p = gb % 2
            t0 = b * 512
            sl = slice(t0, t0 + 512)
            for qi, (dst, dkey, gcol) in enumerate(((QA, "QA", 0), (KA, "KA", 1))):
                A(lambda e, qi=qi, p=p: e.activation(out=SQ1[qi][:], in_=QF[qi][p][:], func=AF.Square),
                  r=[("QF", qi, p)], w=[("SQ1", qi)])
                T(lambda e, qi=qi: e.matmul(B[4 + qi][0:64, :], lhsT=ONB[0:64, 0:64], rhs=SQ1[qi][:], start=True,
                                            stop=True), r=[("SQ1", qi), "ONB"], w=[("B", 4 + qi)])
                A(lambda e, qi=qi: e.activation(out=RQ[qi][:], in_=B[4 + qi][0:64, :], func=AF.Ln, bias=EPS,
                                                scale=1.0 / 64), r=[("B", 4 + qi)], w=[("RQ2", qi)])
                A(lambda e, qi=qi: e.activation(out=RQ2[qi][:], in_=RQ[qi][:], func=AF.Exp, scale=-0.5),
                  r=[("RQ2", qi)], w=[("RQ2", qi)])
                V(lambda e, dst=dst, gcol=gcol, sl=sl, qi=qi, p=p: e.scalar_tensor_tensor(
                    out=dst[0:64, sl], in0=QF[qi][p][:], scalar=GQK[:, gcol:gcol + 1], in1=RQ2[qi][:], op0=OP.mult,
                    op1=OP.mult), r=[("QF", qi, p), ("RQ2", qi), "GQK"], w=[(dkey, b)])
            for tt in range(4):
                T(lambda e, tt=tt, p=p: e.transpose(PTB[:, tt, :], VB[p][:, tt * 128:(tt + 1) * 128],
                                                    IDB[0:64, 0:64]), r=[("VB", p), "IDB"], w=["PTB"])
            for tt in range(4):
                V(lambda e, tt=tt, b=b: e.tensor_copy(out=VTM[:, b * 4 + tt, 0:64], in_=PTB[:, tt, :]),
                  r=["PTB"], w=[("VTM", b * 4 + tt)])
            A(lambda e, p=p: e.activation(out=FE[64:65, :], in_=FF[p][64:65, :], func=AF.Exp, bias=NBF[64:65, 0:1],
                                          scale=-1.0), r=[("FF", p), "NBF"], w=["FE"])
            A(lambda e: e.activation(out=FE[64:65, :], in_=FE[64:65, :], func=AF.Ln, bias=1.0, scale=1.0),
              r=["FE"], w=["FE"])
            V(lambda e: e.tensor_scalar(out=FE[64:65, :], in0=FE[64:65, :], scalar1=-1.0, scalar2=None,
                                        op0=OP.mult), r=["FE"], w=["FE"])
            fp = b % 2
            if b == 0:
                V(lambda e, fp=fp: e.tensor_tensor_scan(out=FR[fp][64:65, :], data0=ONR[64:65, :],
                                                        data1=FE[64:65, :], initial=0.0, op0=OP.mult, op1=OP.add),
                  r=["FE", "ONR"], w=[("FR", fp)])
            else:
                V(lambda e, fp=fp: e.tensor_tensor_scan(out=FR[fp][64:65, :], data0=ONR[64:65, :],
                                                        data1=FE[64:65, :], initial=FR[1 - fp][64:65, 511:512],
                                                        op0=OP.mult, op1=OP.add),
                  r=["FE", "ONR", ("FR", 1 - fp)], w=[("FR", fp)])
            frk = ("FR", fp)
            V(lambda e, fp=fp: e.tensor_scalar(out=AR[64:65, :], in0=FR[fp][64:65, :], scalar1=FR[fp][64:65, 0:1],
                                               scalar2=None, op0=OP.subtract), r=[frk], w=["AR"])
            V(lambda e: e.tensor_copy(out=AH[64:65, :], in_=AR[64:65, :]), r=["AR"], w=["AH"])
            V(lambda e: e.tensor_tensor(out=AL[64:65, :], in0=AR[64:65, :], in1=AH[64:65, :], op=OP.subtract),
              r=["AR", "AH"], w=["AL"])
            VQ(lambda e, sl=sl: e.dma_start(out=QA[64:65, sl], in_=AH[64:65, :]), r=["AH"], w=[("QA", b)])
            VQ(lambda e, sl=sl: e.dma_start(out=QA[65:66, sl], in_=AL[64:65, :]), r=["AL"], w=[("QA", b)])
            for j in range(4):
                T(lambda e, j=j, fp=fp: e.matmul(B[5][:, 8 + j:9 + j], lhsT=FR[fp][64:65, j * 128:(j + 1) * 128],
                                                 rhs=ONF[64:65, 0:1], start=True, stop=True), r=[frk, "ONF"],
                  w=[("B", 5)])
            T(lambda e, fp=fp: e.matmul(B[5][:, 16:17], lhsT=ONF[64:65, :], rhs=FR[fp][64:65, 0:1], start=True,
                                        stop=True), r=[frk, "ONF"], w=[("B", 5)])
            V(lambda e, b=b: e.tensor_copy(out=FCOL[:, 4 * b:4 * b + 4], in_=B[5][:, 8:12]), r=[("B", 5)],
              w=["FCOL"])
            V(lambda e, b=b: e.tensor_copy(out=FQ0[:, b:b + 1], in_=B[5][:, 16:17]), r=[("B", 5)], w=["FQ0"])
            V(lambda e, b=b: e.tensor_scalar(out=CB[:, b, 0:4 * b + 4], in0=FCOL[:, 0:4 * b + 4], scalar1=-1.0,
                                             scalar2=FQ0[:, b:b + 1], op0=OP.mult, op1=OP.add),
              r=["FCOL", "FQ0"], w=[("CB", b)])

        def capture(fn, *args):
            saved = P.ops
            P.ops = []
            fn(*args)
            out = P.ops
            P.ops = saved
            return out

        def merge(la, lb):
            na, nb = len(la), len(lb)
            ia = ib = 0
            while ia < na or ib < nb:
                if ib >= nb or (ia < na and ia * nb <= ib * na):
                    P.ops.append(la[ia])
                    ia += 1
                else:
                    P.ops.append(lb[ib])
                    ib += 1
        def att_tiles():
            lst = []
            for qb in range(16):
                nk = 4 * qb + 4
                for j in range(nk):
                    lst.append((qb, j, nk))
            return lst

        def att_qk(s, i, qb, j, nk):
            sp = i % 2
            t0 = qb * 512
            dj = j - 4 * qb
            c0 = dj * 128 if dj > 0 else 0
            diag = dj >= 0
            T(lambda e: e.matmul(B[sp][:, c0:512], lhsT=KA[:, j * 128:(j + 1) * 128],
                                 rhs=QA[:, t0 + c0:t0 + 512], start=True, stop=(not diag)),
              r=[("KA", j // 4), ("QA", qb)], w=[("B", sp)])
            if diag:
                T(lambda e: e.matmul(B[sp][:, c0:c0 + 128], lhsT=IDB[:], rhs=MSK[:], start=False, stop=True),
                  r=["IDB", "MSK"], w=[("B", sp)])
            A(lambda e: e.activation(out=PT[sp][:, c0:512], in_=B[sp][:, c0:512], func=AF.Exp,
                                     bias=CB[:, qb, j:j + 1], scale=1.0), r=[("B", sp), ("CB", qb)], w=[("PT", sp)])

        def att_pv(s, i, qb, j, nk):
            sp = i % 2
            op_ = qb % 2
            t0 = qb * 512
            dj = j - 4 * qb
            c0 = dj * 128 if dj > 0 else 0
            T(lambda e: e.matmul(B[2 + op_][:, c0:512], lhsT=VTM[:, j, :], rhs=PT[sp][:, c0:512],
                                 start=(j == 0), stop=(j == nk - 1)), r=[("VTM", j), ("PT", sp)], w=[("B", 2 + op_)])
            if j == nk - 1:
                V(lambda e: e.tensor_copy(out=OS[op_][:], in_=B[2 + op_][0:65, :]), r=[("B", 2 + op_)],
                  w=[("OS", op_)])
                V(lambda e: e.reciprocal(out=OS[op_][64:65, :], in_=OS[op_][64:65, :]), r=[("OS", op_)],
                  w=[("OS", op_)])
                T(lambda e: e.matmul(B[4][0:64, :], lhsT=ONF[64:65, 0:64], rhs=OS[op_][64:65, :], start=True,
                                     stop=True), r=[("OS", op_), "ONF"], w=[("B", 4)])
                V(lambda e: e.tensor_tensor(out=YA[op_][:], in0=OS[op_][0:64, :], in1=B[4][0:64, :], op=OP.mult),
                  r=[("OS", op_), ("B", 4)], w=[("YA", op_)])
                if fused:
                    tok = s * S + t0
                    SY(lambda e, tok=tok: e.dma_start(out=snd[tok // TB][0:64, tok % TB:tok % TB + 512],
                                                      in_=YA[op_][:]), r=[("YA", op_)], w=["snd"])
                else:
                    SY(lambda e: e.dma_start(out=yaT[:, s * S + t0:s * S + t0 + 512], in_=YA[op_][:]),
                       r=[("YA", op_)], w=["yaT"])

        def s5_front(s, b, gh):
            u = b * 2 + gh
            bp = u % 2
            WR_, WI_ = WRl[bp], WIl[bp]
            t0 = b * 512
            sl = slice(t0, t0 + 512)
            if b == 0 and gh == 0:
                V(lambda e: e.memset(sm["CR"][:], 0.0), w=["CRI"])
                V(lambda e: e.memset(sm["CI"][:], 0.0), w=["CRI"])
            T(lambda e: e.matmul(B[5][:], lhsT=BRP[64:128, gh, :], rhs=UT[64:128, sl], start=True, stop=True),
              r=["BRP", ("UT", b)], w=[("B", 5)])
            T(lambda e: e.matmul(B[6][:], lhsT=BIP[64:128, gh, :], rhs=UT[64:128, sl], start=True, stop=True),
              r=["BIP", ("UT", b)], w=[("B", 6)])
            V(lambda e: e.tensor_tensor(out=f1[:], in0=B[5][:], in1=TR[:, gh, :], op=OP.mult), r=[("B", 5)] + S5K,
              w=["f1"])
            V(lambda e: e.tensor_tensor(out=f2[:], in0=B[6][:], in1=TI[:, gh, :], op=OP.mult), r=[("B", 6)] + S5K,
              w=["f2"])
            V(lambda e: e.tensor_tensor(out=f3[:], in0=B[5][:], in1=TI[:, gh, :], op=OP.mult), r=[("B", 5)] + S5K,
              w=["f3"])
            V(lambda e: e.tensor_tensor(out=f4[:], in0=B[6][:], in1=TR[:, gh, :], op=OP.mult), r=[("B", 6)] + S5K,
              w=["f4"])
            V(lambda e: e.tensor_tensor(out=f1[:], in0=f1[:], in1=f2[:], op=OP.subtract), r=["f1", "f2"], w=["f1"])
            V(lambda e: e.tensor_tensor(out=f3[:], in0=f3[:], in1=f4[:], op=OP.add), r=["f3", "f4"], w=["f3"])
            V(lambda e: e.tensor_tensor_scan(out=WR_[:], data0=RB[:, gh, :], data1=f1[:],
                                             initial=sm["CR"][:, gh:gh + 1], op0=OP.mult, op1=OP.add),
              r=["f1", "CRI"] + S5K, w=[("WR", bp)])
            V(lambda e: e.tensor_tensor_scan(out=WI_[:], data0=RB[:, gh, :], data1=f3[:],
                                             initial=sm["CI"][:, gh:gh + 1], op0=OP.mult, op1=OP.add),
              r=["f3", "CRI"] + S5K, w=[("WI", bp)])
            cr_, ci_ = sm["CR"][:, gh:gh + 1], sm["CI"][:, gh:gh + 1]
            cl_, sl_ = sm["cL"][:, gh:gh + 1], sm["sL"][:, gh:gh + 1]
            ta_, tb_ = sm["ta"][:, gh:gh + 1], sm["tb"][:, gh:gh + 1]
            kk = dict(r=[("WR", bp), ("WI", bp), "CRI"] + S5K, w=["CRI"])
            V(lambda e: e.tensor_tensor(out=ta_, in0=WR_[:, 511:512], in1=cl_, op=OP.mult), **kk)
            V(lambda e: e.tensor_tensor(out=tb_, in0=WI_[:, 511:512], in1=sl_, op=OP.mult), **kk)
            V(lambda e: e.tensor_tensor(out=cr_, in0=ta_, in1=tb_, op=OP.subtract), **kk)
            V(lambda e: e.tensor_tensor(out=ta_, in0=WR_[:, 511:512], in1=sl_, op=OP.mult), **kk)
            V(lambda e: e.tensor_tensor(out=tb_, in0=WI_[:, 511:512], in1=cl_, op=OP.mult), **kk)
            V(lambda e: e.tensor_tensor(out=ci_, in0=ta_, in1=tb_, op=OP.add), **kk)
            bt = bb[bp]
            for i, (src, key, tab) in enumerate(((WR_, "WR", TC), (WI_, "WI", TS), (WR_, "WR", TS), (WI_, "WI", TC))):
                PL(lambda e, i=i, src=src, tab=tab: e.tensor_tensor(out=bt[i][:], in0=src[:], in1=tab[:, gh, :],
                                                                    op=OP.mult), r=[(key, bp)] + S5K,
                   w=[("bb", bp, i)])

        def s5_cproj(s, b, gh):
            u = b * 2 + gh
            bp = u % 2
            bt = bb[bp]
            for i, (Wc, wkey) in enumerate(((CRP, "CRP"), (CRN, "CRN"), (CIN, "CIN"), (CIN, "CIN"))):
                T(lambda e, i=i, Wc=Wc: e.matmul(B[4][:], lhsT=Wc[:, gh, :], rhs=bt[i][:],
                                                 start=(gh == 0 and i == 0), stop=(gh == 1 and i == 3)),
                  r=[wkey, ("bb", bp, i)], w=[("B", 4)])

        def s5_out(s, b):
            t0 = b * 512
            sl = slice(t0, t0 + 512)
            yp = b % 2
            V(lambda e: e.scalar_tensor_tensor(out=YS[yp][64:128, :], in0=UT[64:128, sl], scalar=DSK[64:128, 0:1],
                                               in1=B[4][64:128, :], op0=OP.mult, op1=OP.add),
              r=[("UT", b), "DSK", ("B", 4)], w=[("YS", yp)])
            if fused:
                tok = s * S + t0
                SY(lambda e, tok=tok: e.dma_start(out=snd[tok // TB][64:128, tok % TB:tok % TB + 512],
                                                  in_=YS[yp][64:128, :]), r=[("YS", yp)], w=["snd"])
            else:
                SY(lambda e: e.dma_start(out=ysT[:, s * S + t0:s * S + t0 + 512], in_=YS[yp][64:128, :]),
                   r=[("YS", yp)], w=["ysT"])

        for s in range(2):
            P.ops.extend(capture(inproj_s1, s, 0))
            for b in range(16):
                la = capture(inproj_s2, s, b)
                lb = capture(inproj_s1, s, b + 1) if b + 1 < 16 else []
                merge(la, lb)
            tiles = att_tiles()
            units = [(b, gh) for b in range(16) for gh in range(2)]
            nt = len(tiles)
            nu = len(units)
            per = (nt + nu - 1) // nu
            ti = 0
            for k in range(nu + 3):
                if 0 <= k - 2 < nu and units[k - 2][1] == 1:
                    s5_out(s, units[k - 2][0])
                if k < nu:
                    s5_front(s, *units[k])
                hi = min(nt, (k + 1) * per) if k < nu - 1 else nt
                while ti < hi:
                    att_qk(s, ti, *tiles[ti])
                    if ti >= 1:
                        att_pv(s, ti - 1, *tiles[ti - 1])
                    ti += 1
                if ti == nt and k == nu - 1:
                    att_pv(s, nt - 1, *tiles[nt - 1])
                if 0 <= k - 1 < nu:
                    s5_cproj(s, *units[k - 1])
        P.emit(barrier=fused)
    return nc


def build_b(nc=None, rcv=None, sem_stack=None, shared=None):
    fused = nc is not None
    if nc is None:
        nc = bass.Bass("TRN2", target_bir_lowering=False)
    dt0 = nc.dram_tensor
    pre = "b_" if fused else ""

    def dt(name, *a, **k):
        return dt0(pre + name, *a, **k)
    xT = dt("xT", [D, TB], F32, kind="ExternalInput").ap()
    if fused:
        lsel = dt("lsel", [128, 8 * 4 * 128], F32, kind="ExternalInput").ap()
    else:
        yaT = dt("yaT", [512, TB], BF16, kind="ExternalInput").ap()
        ysT = dt("ysT", [512, TB], BF16, kind="ExternalInput").ap()
    wgt = dt("wgt", [D, 2048], F32, kind="ExternalInput").ap()
    g1 = dt("g1", [128, 8], F32, kind="ExternalInput").ap()
    g2 = dt("g2", [128, 8], F32, kind="ExternalInput").ap()
    wglu = dt("wglu", [512, 512], F32, kind="ExternalInput").ap()
    wpa = dt("wpa", [512, D], F32, kind="ExternalInput").ap()
    wps = dt("wps", [512, D], F32, kind="ExternalInput").ap()
    wout = dt("wout", [D, D], F32, kind="ExternalInput").ap()
    wr = dt("wr", [D, 36], F32, kind="ExternalInput").ap()
    br = dt("br", [128, 36], F32, kind="ExternalInput").ap()
    weg = dt("weg", [32, D, 256], F32, kind="ExternalInput").ap()
    weu = dt("weu", [32, D, 256], F32, kind="ExternalInput").ap()
    wed = dt("wed", [32, 256, D], F32, kind="ExternalInput").ap()
    onesb = dt("onesb", [128, 128], F32, kind="ExternalInput").ap()
    identf = dt("identf", [128, 128], F32, kind="ExternalInput").ap()
    sel = dt("sel", [32, 32 * 128], F32, kind="ExternalInput").ap()
    outT = dt("outT", [D, TB], F32, kind="ExternalOutput").ap()

    P = Prog(nc, sem_stack=sem_stack, shared=shared)
    V, A, T, G, SY, PL, VQ = _helpers(P)
    with contextlib.ExitStack() as st:
        def sb(name, shape, dtype):
            return st.enter_context(nc.sbuf_tensor(pre + name, shape, dtype))

        def pp(name, shape, dtype=F32):
            return st.enter_context(nc.psum_tensor(pre + name, shape, dtype))
        X = sb("X", [128, 8, TB], F32)
        WG = sb("WG", [128, 8, 2048], BF16)
        H2 = WG
        YY = sb("YY", [128, 2, 4, 512], BF16)
        YAs = YY[:, 0]
        YSs = YY[:, 1]
        WGLU = sb("WGLU", [128, 4, 512], BF16)
        WPA = sb("WPA", [128, 4, D], BF16)
        WPS = sb("WPS", [128, 4, D], BF16)
        WOUT = sb("WOUT", [128, 8, D], BF16)
        WR = sb("WR", [128, 8, 36], F32)
        BR = sb("BR", [128, 36], F32)
        G1 = sb("G1", [128, 8], F32)
        G2 = sb("G2", [128, 8], F32)
        ONB = sb("ONB", [128, 128], BF16)
        IDF = sb("IDF", [128, 128], F32)
        XSQ = sb("XSQ", [128, 8, 512], BF16)
        XBF = sb("XBF", [128, 8, 512], BF16)
        MIX = XSQ
        SELt = WPS[0:32].rearrange("p a b -> p (a b)")
        WEG = [WOUT[:, :, 0:256], XBF[:, :, 0:256]]
        WEU = [WOUT[:, :, 256:512], XBF[:, :, 256:512]]
        WED = [WPA[:, 0:2, :], YY[:, 0].rearrange("p a b -> p (a b)").rearrange("p (f n) -> p f n", f=2)]
        WKEY = [["WOUT", "WOUT", "WPA"], ["XBF", "XBF", "YY"]]
        CT = sb("CT", [32, TB], BF16)
        H2F = sb("H2F", [128, 8, 128], F32)
        RS = sb("RS", [128, 512], F32)
        RSTD = sb("RSTD", [128, 512], F32)
        YG = sb("YG", [128, 4, 512], BF16)
        fa = [sb("fa%d" % i, [128, 512], F32) for i in range(2)]
        fb = [sb("fb%d" % i, [128, 512], F32) for i in range(2)]
        fc = [sb("fc%d" % i, [128, 512], BF16) for i in range(2)]
        HS = [sb("HS%d" % i, [128, 2, 512], BF16) for i in range(2)]
        LG = sb("LG", [128, 4, 36], F32)
        MK = sb("MK", [128, 4, 32], F32)
        CM = sb("CM", [128, 4, 32], F32)
        CM2 = sb("CM2", [128, 4, 32], F32)
        T8 = sb("T8", [128, 4, 8], F32)
        sc1 = {n: sb("sc_" + n, [128, 4], F32) for n in ["gmax", "ngmax", "gsum", "gtop", "d", "s1", "w1", "w2"]}
        sc4 = {n: sb("sc4_" + n, [128, 4, 4], F32) for n in ["mg", "nb", "eg"]}
        B = [pp("B%d" % i, [128, 512]) for i in range(8)]
        if fused:
            LSEL = sb("LSEL", [128, 8, 4, 128], BF16)
            GST = [sb("GST%d" % i, [128, 512], F32) for i in range(2)]
            GSB = [sb("GSB%d" % i, [128, 512], BF16) for i in range(2)]

        for k in range(8):
            SY(lambda e, k=k: e.dma_start(out=X[:, k, :], in_=xT[k * 128:(k + 1) * 128, :]), w=[("X", k)])
        if fused:
            G(lambda e: e.dma_start(out=LSEL[:].rearrange("p a b c -> p (a b c)"), in_=lsel), w=["LSEL"])

        def gather_block(b):
            cnt_ = 0
            for q in range(4):
                for ii in range(2):
                    i = 2 * q + ii
                    for j in range(8):
                        si = cnt_ % 2
                        cnt_ += 1
                        c0 = b * 512
                        SY(lambda e, i=i, j=j, c0=c0, si=si: e.dma_start(out=GST[si][:],
                                                                         in_=rcv[j][i, :, c0:c0 + 512]),
                           w=[("GST", si)])
                        A(lambda e, si=si: e.activation(out=GSB[si][:], in_=GST[si][:], func=AF.Copy),
                          r=[("GST", si)], w=[("GSB", si)])
                        first = (ii == 0 and j == 0)
                        last = (ii == 1 and j == 7)
                        T(lambda e, j=j, ii=ii, si=si, first=first, last=last: e.matmul(
                            B[1][:], lhsT=LSEL[:, j, ii, :], rhs=GSB[si][:], start=first, stop=last),
                          r=["LSEL", ("GSB", si)], w=[("B", 1)])
                        T(lambda e, j=j, ii=ii, si=si, first=first, last=last: e.matmul(
                            B[2][:], lhsT=LSEL[:, j, 2 + ii, :], rhs=GSB[si][:], start=first, stop=last),
                          r=["LSEL", ("GSB", si)], w=[("B", 2)])
                V(lambda e, q=q: e.tensor_copy(out=YAs[:, q, :], in_=B[1][:]), r=[("B", 1)], w=[("YY", 0)])
                V(lambda e, q=q: e.tensor_copy(out=YSs[:, q, :], in_=B[2][:]), r=[("B", 2)], w=[("YY", 1)])

        def ldw(dst, src, key):
            G(lambda e: e.dma_start(out=dst, in_=src), w=[key])
        for (dst, src, key) in [(G1[:], g1, "G1"), (ONB[:], onesb, "ONB")]:
            ldw(dst, src, key)
        for k in range(8):
            ldw(WG[:, k, :], wgt[k * 128:(k + 1) * 128, :], ("WG", k))
        ldw(WGLU[:], wglu.rearrange("(k p) n -> p k n", p=128), "WGLU")
        ldw(WPA[:], wpa.rearrange("(k p) n -> p k n", p=128), "WPA")
        ldw(WPS[:], wps.rearrange("(k p) n -> p k n", p=128), "WPS")
        ldw(WOUT[:], wout.rearrange("(k p) n -> p k n", p=128), "WOUT")
        ldw(WR[:], wr.rearrange("(k p) n -> p k n", p=128), "WR")
        for (dst, src, key) in [(BR[:], br, "BR"), (G2[:], g2, "G2"), (IDF[:], identf, "IDF")]:
            ldw(dst, src, key)
        for k in range(8):
            V(lambda e, k=k: e.tensor_scalar(out=WG[:, k, :], in0=WG[:, k, :], scalar1=G1[:, k:k + 1], scalar2=None,
                                             op0=OP.mult), r=["G1", ("WG", k)], w=[("WG", k)])

        def rmsstat(sl, bank):
            for k in range(8):
                A(lambda e, k=k: e.activation(out=XSQ[:, k, :], in_=X[:, k, sl], func=AF.Square), r=[("X", k)],
                  w=[("XSQ", k)])
            for k in range(8):
                T(lambda e, k=k: e.matmul(B[bank][:], lhsT=ONB[:], rhs=XSQ[:, k, :], start=(k == 0), stop=(k == 7)),
                  r=[("XSQ", k), "ONB"], w=[("B", bank)])
            A(lambda e: e.activation(out=RS[:], in_=B[bank][:], func=AF.Sqrt, bias=EPS, scale=1.0 / D),
              r=[("B", bank)], w=["RS"])
            V(lambda e: e.reciprocal(out=RSTD[:], in_=RS[:]), r=["RS"], w=["RSTD"])

        for b in range(4):
            sl = slice(b * 512, (b + 1) * 512)
            if fused:
                gather_block(b)
            else:
                G(lambda e, sl=sl: e.dma_start(out=YAs, in_=yaT.rearrange("(k p) t -> p k t", p=128)[:, :, sl]),
                  w=[("YY", 0)])
                G(lambda e, sl=sl: e.dma_start(out=YSs, in_=ysT.rearrange("(k p) t -> p k t", p=128)[:, :, sl]),
                  w=[("YY", 1)])
            for k in range(8):
                PL(lambda e, k=k, sl=sl: e.tensor_copy(out=XBF[:, k, :], in_=X[:, k, sl]), r=[("X", k)],
                   w=[("XBF", k)])
            rmsstat(sl, 0)
            for m in range(4):
                q = m % 2
                V(lambda e, m=m, q=q: e.tensor_tensor(out=fa[q][:], in0=YSs[:, m, :], in1=YSs[:, m, :], op=OP.mult),
                  r=[("YY", 1)], w=[("fa", q)])
                V(lambda e, q=q: e.tensor_scalar(out=fa[q][:], in0=fa[q][:], scalar1=0.044715, scalar2=1.0,
                                                 op0=OP.mult, op1=OP.add), r=[("fa", q)], w=[("fa", q)])
                V(lambda e, m=m, q=q: e.tensor_tensor(out=fa[q][:], in0=fa[q][:], in1=YSs[:, m, :], op=OP.mult),
                  r=[("fa", q), ("YY", 1)], w=[("fa", q)])
                A(lambda e, q=q: e.activation(out=fb[q][:], in_=fa[q][:], func=AF.Sigmoid,
                                              scale=1.5957691216057308), r=[("fa", q)], w=[("fb", q)])
                V(lambda e, m=m, q=q: e.tensor_tensor(out=YSs[:, m, :], in0=YSs[:, m, :], in1=fb[q][:], op=OP.mult),
                  r=[("fb", q), ("YY", 1)], w=[("YY", 1)])
            for m in range(4):
                bk = 1 + (m % 2)
                for k in range(4):
                    T(lambda e, m=m, k=k, bk=bk: e.matmul(B[bk][:], lhsT=WGLU[:, k, m * 128:(m + 1) * 128],
                                                          rhs=YSs[:, k, :], start=(k == 0), stop=(k == 3)),
                      r=["WGLU", ("YY", 1)], w=[("B", bk)])
                A(lambda e, bk=bk, m=m: e.activation(out=fa[m % 2][:], in_=B[bk][:], func=AF.Sigmoid),
                  r=[("B", bk)], w=[("fa", m % 2)])
                V(lambda e, m=m: e.tensor_tensor(out=YG[:, m, :], in0=YSs[:, m, :], in1=fa[m % 2][:], op=OP.mult),
                  r=[("YY", 1), ("fa", m % 2)], w=[("YG", m)])
            for n in range(8):
                q = n % 2
                b0, b1, b2, b3 = (0 + 4 * q, 1 + 4 * q, 2 + 4 * q, 3 + 4 * q)
                for k in range(8):
                    T(lambda e, n=n, k=k, b0=b0: e.matmul(B[b0][:], lhsT=WG[:, k, n * 128:(n + 1) * 128],
                                                          rhs=XBF[:, k, :], start=(k == 0), stop=(k == 7)),
                      r=[("WG", k), ("XBF", k)], w=[("B", b0)])
                for k in range(8):
                    T(lambda e, n=n, k=k, b1=b1: e.matmul(B[b1][:],
                                                          lhsT=WG[:, k, 1024 + n * 128:1024 + (n + 1) * 128],
                                                          rhs=XBF[:, k, :], start=(k == 0), stop=(k == 7)),
                      r=[("WG", k), ("XBF", k)], w=[("B", b1)])
                for k in range(4):
                    T(lambda e, n=n, k=k, b2=b2: e.matmul(B[b2][:], lhsT=WPA[:, k, n * 128:(n + 1) * 128],
                                                          rhs=YAs[:, k, :], start=(k == 0), stop=(k == 3)),
                      r=["WPA", ("YY", 0)], w=[("B", b2)])
                for k in range(4):
                    T(lambda e, n=n, k=k, b3=b3: e.matmul(B[b3][:], lhsT=WPS[:, k, n * 128:(n + 1) * 128],
                                                          rhs=YG[:, k, :], start=(k == 0), stop=(k == 3)),
                      r=["WPS", ("YG", k)], w=[("B", b3)])
                V(lambda e, q=q, b0=b0: e.tensor_tensor(out=fa[q][:], in0=B[b0][:], in1=RSTD[:], op=OP.mult),
                  r=[("B", b0), "RSTD"], w=[("fa", q)])
                A(lambda e, q=q: e.activation(out=fa[q][:], in_=fa[q][:], func=AF.Sigmoid), r=[("fa", q)],
                  w=[("fa", q)])
                V(lambda e, q=q, b1=b1: e.tensor_tensor(out=fb[q][:], in0=B[b1][:], in1=RSTD[:], op=OP.mult),
                  r=[("B", b1), "RSTD"], w=[("fb", q)])
                A(lambda e, q=q: e.activation(out=fb[q][:], in_=fb[q][:], func=AF.Sigmoid), r=[("fb", q)],
                  w=[("fb", q)])
                V(lambda e, q=q, b2=b2: e.tensor_tensor(out=fa[q][:], in0=B[b2][:], in1=fa[q][:], op=OP.mult),
                  r=[("B", b2), ("fa", q)], w=[("fa", q)])
                V(lambda e, q=q, b3=b3: e.tensor_tensor(out=fb[q][:], in0=B[b3][:], in1=fb[q][:], op=OP.mult),
                  r=[("B", b3), ("fb", q)], w=[("fb", q)])
                PL(lambda e, n=n, q=q: e.tensor_tensor(out=MIX[:, n, :], in0=fa[q][:], in1=fb[q][:], op=OP.add),
                   r=[("fa", q), ("fb", q)], w=[("XSQ", n)])
            for n in range(8):
                bk = n % 4
                for k in range(8):
                    T(lambda e, n=n, k=k, bk=bk: e.matmul(B[bk][:], lhsT=WOUT[:, k, n * 128:(n + 1) * 128],
                                                          rhs=MIX[:, k, :], start=(k == 0), stop=(k == 7)),
                      r=["WOUT", ("XSQ", k)], w=[("B", bk)])
                V(lambda e, n=n, sl=sl, bk=bk: e.tensor_tensor(out=X[:, n, sl], in0=X[:, n, sl], in1=B[bk][:],
                                                               op=OP.add), r=[("X", n), ("B", bk)], w=[("X", n)])

        def load_expert(ex):
            p = ex % 2
            G(lambda e: e.dma_start(out=WEG[p], in_=weg[ex].rearrange("(k p) n -> p k n", p=128)), w=[WKEY[p][0]])
            G(lambda e: e.dma_start(out=WEU[p], in_=weu[ex].rearrange("(k p) n -> p k n", p=128)), w=[WKEY[p][1]])
            G(lambda e: e.dma_start(out=WED[p], in_=wed[ex].rearrange("(k p) n -> p k n", p=128)), w=[WKEY[p][2]])
        ldw(SELt, sel, "WPS")
        load_expert(0)
        load_expert(1)

        for b in range(4):
            sl = slice(b * 512, (b + 1) * 512)
            rmsstat(sl, 0)
            for tt in range(4):
                ts_ = slice(tt * 128, (tt + 1) * 128)
                gs_ = slice(b * 512 + tt * 128, b * 512 + (tt + 1) * 128)
                for n in range(8):
                    V(lambda e, n=n, gs_=gs_, ts_=ts_: e.scalar_tensor_tensor(
                        out=H2F[:, n, :], in0=X[:, n, gs_], scalar=G2[:, n:n + 1], in1=RSTD[:, ts_], op0=OP.mult,
                        op1=OP.mult), r=[("X", n), "G2", "RSTD"], w=[("H2F", n)])
                    PL(lambda e, n=n, gs_=gs_: e.tensor_copy(out=H2[:, n, gs_], in_=H2F[:, n, :]), r=[("H2F", n)],
                       w=[("WG", n)])
                for k in range(8):
                    T(lambda e, k=k, tt=tt: e.matmul(B[1 + tt][:, 0:36], lhsT=H2F[:, k, :], rhs=WR[:, k, :],
                                                     start=(k == 0), stop=(k == 7)), r=[("H2F", k), "WR"],
                      w=[("B", 1 + tt)])
            steps = []

            def chain(tt):
                RT = dict(r=[("RT", tt)], w=[("RT", tt)])
                LGt, MKt, CMt, CM2t, T8t = LG[:, tt, :], MK[:, tt, :], CM[:, tt, :], CM2[:, tt, :], T8[:, tt, :]
                g = {n: t[:, tt:tt + 1] for n, t in sc1.items()}
                eg, mg, nb_ = sc4["eg"][:, tt, :], sc4["mg"][:, tt, :], sc4["nb"][:, tt, :]
                ops = []
                ops.append(lambda: V(lambda e: e.tensor_tensor(out=LGt, in0=B[1 + tt][:, 0:36], in1=BR[:], op=OP.add),
                                     r=[("B", 1 + tt), "BR", ("RT", tt)], w=[("RT", tt)]))
                ops.append(lambda: V(lambda e: e.tensor_reduce(out=g["gmax"], in_=LGt[:, 0:4], axis=AX.X, op=OP.max),
                                     **RT))
                ops.append(lambda: V(lambda e: e.tensor_scalar(out=g["ngmax"], in0=g["gmax"], scalar1=-1.0,
                                                               scalar2=None, op0=OP.mult), **RT))
                ops.append(lambda: A(lambda e: e.activation(out=eg, in_=LGt[:, 0:4], func=AF.Exp, bias=g["ngmax"],
                                                            scale=1.0), **RT))
                ops.append(lambda: V(lambda e: e.tensor_reduce(out=g["gsum"], in_=eg, axis=AX.X, op=OP.add), **RT))
                ops.append(lambda: V(lambda e: e.reciprocal(out=g["gtop"], in_=g["gsum"]), **RT))
                ops.append(lambda: V(lambda e: e.tensor_scalar(out=mg, in0=LGt[:, 0:4], scalar1=g["gmax"],
                                                               scalar2=None, op0=OP.is_equal), **RT))
                ops.append(lambda: V(lambda e: e.tensor_scalar(out=nb_, in0=mg, scalar1=-1.0, scalar2=1e30,
                                                               op0=OP.add, op1=OP.mult), **RT))
                for gi in range(4):
                    ops.append(lambda gi=gi: V(lambda e: e.tensor_scalar(
                        out=MKt[:, gi * 8:(gi + 1) * 8], in0=LGt[:, 4 + gi * 8:4 + (gi + 1) * 8],
                        scalar1=mg[:, gi:gi + 1], scalar2=nb_[:, gi:gi + 1], op0=OP.mult, op1=OP.add), **RT))
                ops.append(lambda: V(lambda e: e.max(out=T8t, in_=MKt), **RT))
                ops.append(lambda: V(lambda e: e.tensor_tensor(out=g["d"], in0=T8t[:, 1:2], in1=T8t[:, 0:1],
                                                               op=OP.subtract), **RT))
                ops.append(lambda: A(lambda e: e.activation(out=g["s1"], in_=g["d"], func=AF.Exp), **RT))
                ops.append(lambda: V(lambda e: e.tensor_scalar(out=g["s1"], in0=g["s1"], scalar1=1.0, scalar2=None,
                                                               op0=OP.add), **RT))
                ops.append(lambda: V(lambda e: e.reciprocal(out=g["s1"], in_=g["s1"]), **RT))
                ops.append(lambda: V(lambda e: e.tensor_tensor(out=g["w1"], in0=g["s1"], in1=g["gtop"], op=OP.mult),
                                     **RT))
                ops.append(lambda: V(lambda e: e.tensor_tensor(out=g["w2"], in0=g["gtop"], in1=g["w1"],
                                                               op=OP.subtract), **RT))
                ops.append(lambda: V(lambda e: e.tensor_scalar(out=CMt, in0=MKt, scalar1=T8t[:, 0:1],
                                                               scalar2=g["w1"], op0=OP.is_equal, op1=OP.mult), **RT))
                ops.append(lambda: V(lambda e: e.tensor_scalar(out=CM2t, in0=MKt, scalar1=T8t[:, 1:2],
                                                               scalar2=g["w2"], op0=OP.is_equal, op1=OP.mult), **RT))
                ops.append(lambda: V(lambda e: e.tensor_tensor(out=CMt, in0=CMt, in1=CM2t, op=OP.add), **RT))
                ops.append(lambda: T(lambda e: e.transpose(B[5][0:32, tt * 128:(tt + 1) * 128], CMt, IDF[:]),
                                     r=[("RT", tt), "IDF"], w=[("B", 5)]))
                return ops
            chains = [chain(tt) for tt in range(4)]
            for i in range(len(chains[0])):
                for tt in range(4):
                    chains[tt][i]()
            V(lambda e, b=b: e.tensor_copy(out=CT[:, b * 512:(b + 1) * 512], in_=B[5][0:32, :]), r=[("B", 5)],
              w=[("CT", b)])

        units = [(ex, b) for ex in range(32) for b in range(4)]

        def up(ui):
            ex, b = units[ui]
            p = ex % 2
            hp = ui % 2
            sl = slice(b * 512, (b + 1) * 512)
            T(lambda e: e.matmul(B[0][:], lhsT=SELt[:, ex * 128:(ex + 1) * 128], rhs=CT[:, sl], start=True,
                                 stop=True), r=["WPS", ("CT", b)], w=[("B", 0)])
            A(lambda e: e.activation(out=fc[hp][:], in_=B[0][:], func=AF.Copy), r=[("B", 0)], w=[("fc", hp)])
            for f in range(2):
                bg, bu = 1 + 2 * f, 2 + 2 * f
                for k in range(8):
                    T(lambda e, f=f, k=k, bg=bg: e.matmul(B[bg][:], lhsT=WEG[p][:, k, f * 128:(f + 1) * 128],
                                                          rhs=H2[:, k, sl], start=(k == 0), stop=(k == 7)),
                      r=[WKEY[p][0], ("WG", k)], w=[("B", bg)])
                for k in range(8):
                    T(lambda e, f=f, k=k, bu=bu: e.matmul(B[bu][:], lhsT=WEU[p][:, k, f * 128:(f + 1) * 128],
                                                          rhs=H2[:, k, sl], start=(k == 0), stop=(k == 7)),
                      r=[WKEY[p][1], ("WG", k)], w=[("B", bu)])
                A(lambda e, f=f, bg=bg: e.activation(out=fa[f][:], in_=B[bg][:], func=AF.Silu), r=[("B", bg)],
                  w=[("fa", f)])
                V(lambda e, f=f, bu=bu: e.tensor_tensor(out=fb[f][:], in0=B[bu][:], in1=fc[hp][:], op=OP.mult),
                  r=[("B", bu), ("fc", hp)], w=[("fb", f)])
                PL(lambda e, f=f: e.tensor_tensor(out=HS[hp][:, f, :], in0=fa[f][:], in1=fb[f][:], op=OP.mult),
                   r=[("fa", f), ("fb", f)], w=[("HS", hp, f)])

        def down(ui):
            ex, b = units[ui]
            p = ex % 2
            hp = ui % 2
            sl = slice(b * 512, (b + 1) * 512)
            for n in range(8):
                bk = 5 + (n % 3)
                for f in range(2):
                    T(lambda e, n=n, f=f, bk=bk: e.matmul(B[bk][:], lhsT=WED[p][:, f, n * 128:(n + 1) * 128],
                                                          rhs=HS[hp][:, f, :], start=(f == 0), stop=(f == 1)),
                      r=[WKEY[p][2], ("HS", hp, f)], w=[("B", bk)])
                V(lambda e, n=n, bk=bk: e.tensor_tensor(out=X[:, n, sl], in0=X[:, n, sl], in1=B[bk][:], op=OP.add),
                  r=[("X", n), ("B", bk)], w=[("X", n)])
            if b == 3 and ex + 2 < 32:
                load_expert(ex + 2)
        for ui in range(len(units)):
            up(ui)
            if ui >= 1:
                down(ui - 1)
        down(len(units) - 1)
        for k in range(8):
            SY(lambda e, k=k: e.dma_start(out=outT[k * 128:(k + 1) * 128, :], in_=X[:, k, :]), r=[("X", k)],
               w=["outT"])
        P.emit()
    return nc


def _consts():
    onesb = np.ones((128, 128), np.float32)
    ident = np.eye(128, dtype=np.float32)
    iota1 = np.tile(np.arange(1, 513, dtype=np.float32)[None, :], (128, 1))
    p = np.arange(128)
    maskd = np.where(p[:, None] > p[None, :], -30000.0, 0.0).astype(np.float32)
    sel = np.zeros((32, 32, 128), np.float32)
    for e in range(32):
        sel[e, e, :] = 1.0
    return onesb, ident, iota1, maskd, sel.reshape(32, 32 * 128)


def stage_a_inputs(x, norm_mix_g, w_in, b_forget, q_norm_g, k_norm_g, ssm_A_re, ssm_A_im, ssm_log_dt, ssm_B_re,
                   ssm_B_im, ssm_C_re, ssm_C_im, ssm_D):
    onesb, ident, iota1, maskd, _ = _consts()
    xT = np.ascontiguousarray(x.reshape(NTOK, D).T)
    w = w_in[0]
    g1 = np.ascontiguousarray(norm_mix_g[0].reshape(8, 128).T)
    maps = []
    for c in range(NCORES):
        wq = np.ascontiguousarray(w[:, c * 64:(c + 1) * 64])
        wk = np.ascontiguousarray(np.concatenate([w[:, 512 + c * 64:512 + (c + 1) * 64],
                                                  w[:, 1536 + c:1537 + c]], axis=1))
        wv = w[:, 1024 + c * 64:1024 + (c + 1) * 64]
        wu = w[:, 1544 + c * 64:1544 + (c + 1) * 64]
        wvu = np.ascontiguousarray(np.concatenate([wv, wu], axis=1))
        nbf = np.full((65, 2), b_forget[0, c], np.float32)
        gqk = np.ascontiguousarray(np.stack([q_norm_g[0], k_norm_g[0]], axis=1)).astype(np.float32)
        gs = np.arange(4 * c, 4 * c + 4)
        are = np.zeros((128, 2), np.float32)
        aim = np.zeros((128, 2), np.float32)
        ldt = np.zeros((128, 2), np.float32)
        brp = np.zeros((64, 2, 128), np.float32)
        bip = np.zeros((64, 2, 128), np.float32)
        crp = np.zeros((128, 2, 128), np.float32)
        cip = np.zeros((128, 2, 128), np.float32)
        for gh in range(2):
            for gl in range(2):
                gloc = 2 * gh + gl
                g = gs[gloc]
                are[gl * 64:(gl + 1) * 64, gh] = ssm_A_re[0, g]
                aim[gl * 64:(gl + 1) * 64, gh] = ssm_A_im[0, g]
                ldt[gl * 64:(gl + 1) * 64, gh] = ssm_log_dt[0, g]
                brp[gloc * 16:(gloc + 1) * 16, gh, gl * 64:(gl + 1) * 64] = ssm_B_re[0, g].T
                bip[gloc * 16:(gloc + 1) * 16, gh, gl * 64:(gl + 1) * 64] = ssm_B_im[0, g].T
                crp[gl * 64:(gl + 1) * 64, gh, 64 + gloc * 16:64 + (gloc + 1) * 16] = ssm_C_re[0, g].T
                cip[gl * 64:(gl + 1) * 64, gh, 64 + gloc * 16:64 + (gloc + 1) * 16] = ssm_C_im[0, g].T
        dsk = np.ascontiguousarray(np.repeat(ssm_D[0, c * 64:(c + 1) * 64][:, None], 2, axis=1)).astype(np.float32)
        maps.append(dict(xT=xT, wq=wq, wk=wk, wvu=wvu, g1=g1, nbf=nbf, gqk=gqk, are=are, aim=aim, ldt=ldt,
                         brp=brp, bip=bip, crp=crp, cip=cip, dsk=dsk, onesb=onesb, identb=ident, iota1=iota1,
                         maskd=maskd))
    return maps


def stage_b_inputs(x, ya_full, ys_full, norm_mix_g, w_in, w_glu, w_proj_attn, w_proj_ssm, w_out, norm_ffn_g,
                   w_router_group, b_router_group, w_router_expert, b_router_expert, w_expert_gate, w_expert_up,
                   w_expert_down):
    onesb, ident, iota1, maskd, sel = _consts()
    xf = x.reshape(NTOK, D)
    g1 = np.ascontiguousarray(norm_mix_g[0].reshape(8, 128).T)
    g2 = np.ascontiguousarray(norm_ffn_g[0].reshape(8, 128).T)
    wgt = np.ascontiguousarray(w_in[0][:, 2056:4104])
    wr = np.ascontiguousarray(np.concatenate([w_router_group[0], w_router_expert[0]], axis=1))
    br = np.ascontiguousarray(np.tile(np.concatenate([b_router_group[0], b_router_expert[0]])[None, :], (128, 1)))
    maps = []
    for c in range(NCORES):
        tsl = slice(c * TB, (c + 1) * TB)
        maps.append(dict(xT=np.ascontiguousarray(xf[tsl].T),
                         yaT=None if ya_full is None else np.ascontiguousarray(ya_full[:, tsl]),
                         ysT=None if ys_full is None else np.ascontiguousarray(ys_full[:, tsl]), wgt=wgt, g1=g1, g2=g2, wglu=w_glu[0],
                         wpa=w_proj_attn[0], wps=w_proj_ssm[0], wout=w_out[0], wr=wr, br=br, weg=w_expert_gate[0],
                         weu=w_expert_up[0], wed=w_expert_down[0], onesb=onesb, identf=ident, sel=sel))
    return maps


def run_a(inputs):
    nc = build_a()
    keys = ["x", "norm_mix_g", "w_in", "b_forget", "q_norm_g", "k_norm_g", "ssm_A_re", "ssm_A_im", "ssm_log_dt",
            "ssm_B_re", "ssm_B_im", "ssm_C_re", "ssm_C_im", "ssm_D"]
    maps = stage_a_inputs(*[np.asarray(inputs[k], np.float32) for k in keys])
    res = run_bass_kernel_spmd(nc, maps, core_ids=list(range(NCORES)))
    ya = np.concatenate([np.asarray(res.results[c]["yaT"]) for c in range(NCORES)], axis=0)
    ys = np.concatenate([np.asarray(res.results[c]["ysT"]) for c in range(NCORES)], axis=0)
    return ya, ys


def run_b(inputs, ya, ys):
    nc = build_b()
    keys = ["norm_mix_g", "w_in", "w_glu", "w_proj_attn", "w_proj_ssm", "w_out", "norm_ffn_g", "w_router_group",
            "b_router_group", "w_router_expert", "b_router_expert", "w_expert_gate", "w_expert_up", "w_expert_down"]
    maps = stage_b_inputs(np.asarray(inputs["x"], np.float32), ya, ys,
                          *[np.asarray(inputs[k], np.float32) for k in keys])
    res = run_bass_kernel_spmd(nc, maps, core_ids=list(range(NCORES)))
    out = np.concatenate([np.asarray(res.results[c]["outT"]).T for c in range(NCORES)], axis=0)
    return out.reshape(2, S, D).astype(np.float32)


def build_fused():
    nc = bass.Bass("TRN2", target_bir_lowering=False)
    snd_t = [nc.dram_tensor("snd%d" % j, [1024, 256], F32) for j in range(8)]
    rcv_t = [nc.dram_tensor("rcv%d" % j, [8192, 256], F32) for j in range(8)]
    snd = [t.ap().rearrange("(p a) c -> p (a c)", a=8) for t in snd_t]
    rcv = [t.ap().rearrange("(i p a) c -> i p (a c)", i=8, a=8) for t in rcv_t]
    shared = {"sems": None, "base": {}}
    with contextlib.ExitStack() as sem_stack:
        build_a(nc, snd, sem_stack, shared)
        ccs = sem_stack.enter_context(nc.semaphore("ccs"))
        bar = sem_stack.enter_context(nc.semaphore("bar"))
        with nc.Block() as block:
            @block.gpsimd
            def _(g):
                for j in range(8):
                    g.collective_compute("AllGather", OP.bypass, replica_groups=[list(range(NCORES))],
                                         ins=[snd_t[j].ap().opt()], outs=[rcv_t[j].ap().opt()]).then_inc(ccs)
                g.wait_ge(ccs, 8)
                g.sem_inc(bar, 1)

            @block.tensor
            def _(e):
                e.wait_ge(bar, 1)

            @block.vector
            def _(e):
                e.wait_ge(bar, 1)

            @block.scalar
            def _(e):
                e.wait_ge(bar, 1)

            @block.sync
            def _(e):
                e.wait_ge(bar, 1)
        build_b(nc, rcv, sem_stack, shared)
    return nc


def kernel_fused(**inputs):
    nc = build_fused()
    keys_a = ["x", "norm_mix_g", "w_in", "b_forget", "q_norm_g", "k_norm_g", "ssm_A_re", "ssm_A_im", "ssm_log_dt",
              "ssm_B_re", "ssm_B_im", "ssm_C_re", "ssm_C_im", "ssm_D"]
    maps_a = stage_a_inputs(*[np.asarray(inputs[k], np.float32) for k in keys_a])
    keys_b = ["norm_mix_g", "w_in", "w_glu", "w_proj_attn", "w_proj_ssm", "w_out", "norm_ffn_g", "w_router_group",
              "b_router_group", "w_router_expert", "b_router_expert", "w_expert_gate", "w_expert_up", "w_expert_down"]
    maps_b = stage_b_inputs(np.asarray(inputs["x"], np.float32), None, None,
                            *[np.asarray(inputs[k], np.float32) for k in keys_b])
    maps = []
    for c in range(NCORES):
        m = dict(maps_a[c])
        lsel = np.zeros((128, 8, 4, 128), np.float32)
        for k in range(64):
            lsel[k, c, 0, k] = 1.0
            lsel[k, c, 1, 64 + k] = 1.0
            lsel[64 + k, c, 2, k] = 1.0
            lsel[64 + k, c, 3, 64 + k] = 1.0
        m["b_lsel"] = lsel.reshape(128, 8 * 4 * 128)
        for k, v in maps_b[c].items():
            if k in ("yaT", "ysT"):
                continue
            m["b_" + k] = v
        maps.append(m)
    res = run_bass_kernel_spmd(nc, maps, core_ids=list(range(NCORES)))
    out = np.concatenate([np.asarray(res.results[c]["b_outT"]).T for c in range(NCORES)], axis=0)
    return out.reshape(2, S, D).astype(np.float32)


def kernel(**inputs):
    ya, ys = run_a(inputs)
    return run_b(inputs, ya, ys)
```

```python
import contextlib
import numpy as np
import ml_dtypes
import concourse.bass as bass
import concourse.mybir as mybir
from concourse.bass_utils import run_bass_kernel_spmd

F32 = mybir.dt.float32
BF16 = mybir.dt.bfloat16
AF = mybir.ActivationFunctionType
OP = mybir.AluOpType
AX = mybir.AxisListType

NCORES = 8
D = 1024
S = 8192
NTOK = 16384
TB = 2048
EPS = 1e-6
CH = 3000
NDS = 3
CHBIG = 30000
DEBUG_GATHER = False
MAGIC = 12582912.0
INV2PI = 0.15915494309189535
C1 = 6.28125
C2 = 0.0019353071795864769
PI = 3.141592653589793

STREAMS = {'pe': ('tensor', False), 'dve': ('vector', False), 'act': ('scalar', False),
           'pool': ('gpsimd', False), 'gq': ('gpsimd', True), 'sq': ('sync', True), 'vq': ('scalar', True),
           'cc': ('gpsimd', False)}


def _norm(k):
    if isinstance(k, tuple):
        return k[0], k[1:]
    return k, None


class Prog:
    def __init__(self, nc, sem_stack=None, shared=None):
        self.nc = nc
        self.ops = []
        self.sem_stack = sem_stack
        self.shared = shared if shared is not None else {"sems": None, "base": {}}

    def add(self, st, fn, r=(), w=()):
        self.ops.append((st, fn, tuple(r), tuple(w)))

    def emit(self, barrier=False):
        nc = self.nc
        ops = self.ops
        base = self.shared["base"]
        cnt = {}
        idx = []
        for (st, fn, r, w) in ops:
            idx.append(base.get(st, 0) + cnt.get(st, 0))
            cnt[st] = cnt.get(st, 0) + 1
        tot = {st: base.get(st, 0) + cnt[st] for st in cnt}
        writers = {}
        readers = {}
        deps = []

        def conf(a, b):
            return a is None or b is None or a == b
        for i, (st, fn, r, w) in enumerate(ops):
            d = set()
            for k in r:
                name, sub = _norm(k)
                for (s2, o) in writers.get(name, ()):
                    if conf(sub, s2):
                        d.add(o)
            for k in w:
                name, sub = _norm(k)
                for (s2, o) in writers.get(name, ()):
                    if conf(sub, s2):
                        d.add(o)
                for (s2, o) in readers.get(name, ()):
                    if conf(sub, s2):
                        d.add(o)
            for k in w:
                name, sub = _norm(k)
                writers[name] = [(s2, o) for (s2, o) in writers.get(name, []) if not (sub is None or s2 == sub)]
                writers[name].append((sub, i))
                readers[name] = [(s2, o) for (s2, o) in readers.get(name, []) if not conf(sub, s2)]
            for k in r:
                name, sub = _norm(k)
                readers.setdefault(name, []).append((sub, i))
            d.discard(i)
            need = {}
            needd = set()
            for o in d:
                s2 = ops[o][0]
                if STREAMS[s2][1]:
                    needd.add((s2, idx[o]))
                elif need.get(s2, -1) < idx[o]:
                    need[s2] = idx[o]
            deps.append((need, needd))

        with contextlib.ExitStack() as st_:
            if self.shared["sems"] is None:
                sstack = self.sem_stack if self.sem_stack is not None else st_
                sems = {}
                for s in STREAMS:
                    if self.sem_stack is None and s not in cnt:
                        continue
                    if STREAMS[s][1]:
                        sems[s] = [sstack.enter_context(nc.semaphore("d_%s_%d" % (s, i))) for i in range(NDS)]
                    else:
                        nchunk = 1 if self.sem_stack is not None else (cnt[s] // CH + 1)
                        sems[s] = [sstack.enter_context(nc.semaphore("s_%s_%d" % (s, i))) for i in range(nchunk)]
                if self.sem_stack is not None:
                    self.shared["sems"] = sems
            else:
                sems = self.shared["sems"]
            chdiv = CHBIG if self.sem_stack is not None else CH
            block = st_.enter_context(nc.Block())

            def section(ename, eng):
                waited = {}
                waitedd = {}

                def wait_c(s2, j):
                    if waited.get(s2, -1) >= j:
                        return
                    eng.wait_ge(sems[s2][j // chdiv], (j % chdiv) + 1)
                    waited[s2] = j

                def wait_d(s2, j):
                    key = (s2, j % NDS)
                    if waitedd.get(key, -1) >= j:
                        return
                    eng.wait_ge(sems[s2][j % NDS], (j // NDS + 1) * 16)
                    waitedd[key] = j
                for i, (st, fn, r, w) in enumerate(ops):
                    if STREAMS[st][0] != ename:
                        continue
                    need, needd = deps[i]
                    for s2, j in need.items():
                        if st == 'pe' and s2 == 'pe':
                            continue
                        wait_c(s2, j)
                    for (s2, j) in sorted(needd):
                        wait_d(s2, j)
                    j = idx[i]
                    if STREAMS[st][1]:
                        if j >= NDS:
                            wait_d(st, j - NDS)
                        inst = fn(eng)
                        inst.then_inc(sems[st][j % NDS], 16)
                    else:
                        inst = fn(eng)
                        inst.then_inc(sems[st][j // chdiv], 1)
                if ename == 'sync' or barrier:
                    for s2 in cnt:
                        if STREAMS[s2][1]:
                            for j in range(max(0, tot[s2] - NDS), tot[s2]):
                                wait_d(s2, j)
                        else:
                            wait_c(s2, tot[s2] - 1)

            @block.tensor
            def _(eng):
                section('tensor', eng)

            @block.vector
            def _(eng):
                section('vector', eng)

            @block.scalar
            def _(eng):
                section('scalar', eng)

            @block.gpsimd
            def _(eng):
                section('gpsimd', eng)

            @block.sync
            def _(eng):
                section('sync', eng)
        for st in cnt:
            base[st] = tot[st]
        self.ops = []


def _helpers(P):
    def mk(st):
        def f(fn, r=(), w=()):
            P.add(st, fn, r, w)
        return f
    return mk('dve'), mk('act'), mk('pe'), mk('gq'), mk('sq'), mk('pool'), mk('vq')


def build_a(nc=None, snd=None, sem_stack=None, shared=None):
    fused = nc is not None
    if nc is None:
        nc = bass.Bass("TRN2", target_bir_lowering=False)
    dt = nc.dram_tensor
    xT = dt("xT", [D, NTOK], F32, kind="ExternalInput").ap()
    wq = dt("wq", [D, 64], F32, kind="ExternalInput").ap()
    wk = dt("wk", [D, 65], F32, kind="ExternalInput").ap()
    wvu = dt("wvu", [D, 128], F32, kind="ExternalInput").ap()
    g1 = dt("g1", [128, 8], F32, kind="ExternalInput").ap()
    nbf = dt("nbf", [65, 2], F32, kind="ExternalInput").ap()
    gqk = dt("gqk", [64, 2], F32, kind="ExternalInput").ap()
    are = dt("are", [128, 2], F32, kind="ExternalInput").ap()
    aim = dt("aim", [128, 2], F32, kind="ExternalInput").ap()
    ldt = dt("ldt", [128, 2], F32, kind="ExternalInput").ap()
    brp = dt("brp", [64, 2, 128], F32, kind="ExternalInput").ap()
    bip = dt("bip", [64, 2, 128], F32, kind="ExternalInput").ap()
    crp = dt("crp", [128, 2, 128], F32, kind="ExternalInput").ap()
    cip = dt("cip", [128, 2, 128], F32, kind="ExternalInput").ap()
    dsk = dt("dsk", [64, 2], F32, kind="ExternalInput").ap()
    onesb = dt("onesb", [128, 128], F32, kind="ExternalInput").ap()
    identb = dt("identb", [128, 128], F32, kind="ExternalInput").ap()
    iota1 = dt("iota1", [128, 512], F32, kind="ExternalInput").ap()
    maskd = dt("maskd", [128, 128], F32, kind="ExternalInput").ap()
    if not fused:
        yaT = dt("yaT", [64, NTOK], BF16, kind="ExternalOutput").ap()
        ysT = dt("ysT", [64, NTOK], BF16, kind="ExternalOutput").ap()

    P = Prog(nc, sem_stack=sem_stack, shared=shared)
    V, A, T, G, SY, PL, VQ = _helpers(P)
    with contextlib.ExitStack() as st:
        def sb(name, shape, dtype):
            return st.enter_context(nc.sbuf_tensor(name, shape, dtype))

        def pp(name, shape, dtype=F32):
            return st.enter_context(nc.psum_tensor(name, shape, dtype))
        WQ = sb("WQ", [128, 8, 64], BF16)
        WK = sb("WK", [128, 8, 65], BF16)
        WVU = sb("WVU", [128, 8, 128], BF16)
        G1 = sb("G1", [128, 8], F32)
        NBF = sb("NBF", [65, 2], F32)
        GQK = sb("GQK", [64, 2], F32)
        ARE = sb("ARE", [128, 2], F32)
        AIM = sb("AIM", [128, 2], F32)
        LDT = sb("LDT", [128, 2], F32)
        BRP = sb("BRP", [128, 2, 128], BF16)
        BIP = sb("BIP", [128, 2, 128], BF16)
        CRP = sb("CRP", [128, 2, 128], BF16)
        CIP = sb("CIP", [128, 2, 128], BF16)
        CRN = sb("CRN", [128, 2, 128], BF16)
        CIN = sb("CIN", [128, 2, 128], BF16)
        DSK = sb("DSK", [128, 2], F32)
        ONB = sb("ONB", [128, 128], BF16)
        ONF = sb("ONF", [128, 128], F32)
        IDB = sb("IDB", [128, 128], BF16)
        IOT = sb("IOT", [128, 512], F32)
        MSK = sb("MSK", [128, 128], BF16)
        sm = {n: sb("sm_" + n, [128, 2], F32) for n in
              ["dt", "th", "rho", "k", "r", "ar", "sn", "cs", "lr", "li", "nr", "den", "t1", "t2", "cr", "ci",
               "cL", "sL", "CR", "CI", "ta", "tb"]}
        TR = sb("TR", [128, 2, 512], F32)
        TI = sb("TI", [128, 2, 512], F32)
        TC = sb("TC", [128, 2, 512], F32)
        TS = sb("TS", [128, 2, 512], F32)
        RB = sb("RB", [128, 2, 512], F32)
        QA = sb("QA", [128, S], BF16)
        KA = sb("KA", [128, S], BF16)
        VTM = sb("VTM", [128, 64, 128], BF16)
        UT = sb("UT", [128, S], BF16)
        FR = [sb("FR%d" % i, [65, 512], F32) for i in range(2)]
        AR = sb("AR", [65, 512], F32)
        AH = sb("AH", [65, 512], BF16)
        AL = sb("AL", [65, 512], BF16)
        FCOL = sb("FCOL", [128, 64], F32)
        FQ0 = sb("FQ0", [128, 16], F32)
        CB = sb("CB", [128, 16, 64], F32)
        YA = [sb("YA%d" % i, [64, 512], F32 if fused else BF16) for i in range(2)]
        YS = [sb("YS%d" % i, [128, 512], F32 if fused else BF16) for i in range(2)]
        XSQ = sb("XSQ", [128, 8, 512], BF16)
        XBF = [sb("XBF%d" % i, [128, 8, 512], BF16) for i in range(2)]
        RSTD = [sb("RSTD%d" % i, [128, 512], F32) for i in range(2)]
        RS = RSTD
        XST = [sb("XST%d" % i, [128, 512], F32) for i in range(2)]
        QF = [[sb("QF%d_%d" % (i, j), [64, 512], F32) for j in range(2)] for i in range(2)]
        SQ1 = [sb("SQ1%d" % i, [64, 512], BF16) for i in range(2)]
        RQ2 = [sb("RQ2%d" % i, [64, 512], F32) for i in range(2)]
        RQ = RQ2
        VB = [sb("VB%d" % i, [64, 512], BF16) for i in range(2)]
        FF = [sb("FF%d" % i, [65, 512], F32) for i in range(2)]
        FE = sb("FE", [65, 512], F32)
        ONR = sb("ONR", [65, 512], F32)
        PT = [sb("PT%d" % i, [128, 512], BF16) for i in range(2)]
        OS = [sb("OS%d" % i, [65, 512], F32) for i in range(2)]
        f1 = sb("f1", [128, 512], F32)
        f2 = sb("f2", [128, 512], F32)
        f3 = sb("f3", [128, 512], F32)
        f4 = sb("f4", [128, 512], F32)
        PH, PK, PA2 = f1, f2, f3
        WRl = [sb("WRr%d" % i, [128, 512], F32) for i in range(2)]
        WIl = [sb("WIi%d" % i, [128, 512], F32) for i in range(2)]
        bb = [[sb("b%d_%d" % (i, j), [128, 512], BF16) for i in range(4)] for j in range(2)]
        B = [pp("B%d" % i, [128, 512]) for i in range(7)]
        PTB = pp("PTB", [128, 4, 64], BF16)

        def ldw(dst, src, key):
            G(lambda e: e.dma_start(out=dst, in_=src), w=[key])
        ldw(WQ[:], wq.rearrange("(k p) n -> p k n", p=128), "WQ")
        ldw(WK[:], wk.rearrange("(k p) n -> p k n", p=128), "WK")
        ldw(WVU[:], wvu.rearrange("(k p) n -> p k n", p=128), "WVU")
        for (dst, src, key) in [(G1[:], g1, "G1"), (NBF[:], nbf, "NBF"), (GQK[:], gqk, "GQK"), (ARE[:], are, "ARE"),
                                (AIM[:], aim, "AIM"), (LDT[:], ldt, "LDT"), (BRP[64:128], brp, "BRP"),
                                (BIP[64:128], bip, "BIP"), (CRP[:], crp, "CRP"), (CIP[:], cip, "CIP"),
                                (DSK[64:128], dsk, "DSK"), (ONB[:], onesb, "ONB"), (ONF[:], onesb, "ONF"),
                                (IDB[:], identb, "IDB"), (IOT[:], iota1, "IOT"), (MSK[:], maskd, "MSK")]:
            ldw(dst, src, key)

        def xload(gb):
            T0 = gb * 512
            p = gb % 2
            for k in range(8):
                si = (gb * 8 + k) % 4
                if k < 4:
                    G(lambda e, k=k, T0=T0, p=p: e.dma_start(out=XBF[p][:, k, :],
                                                              in_=xT[k * 128:(k + 1) * 128, T0:T0 + 512]),
                      w=[("XBF", p, k)])
                else:
                    si = k % 2
                    SY(lambda e, k=k, T0=T0, si=si: e.dma_start(out=XST[si][:],
                                                                in_=xT[k * 128:(k + 1) * 128, T0:T0 + 512]),
                       w=[("XST", si)])
                    A(lambda e, k=k, p=p, si=si: e.activation(out=XBF[p][:, k, :], in_=XST[si][:], func=AF.Copy),
                      r=[("XST", si)], w=[("XBF", p, k)])
        xload(0)
        for (Wt, key) in ((WQ, "WQ"), (WK, "WK"), (WVU, "WVU")):
            for k in range(8):
                V(lambda e, Wt=Wt, k=k: e.tensor_scalar(out=Wt[:, k, :], in0=Wt[:, k, :], scalar1=G1[:, k:k + 1],
                                                        scalar2=None, op0=OP.mult), r=["G1", key], w=[key])
        V(lambda e: e.tensor_scalar(out=GQK[:, 0:1], in0=GQK[:, 0:1], scalar1=0.125, scalar2=None, op0=OP.mult),
          r=["GQK"], w=["GQK"])
        V(lambda e: e.tensor_scalar(out=NBF[:], in0=NBF[:], scalar1=-1.0, scalar2=None, op0=OP.mult), r=["NBF"],
          w=["NBF"])
        V(lambda e: e.memset(ONR[:], 1.0), w=["ONR"])
        V(lambda e: e.memset(QA[:], 0.0), w=["QA"])
        V(lambda e: e.memset(KA[:], 0.0), w=["KA"])
        V(lambda e: e.memset(KA[64:66, :], 1.0), r=["KA"], w=["KA"])
        V(lambda e: e.memset(VTM[:], 1.0), w=["VTM"])
        V(lambda e: e.tensor_scalar(out=CRN[:], in0=CRP[:], scalar1=-1.0, scalar2=None, op0=OP.mult), r=["CRP"],
          w=["CRN"])
        V(lambda e: e.tensor_scalar(out=CIN[:], in0=CIP[:], scalar1=-1.0, scalar2=None, op0=OP.mult), r=["CIP"],
          w=["CIN"])

        def VS(fn):
            V(fn, r=["S5T", "ARE", "AIM", "LDT", "IOT"], w=["S5T"])

        def AS(fn):
            A(fn, r=["S5T", "LDT"], w=["S5T"])

        def rred(out, in_, k_t):
            VS(lambda e: e.tensor_scalar(out=k_t, in0=in_, scalar1=INV2PI, scalar2=MAGIC, op0=OP.mult, op1=OP.add))
            VS(lambda e: e.tensor_scalar(out=k_t, in0=k_t, scalar1=MAGIC, scalar2=None, op0=OP.subtract))
            VS(lambda e: e.scalar_tensor_tensor(out=out, in0=k_t, scalar=-C1, in1=in_, op0=OP.mult, op1=OP.add))
            VS(lambda e: e.scalar_tensor_tensor(out=out, in0=k_t, scalar=-C2, in1=out, op0=OP.mult, op1=OP.add))
            VS(lambda e: e.tensor_scalar(out=out, in0=out, scalar1=PI, scalar2=-PI, op0=OP.min, op1=OP.max))

        def sincos(sn, cs, r, tmp):
            AS(lambda e: e.activation(out=sn, in_=r, func=AF.Sin))
            VS(lambda e: e.tensor_scalar(out=tmp, in0=r, scalar1=-1.0, scalar2=None, op0=OP.mult))
            VS(lambda e: e.tensor_tensor(out=tmp, in0=tmp, in1=r, op=OP.max))
            VS(lambda e: e.tensor_scalar(out=tmp, in0=tmp, scalar1=-1.0, scalar2=PI / 2, op0=OP.mult, op1=OP.add))
            AS(lambda e: e.activation(out=cs, in_=tmp, func=AF.Sin))
        s_ = {k: v[:] for k, v in sm.items()}
        AS(lambda e: e.activation(out=s_["dt"], in_=LDT[:], func=AF.Exp))
        VS(lambda e: e.tensor_tensor(out=s_["th"], in0=AIM[:], in1=s_["dt"], op=OP.mult))
        VS(lambda e: e.tensor_tensor(out=s_["t1"], in0=ARE[:], in1=s_["dt"], op=OP.mult))
        AS(lambda e: e.activation(out=s_["rho"], in_=s_["t1"], func=AF.Exp))
        rred(s_["r"], s_["th"], s_["k"])
        sincos(s_["sn"], s_["cs"], s_["r"], s_["ar"])
        VS(lambda e: e.tensor_tensor(out=s_["lr"], in0=s_["rho"], in1=s_["cs"], op=OP.mult))
        VS(lambda e: e.tensor_tensor(out=s_["li"], in0=s_["rho"], in1=s_["sn"], op=OP.mult))
        VS(lambda e: e.tensor_scalar(out=s_["nr"], in0=s_["lr"], scalar1=-1.0, scalar2=None, op0=OP.add))
        VS(lambda e: e.tensor_tensor(out=s_["t1"], in0=ARE[:], in1=ARE[:], op=OP.mult))
        VS(lambda e: e.tensor_tensor(out=s_["t2"], in0=AIM[:], in1=AIM[:], op=OP.mult))
        VS(lambda e: e.tensor_tensor(out=s_["den"], in0=s_["t1"], in1=s_["t2"], op=OP.add))
        VS(lambda e: e.reciprocal(out=s_["den"], in_=s_["den"]))
        VS(lambda e: e.tensor_tensor(out=s_["t1"], in0=s_["nr"], in1=ARE[:], op=OP.mult))
        VS(lambda e: e.tensor_tensor(out=s_["t2"], in0=s_["li"], in1=AIM[:], op=OP.mult))
        VS(lambda e: e.tensor_tensor(out=s_["t1"], in0=s_["t1"], in1=s_["t2"], op=OP.add))
        VS(lambda e: e.tensor_tensor(out=s_["cr"], in0=s_["t1"], in1=s_["den"], op=OP.mult))
        VS(lambda e: e.tensor_tensor(out=s_["t1"], in0=s_["li"], in1=ARE[:], op=OP.mult))
        VS(lambda e: e.tensor_tensor(out=s_["t2"], in0=s_["nr"], in1=AIM[:], op=OP.mult))
        VS(lambda e: e.tensor_tensor(out=s_["t1"], in0=s_["t1"], in1=s_["t2"], op=OP.subtract))
        VS(lambda e: e.tensor_tensor(out=s_["ci"], in0=s_["t1"], in1=s_["den"], op=OP.mult))
        for gh in range(2):
            VS(lambda e, gh=gh: e.tensor_scalar(out=PH[:], in0=IOT[:], scalar1=sm["th"][:, gh:gh + 1], scalar2=None,
                                                op0=OP.mult))
            rred(PA2[:], PH[:], PK[:])
            sincos(TS[:, gh, :], TC[:, gh, :], PA2[:], PK[:])
            VS(lambda e, gh=gh: e.tensor_scalar(out=PH[:], in0=TS[:, gh, :], scalar1=sm["ci"][:, gh:gh + 1],
                                                scalar2=None, op0=OP.mult))
            VS(lambda e, gh=gh: e.scalar_tensor_tensor(out=TR[:, gh, :], in0=TC[:, gh, :],
                                                       scalar=sm["cr"][:, gh:gh + 1], in1=PH[:], op0=OP.mult,
                                                       op1=OP.add))
            VS(lambda e, gh=gh: e.tensor_scalar(out=PH[:], in0=TS[:, gh, :], scalar1=sm["cr"][:, gh:gh + 1],
                                                scalar2=None, op0=OP.mult))
            VS(lambda e, gh=gh: e.scalar_tensor_tensor(out=TI[:, gh, :], in0=TC[:, gh, :],
                                                       scalar=sm["ci"][:, gh:gh + 1], in1=PH[:], op0=OP.mult,
                                                       op1=OP.subtract))
            VS(lambda e, gh=gh: e.tensor_scalar(out=RB[:, gh, :], in0=IOT[:], scalar1=0.0,
                                                scalar2=sm["rho"][:, gh:gh + 1], op0=OP.mult, op1=OP.add))
            VS(lambda e, gh=gh: e.tensor_copy(out=sm["cL"][:, gh:gh + 1], in_=TC[:, gh, 511:512]))
            VS(lambda e, gh=gh: e.tensor_copy(out=sm["sL"][:, gh:gh + 1], in_=TS[:, gh, 511:512]))
        S5K = ["S5T"]
        V(lambda e: e.memset(f4[0:1, 0:2], 0.0), r=["S5T"], w=["f1", "f2", "f3", "f4"])

        def inproj_s1(s, b):
            gb = s * 16 + b
            p = gb % 2
            t0 = b * 512
            sl = slice(t0, t0 + 512)
            xk = ("XBF", p)
            if gb + 1 < 32:
                xload(gb + 1)
            for k in range(8):
                V(lambda e, k=k, p=p: e.tensor_tensor(out=XSQ[:, k, :], in0=XBF[p][:, k, :], in1=XBF[p][:, k, :],
                                                      op=OP.mult), r=[("XBF", p, k)], w=[("XSQ", k)])
            for k in range(8):
                T(lambda e, k=k, p=p: e.matmul(B[p][:], lhsT=ONB[:], rhs=XSQ[:, k, :], start=(k == 0), stop=(k == 7)),
                  r=[("XSQ", k), "ONB"], w=[("B", p)])
            A(lambda e, p=p: e.activation(out=RS[p][:], in_=B[p][:], func=AF.Ln, bias=EPS, scale=1.0 / D),
              r=[("B", p)], w=[("RSTD", p)])
            A(lambda e, p=p: e.activation(out=RSTD[p][:], in_=RS[p][:], func=AF.Exp, scale=-0.5), r=[("RSTD", p)],
              w=[("RSTD", p)])
            rk = ("RSTD", p)
            for qi, (Wt, wkey, M) in enumerate(((WQ, "WQ", 64), (WK, "WK", 65))):
                pb = 2 + qi
                for k in range(8):
                    T(lambda e, k=k, Wt=Wt, pb=pb, p=p, M=M: e.matmul(B[pb][0:M, :], lhsT=Wt[:, k, :],
                                                                       rhs=XBF[p][:, k, :], start=(k == 0),
                                                                       stop=(k == 7)),
                      r=[wkey, ("XBF", p, k)], w=[("B", pb)])
                V(lambda e, pb=pb, qi=qi, p=p: e.tensor_tensor(out=QF[qi][p][:], in0=B[pb][0:64, :],
                                                               in1=RSTD[p][0:64, :], op=OP.mult),
                  r=[("B", pb), rk], w=[("QF", qi, p)])
                if qi == 1:
                    V(lambda e, p=p: e.tensor_tensor(out=FF[p][64:65, :], in0=B[3][64:65, :],
                                                     in1=RSTD[p][64:65, :], op=OP.mult), r=[("B", 3), rk],
                      w=[("FF", p)])
            for k in range(8):
                T(lambda e, k=k, p=p: e.matmul(B[6][:], lhsT=WVU[:, k, :], rhs=XBF[p][:, k, :], start=(k == 0),
                                               stop=(k == 7)), r=["WVU", ("XBF", p, k)], w=[("B", 6)])
            V(lambda e, p=p: e.tensor_tensor(out=VB[p][:], in0=B[6][0:64, :], in1=RSTD[p][0:64, :], op=OP.mult),
              r=[("B", 6), rk], w=[("VB", p)])
            V(lambda e, sl=sl, p=p: e.tensor_tensor(out=UT[64:128, sl], in0=B[6][64:128, :], in1=RSTD[p][64:128, :],
                                                    op=OP.mult), r=[("B", 6), rk], w=[("UT", b)])

        def inproj_s2(s, b):
            gb = s * 16 + b
            p = gb % 2
            t0 = b * 512
            sl = slice(t0, t0 + 512)
            for qi, (dst, dkey, gcol) in enumerate(((QA, "QA", 0), (KA, "KA", 1))):
                A(lambda e, qi=qi, p=p: e.activation(out=SQ1[qi][:], in_=QF[qi][p][:], func=AF.Square),
                  r=[("QF", qi, p)], w=[("SQ1", qi)])
                T(lambda e, qi=qi: e.matmul(B[4 + qi][0:64, :], lhsT=ONB[0:64, 0:64], rhs=SQ1[qi][:], start=True,
                                            stop=True), r=[("SQ1", qi), "ONB"], w=[("B", 4 + qi)])
                A(lambda e, qi=qi: e.activation(out=RQ[qi][:], in_=B[4 + qi][0:64, :], func=AF.Ln, bias=EPS,
                                                scale=1.0 / 64), r=[("B", 4 + qi)], w=[("RQ2", qi)])
                A(lambda e, qi=qi: e.activation(out=RQ2[qi][:], in_=RQ[qi][:], func=AF.Exp, scale=-0.5),
                  r=[("RQ2", qi)], w=[("RQ2", qi)])
                V(lambda e, dst=dst, gcol=gcol, sl=sl, qi=qi, p=p: e.scalar_tensor_tensor(
                    out=dst[0:64, sl], in0=QF[qi][p][:], scalar=GQK[:, gcol:gcol + 1], in1=RQ2[qi][:], op0=OP.mult,
                    op1=OP.mult), r=[("QF", qi, p), ("RQ2", qi), "GQK"], w=[(dkey, b)])
            for tt in range(4):
                T(lambda e, tt=tt, p=p: e.transpose(PTB[:, tt, :], VB[p][:, tt * 128:(tt + 1) * 128],
                                                    IDB[0:64, 0:64]), r=[("VB", p), "IDB"], w=["PTB"])
            for tt in range(4):
                V(lambda e, tt=tt, b=b: e.tensor_copy(out=VTM[:, b * 4 + tt, 0:64], in_=PTB[:, tt, :]),
                  r=["PTB"], w=[("VTM", b * 4 + tt)])
            A(lambda e, p=p: e.activation(out=FE[64:65, :], in_=FF[p][64:65, :], func=AF.Exp, bias=NBF[64:65, 0:1],
                                          scale=-1.0), r=[("FF", p), "NBF"], w=["FE"])
            A(lambda e: e.activation(out=FE[64:65, :], in_=FE[64:65, :], func=AF.Ln, bias=1.0, scale=1.0),
              r=["FE"], w=["FE"])
            V(lambda e: e.tensor_scalar(out=FE[64:65, :], in0=FE[64:65, :], scalar1=-1.0, scalar2=None,
                                        op0=OP.mult), r=["FE"], w=["FE"])
            fp = b % 2
            if b == 0:
                V(lambda e, fp=fp: e.tensor_tensor_scan(out=FR[fp][64:65, :], data0=ONR[64:65, :],
                                                        data1=FE[64:65, :], initial=0.0, op0=OP.mult, op1=OP.add),
                  r=["FE", "ONR"], w=[("FR", fp)])
            else:
                V(lambda e, fp=fp: e.tensor_tensor_scan(out=FR[fp][64:65, :], data0=ONR[64:65, :],
                                                        data1=FE[64:65, :], initial=FR[1 - fp][64:65, 511:512],
                                                        op0=OP.mult, op1=OP.add),
                  r=["FE", "ONR", ("FR", 1 - fp)], w=[("FR", fp)])
            frk = ("FR", fp)
            V(lambda e, fp=fp: e.tensor_scalar(out=AR[64:65, :], in0=FR[fp][64:65, :], scalar1=FR[fp][64:65, 0:1],
                                               scalar2=None, op0=OP.subtract), r=[frk], w=["AR"])
            V(lambda e: e.tensor_copy(out=AH[64:65, :], in_=AR[64:65, :]), r=["AR"], w=["AH"])
            V(lambda e: e.tensor_tensor(out=AL[64:65, :], in0=AR[64:65, :], in1=AH[64:65, :], op=OP.subtract),
              r=["AR", "AH"], w=["AL"])
            VQ(lambda e, sl=sl: e.dma_start(out=QA[64:65, sl], in_=AH[64:65, :]), r=["AH"], w=[("QA", b)])
            VQ(lambda e, sl=sl: e.dma_start(out=QA[65:66, sl], in_=AL[64:65, :]), r=["AL"], w=[("QA", b)])
            for j in range(4):
                T(lambda e, j=j, fp=fp: e.matmul(B[5][:, 8 + j:9 + j], lhsT=FR[fp][64:65, j * 128:(j + 1) * 128],
                                                 rhs=ONF[64:65, 0:1], start=True, stop=True), r=[frk, "ONF"],
                  w=[("B", 5)])
            T(lambda e, fp=fp: e.matmul(B[5][:, 16:17], lhsT=ONF[64:65, :], rhs=FR[fp][64:65, 0:1], start=True,
                                        stop=True), r=[frk, "ONF"], w=[("B", 5)])
            V(lambda e, b=b: e.tensor_copy(out=FCOL[:, 4 * b:4 * b + 4], in_=B[5][:, 8:12]), r=[("B", 5)],
              w=["FCOL"])
            V(lambda e, b=b: e.tensor_copy(out=FQ0[:, b:b + 1], in_=B[5][:, 16:17]), r=[("B", 5)], w=["FQ0"])
            V(lambda e, b=b: e.tensor_scalar(out=CB[:, b, 0:4 * b + 4], in0=FCOL[:, 0:4 * b + 4], scalar1=-1.0,
                                             scalar2=FQ0[:, b:b + 1], op0=OP.mult, op1=OP.add),
              r=["FCOL", "FQ0"], w=[("CB", b)])

        def capture(fn, *args):
            saved = P.ops
            P.ops = []
            fn(*args)
            out = P.ops
            P.ops = saved
            return out

        def merge(la, lb):
            na, nb = len(la), len(lb)
            ia = ib = 0
            while ia < na or ib < nb:
                if ib >= nb or (ia < na and ia * nb <= ib * na):
                    P.ops.append(la[ia])
                    ia += 1
                else:
                    P.ops.append(lb[ib])
                    ib += 1
        def att_tiles():
            lst = []
            for qb in range(16):
                nk = 4 * qb + 4
                for j in range(nk):
                    lst.append((qb, j, nk))
            return lst

        def att_qk(s, i, qb, j, nk):
            sp = i % 2
            t0 = qb * 512
            dj = j - 4 * qb
            c0 = dj * 128 if dj > 0 else 0
            diag = dj >= 0
            T(lambda e: e.matmul(B[sp][:, c0:512], lhsT=KA[:, j * 128:(j + 1) * 128],
                                 rhs=QA[:, t0 + c0:t0 + 512], start=True, stop=(not diag)),
              r=[("KA", j // 4), ("QA", qb)], w=[("B", sp)])
            if diag:
                T(lambda e: e.matmul(B[sp][:, c0:c0 + 128], lhsT=IDB[:], rhs=MSK[:], start=False, stop=True),
                  r=["IDB", "MSK"], w=[("B", sp)])
            A(lambda e: e.activation(out=PT[sp][:, c0:512], in_=B[sp][:, c0:512], func=AF.Exp,
                                     bias=CB[:, qb, j:j + 1], scale=1.0), r=[("B", sp), ("CB", qb)], w=[("PT", sp)])

        def att_pv(s, i, qb, j, nk):
            sp = i % 2
            op_ = qb % 2
            t0 = qb * 512
            dj = j - 4 * qb
            c0 = dj * 128 if dj > 0 else 0
            T(lambda e: e.matmul(B[2 + op_][:, c0:512], lhsT=VTM[:, j, :], rhs=PT[sp][:, c0:512],
                                 start=(j == 0), stop=(j == nk - 1)), r=[("VTM", j), ("PT", sp)], w=[("B", 2 + op_)])
            if j == nk - 1:
                V(lambda e: e.tensor_copy(out=OS[op_][:], in_=B[2 + op_][0:65, :]), r=[("B", 2 + op_)],
                  w=[("OS", op_)])
                V(lambda e: e.reciprocal(out=OS[op_][64:65, :], in_=OS[op_][64:65, :]), r=[("OS", op_)],
                  w=[("OS", op_)])
                T(lambda e: e.matmul(B[4][0:64, :], lhsT=ONF[64:65, 0:64], rhs=OS[op_][64:65, :], start=True,
                                     stop=True), r=[("OS", op_), "ONF"], w=[("B", 4)])
                V(lambda e: e.tensor_tensor(out=YA[op_][:], in0=OS[op_][0:64, :], in1=B[4][0:64, :], op=OP.mult),
                  r=[("OS", op_), ("B", 4)], w=[("YA", op_)])
                if fused:
                    tok = s * S + t0
                    G(lambda e, tok=tok: e.dma_start(out=snd[tok // TB][0:64, tok % TB:tok % TB + 512],
                                                      in_=YA[op_][:]), r=[("YA", op_)], w=["snd"])
                else:
                    SY(lambda e: e.dma_start(out=yaT[:, s * S + t0:s * S + t0 + 512], in_=YA[op_][:]),
                       r=[("YA", op_)], w=["yaT"])

        def s5_front(s, b, gh):
            u = b * 2 + gh
            bp = u % 2
            WR_, WI_ = WRl[bp], WIl[bp]
            t0 = b * 512
            sl = slice(t0, t0 + 512)
            if b == 0 and gh == 0:
                V(lambda e: e.memset(sm["CR"][:], 0.0), w=["CRI"])
                V(lambda e: e.memset(sm["CI"][:], 0.0), w=["CRI"])
            T(lambda e: e.matmul(B[5][:], lhsT=BRP[64:128, gh, :], rhs=UT[64:128, sl], start=True, stop=True),
              r=["BRP", ("UT", b)], w=[("B", 5)])
            T(lambda e: e.matmul(B[6][:], lhsT=BIP[64:128, gh, :], rhs=UT[64:128, sl], start=True, stop=True),
              r=["BIP", ("UT", b)], w=[("B", 6)])
            V(lambda e: e.tensor_tensor(out=f1[:], in0=B[5][:], in1=TR[:, gh, :], op=OP.mult), r=[("B", 5)] + S5K,
              w=["f1"])
            V(lambda e: e.tensor_tensor(out=f2[:], in0=B[6][:], in1=TI[:, gh, :], op=OP.mult), r=[("B", 6)] + S5K,
              w=["f2"])
            V(lambda e: e.tensor_tensor(out=f3[:], in0=B[5][:], in1=TI[:, gh, :], op=OP.mult), r=[("B", 5)] + S5K,
              w=["f3"])
            V(lambda e: e.tensor_tensor(out=f4[:], in0=B[6][:], in1=TR[:, gh, :], op=OP.mult), r=[("B", 6)] + S5K,
              w=["f4"])
            V(lambda e: e.tensor_tensor(out=f1[:], in0=f1[:], in1=f2[:], op=OP.subtract), r=["f1", "f2"], w=["f1"])
            V(lambda e: e.tensor_tensor(out=f3[:], in0=f3[:], in1=f4[:], op=OP.add), r=["f3", "f4"], w=["f3"])
            V(lambda e: e.tensor_tensor_scan(out=WR_[:], data0=RB[:, gh, :], data1=f1[:],
                                             initial=sm["CR"][:, gh:gh + 1], op0=OP.mult, op1=OP.add),
              r=["f1", "CRI"] + S5K, w=[("WR", bp)])
            V(lambda e: e.tensor_tensor_scan(out=WI_[:], data0=RB[:, gh, :], data1=f3[:],
                                             initial=sm["CI"][:, gh:gh + 1], op0=OP.mult, op1=OP.add),
              r=["f3", "CRI"] + S5K, w=[("WI", bp)])
            cr_, ci_ = sm["CR"][:, gh:gh + 1], sm["CI"][:, gh:gh + 1]
            cl_, sl_ = sm["cL"][:, gh:gh + 1], sm["sL"][:, gh:gh + 1]
            ta_, tb_ = sm["ta"][:, gh:gh + 1], sm["tb"][:, gh:gh + 1]
            kk = dict(r=[("WR", bp), ("WI", bp), "CRI"] + S5K, w=["CRI"])
            V(lambda e: e.tensor_tensor(out=ta_, in0=WR_[:, 511:512], in1=cl_, op=OP.mult), **kk)
            V(lambda e: e.tensor_tensor(out=tb_, in0=WI_[:, 511:512], in1=sl_, op=OP.mult), **kk)
            V(lambda e: e.tensor_tensor(out=cr_, in0=ta_, in1=tb_, op=OP.subtract), **kk)
            V(lambda e: e.tensor_tensor(out=ta_, in0=WR_[:, 511:512], in1=sl_, op=OP.mult), **kk)
            V(lambda e: e.tensor_tensor(out=tb_, in0=WI_[:, 511:512], in1=cl_, op=OP.mult), **kk)
            V(lambda e: e.tensor_tensor(out=ci_, in0=ta_, in1=tb_, op=OP.add), **kk)
            bt = bb[bp]
            for i, (src, key, tab) in enumerate(((WR_, "WR", TC), (WI_, "WI", TS), (WR_, "WR", TS), (WI_, "WI", TC))):
                PL(lambda e, i=i, src=src, tab=tab: e.tensor_tensor(out=bt[i][:], in0=src[:], in1=tab[:, gh, :],
                                                                    op=OP.mult), r=[(key, bp)] + S5K,
                   w=[("bb", bp, i)])

        def s5_cproj(s, b, gh):
            u = b * 2 + gh
            bp = u % 2
            bt = bb[bp]
            for i, (Wc, wkey) in enumerate(((CRP, "CRP"), (CRN, "CRN"), (CIN, "CIN"), (CIN, "CIN"))):
                T(lambda e, i=i, Wc=Wc: e.matmul(B[4][:], lhsT=Wc[:, gh, :], rhs=bt[i][:],
                                                 start=(gh == 0 and i == 0), stop=(gh == 1 and i == 3)),
                  r=[wkey, ("bb", bp, i)], w=[("B", 4)])

        def s5_out(s, b):
            t0 = b * 512
            sl = slice(t0, t0 + 512)
            yp = b % 2
            V(lambda e: e.scalar_tensor_tensor(out=YS[yp][64:128, :], in0=UT[64:128, sl], scalar=DSK[64:128, 0:1],
                                               in1=B[4][64:128, :], op0=OP.mult, op1=OP.add),
              r=[("UT", b), "DSK", ("B", 4)], w=[("YS", yp)])
            if fused:
                tok = s * S + t0
                G(lambda e, tok=tok: e.dma_start(out=snd[tok // TB][64:128, tok % TB:tok % TB + 512],
                                                  in_=YS[yp][64:128, :]), r=[("YS", yp)], w=["snd"])
            else:
                SY(lambda e: e.dma_start(out=ysT[:, s * S + t0:s * S + t0 + 512], in_=YS[yp][64:128, :]),
                   r=[("YS", yp)], w=["ysT"])

        for s in range(2):
            P.ops.extend(capture(inproj_s1, s, 0))
            for b in range(16):
                la = capture(inproj_s2, s, b)
                lb = capture(inproj_s1, s, b + 1) if b + 1 < 16 else []
                merge(la, lb)
            tiles = att_tiles()
            units = [(b, gh) for b in range(16) for gh in range(2)]
            nt = len(tiles)
            nu = len(units)
            per = (nt + nu - 1) // nu
            ti = 0
            for k in range(nu + 3):
                if 0 <= k - 2 < nu and units[k - 2][1] == 1:
                    s5_out(s, units[k - 2][0])
                if k < nu:
                    s5_front(s, *units[k])
                hi = min(nt, (k + 1) * per) if k < nu - 1 else nt
                while ti < hi:
                    att_qk(s, ti, *tiles[ti])
                    if ti >= 1:
                        att_pv(s, ti - 1, *tiles[ti - 1])
                    ti += 1
                if ti == nt and k == nu - 1:
                    att_pv(s, nt - 1, *tiles[nt - 1])
                if 0 <= k - 1 < nu:
                    s5_cproj(s, *units[k - 1])
        P.emit(barrier=fused)
    return nc


def build_b(nc=None, rcv=None, sem_stack=None, shared=None, coll=None):
    fused = nc is not None
    if nc is None:
        nc = bass.Bass("TRN2", target_bir_lowering=False)
    dt0 = nc.dram_tensor
    pre = "b_" if fused else ""

    def dt(name, *a, **k):
        return dt0(pre + name, *a, **k)
    xT = dt("xT", [D, TB], F32, kind="ExternalInput").ap()
    if fused:
        lsel = dt("lsel", [128, 8 * 4 * 128], F32, kind="ExternalInput").ap()
    else:
        yaT = dt("yaT", [512, TB], BF16, kind="ExternalInput").ap()
        ysT = dt("ysT", [512, TB], BF16, kind="ExternalInput").ap()
    wgt = dt("wgt", [D, 2048], F32, kind="ExternalInput").ap()
    g1 = dt("g1", [128, 8], F32, kind="ExternalInput").ap()
    g2 = dt("g2", [128, 8], F32, kind="ExternalInput").ap()
    wglu = dt("wglu", [512, 512], F32, kind="ExternalInput").ap()
    wpa = dt("wpa", [512, D], F32, kind="ExternalInput").ap()
    wps = dt("wps", [512, D], F32, kind="ExternalInput").ap()
    wout = dt("wout", [D, D], F32, kind="ExternalInput").ap()
    wr = dt("wr", [D, 36], F32, kind="ExternalInput").ap()
    br = dt("br", [128, 36], F32, kind="ExternalInput").ap()
    weg = dt("weg", [32, D, 256], F32, kind="ExternalInput").ap()
    weu = dt("weu", [32, D, 256], F32, kind="ExternalInput").ap()
    wed = dt("wed", [32, 256, D], F32, kind="ExternalInput").ap()
    onesb = dt("onesb", [128, 128], F32, kind="ExternalInput").ap()
    identf = dt("identf", [128, 128], F32, kind="ExternalInput").ap()
    sel = dt("sel", [32, 32 * 128], F32, kind="ExternalInput").ap()
    outT = dt("outT", [D, TB], F32, kind="ExternalOutput").ap()

    P = Prog(nc, sem_stack=sem_stack, shared=shared)
    V, A, T, G, SY, PL, VQ = _helpers(P)
    with contextlib.ExitStack() as st:
        def sb(name, shape, dtype):
            return st.enter_context(nc.sbuf_tensor(pre + name, shape, dtype))

        def pp(name, shape, dtype=F32):
            return st.enter_context(nc.psum_tensor(pre + name, shape, dtype))
        X = sb("X", [128, 8, TB], F32)
        WG = sb("WG", [128, 8, 2048], BF16)
        H2 = WG
        YY = sb("YY", [128, 2, 4, 512], BF16)
        YAs = YY[:, 0]
        YSs = YY[:, 1]
        WGLU = sb("WGLU", [128, 4, 512], BF16)
        WPA = sb("WPA", [128, 4, D], BF16)
        WPS = sb("WPS", [128, 4, D], BF16)
        WOUT = sb("WOUT", [128, 8, D], BF16)
        WR = sb("WR", [128, 8, 36], F32)
        BR = sb("BR", [128, 36], F32)
        G1 = sb("G1", [128, 8], F32)
        G2 = sb("G2", [128, 8], F32)
        ONB = sb("ONB", [128, 128], BF16)
        IDF = sb("IDF", [128, 128], F32)
        XSQ = sb("XSQ", [128, 8, 512], BF16)
        XBF = sb("XBF", [128, 8, 512], BF16)
        MIX = XSQ
        SELt = WPS[0:32].rearrange("p a b -> p (a b)")
        WEG = [WOUT[:, :, 0:256], XBF[:, :, 0:256]]
        WEU = [WOUT[:, :, 256:512], XBF[:, :, 256:512]]
        WED = [WPA[:, 0:2, :], YY[:, 0].rearrange("p a b -> p (a b)").rearrange("p (f n) -> p f n", f=2)]
        WKEY = [["WOUT", "WOUT", "WPA"], ["XBF", "XBF", "YY"]]
        CT = sb("CT", [32, TB], BF16)
        H2F = sb("H2F", [128, 8, 128], F32)
        RS = sb("RS", [128, 512], F32)
        RSTD = sb("RSTD", [128, 512], F32)
        YG = sb("YG", [128, 4, 512], BF16)
        fa = [sb("fa%d" % i, [128, 512], F32) for i in range(2)]
        fb = [sb("fb%d" % i, [128, 512], F32) for i in range(2)]
        fc = [sb("fc%d" % i, [128, 512], BF16) for i in range(2)]
        HS = [sb("HS%d" % i, [128, 2, 512], BF16) for i in range(2)]
        LG = sb("LG", [128, 4, 36], F32)
        MK = sb("MK", [128, 4, 32], F32)
        CM = sb("CM", [128, 4, 32], F32)
        CM2 = sb("CM2", [128, 4, 32], F32)
        T8 = sb("T8", [128, 4, 8], F32)
        sc1 = {n: sb("sc_" + n, [128, 4], F32) for n in ["gmax", "ngmax", "gsum", "gtop", "d", "s1", "w1", "w2"]}
        sc4 = {n: sb("sc4_" + n, [128, 4, 4], F32) for n in ["mg", "nb", "eg"]}
        B = [pp("B%d" % i, [128, 512]) for i in range(8)]
        if fused:
            LSEL = sb("LSEL", [128, 8, 4, 128], BF16)
            GST = [sb("GST%d" % i, [128, 512], F32) for i in range(2)]
            GSB = [sb("GSB%d" % i, [128, 512], BF16) for i in range(2)]

        for k in range(8):
            SY(lambda e, k=k: e.dma_start(out=X[:, k, :], in_=xT[k * 128:(k + 1) * 128, :]), w=[("X", k)])
        if fused:
            G(lambda e: e.dma_start(out=LSEL[:].rearrange("p a b c -> p (a b c)"), in_=lsel), w=["LSEL"])
            snd_t, rcv_t = coll if coll is not None else ([], [])
            for j in range(len(snd_t)):
                P.add('cc', lambda e, j=j: e.collective_compute(
                    "AllGather", OP.bypass, replica_groups=[list(range(NCORES))], ins=[snd_t[j].ap().opt()],
                    outs=[rcv_t[j].ap().opt()]), r=(), w=[("rcv", j)])

        def gather_block(b):
            cnt_ = 0
            for q in range(4):
                for ii in range(2):
                    i = 2 * q + ii
                    for j in range(8):
                        si = cnt_ % 2
                        cnt_ += 1
                        c0 = b * 512
                        G(lambda e, i=i, j=j, c0=c0, si=si: e.dma_start(out=GST[si][:],
                                                                         in_=rcv[j][i, :, c0:c0 + 512]),
                           r=[("rcv", j)], w=[("GST", si)])
                        A(lambda e, si=si: e.activation(out=GSB[si][:], in_=GST[si][:], func=AF.Copy),
                          r=[("GST", si)], w=[("GSB", si)])
                        first = (ii == 0 and j == 0)
                        last = (ii == 1 and j == 7)
                        T(lambda e, j=j, ii=ii, si=si, first=first, last=last: e.matmul(
                            B[1][:], lhsT=LSEL[:, j, ii, :], rhs=GSB[si][:], start=first, stop=last),
                          r=["LSEL", ("GSB", si)], w=[("B", 1)])
                        T(lambda e, j=j, ii=ii, si=si, first=first, last=last: e.matmul(
                            B[2][:], lhsT=LSEL[:, j, 2 + ii, :], rhs=GSB[si][:], start=first, stop=last),
                          r=["LSEL", ("GSB", si)], w=[("B", 2)])
                V(lambda e, q=q: e.tensor_copy(out=YAs[:, q, :], in_=B[1][:]), r=[("B", 1)], w=[("YY", 0)])
                V(lambda e, q=q: e.tensor_copy(out=YSs[:, q, :], in_=B[2][:]), r=[("B", 2)], w=[("YY", 1)])

        def ldw(dst, src, key):
            G(lambda e: e.dma_start(out=dst, in_=src), w=[key])
        for (dst, src, key) in [(G1[:], g1, "G1"), (ONB[:], onesb, "ONB")]:
            ldw(dst, src, key)
        for k in range(8):
            ldw(WG[:, k, :], wgt[k * 128:(k + 1) * 128, :], ("WG", k))
        ldw(WGLU[:], wglu.rearrange("(k p) n -> p k n", p=128), "WGLU")
        ldw(WPA[:], wpa.rearrange("(k p) n -> p k n", p=128), "WPA")
        ldw(WPS[:], wps.rearrange("(k p) n -> p k n", p=128), "WPS")
        ldw(WOUT[:], wout.rearrange("(k p) n -> p k n", p=128), "WOUT")
        ldw(WR[:], wr.rearrange("(k p) n -> p k n", p=128), "WR")
        for (dst, src, key) in [(BR[:], br, "BR"), (G2[:], g2, "G2"), (IDF[:], identf, "IDF")]:
            ldw(dst, src, key)
        for k in range(8):
            V(lambda e, k=k: e.tensor_scalar(out=WG[:, k, :], in0=WG[:, k, :], scalar1=G1[:, k:k + 1], scalar2=None,
                                             op0=OP.mult), r=["G1", ("WG", k)], w=[("WG", k)])

        def rmsstat(sl, bank):
            for k in range(8):
                A(lambda e, k=k: e.activation(out=XSQ[:, k, :], in_=X[:, k, sl], func=AF.Square), r=[("X", k)],
                  w=[("XSQ", k)])
            for k in range(8):
                T(lambda e, k=k: e.matmul(B[bank][:], lhsT=ONB[:], rhs=XSQ[:, k, :], start=(k == 0), stop=(k == 7)),
                  r=[("XSQ", k), "ONB"], w=[("B", bank)])
            A(lambda e: e.activation(out=RS[:], in_=B[bank][:], func=AF.Sqrt, bias=EPS, scale=1.0 / D),
              r=[("B", bank)], w=["RS"])
            V(lambda e: e.reciprocal(out=RSTD[:], in_=RS[:]), r=["RS"], w=["RSTD"])

        for b in range(4):
            sl = slice(b * 512, (b + 1) * 512)
            if fused:
                gather_block(b)
                if DEBUG_GATHER and b == 1:
                    dbg = dt("dbg", [128, 8 * 512], BF16, kind="ExternalOutput").ap()
                    SY(lambda e: e.dma_start(out=dbg, in_=YY[:].rearrange("p a b c -> p (a b c)")),
                       r=[("YY", 0), ("YY", 1)], w=["dbg"])
            else:
                G(lambda e, sl=sl: e.dma_start(out=YAs, in_=yaT.rearrange("(k p) t -> p k t", p=128)[:, :, sl]),
                  w=[("YY", 0)])
                G(lambda e, sl=sl: e.dma_start(out=YSs, in_=ysT.rearrange("(k p) t -> p k t", p=128)[:, :, sl]),
                  w=[("YY", 1)])
            for k in range(8):
                PL(lambda e, k=k, sl=sl: e.tensor_copy(out=XBF[:, k, :], in_=X[:, k, sl]), r=[("X", k)],
                   w=[("XBF", k)])
            rmsstat(sl, 0)
            for m in range(4):
                q = m % 2
                V(lambda e, m=m, q=q: e.tensor_tensor(out=fa[q][:], in0=YSs[:, m, :], in1=YSs[:, m, :], op=OP.mult),
                  r=[("YY", 1)], w=[("fa", q)])
                V(lambda e, q=q: e.tensor_scalar(out=fa[q][:], in0=fa[q][:], scalar1=0.044715, scalar2=1.0,
                                                 op0=OP.mult, op1=OP.add), r=[("fa", q)], w=[("fa", q)])
                V(lambda e, m=m, q=q: e.tensor_tensor(out=fa[q][:], in0=fa[q][:], in1=YSs[:, m, :], op=OP.mult),
                  r=[("fa", q), ("YY", 1)], w=[("fa", q)])
                A(lambda e, q=q: e.activation(out=fb[q][:], in_=fa[q][:], func=AF.Sigmoid,
                                              scale=1.5957691216057308), r=[("fa", q)], w=[("fb", q)])
                V(lambda e, m=m, q=q: e.tensor_tensor(out=YSs[:, m, :], in0=YSs[:, m, :], in1=fb[q][:], op=OP.mult),
                  r=[("fb", q), ("YY", 1)], w=[("YY", 1)])
            for m in range(4):
                bk = 1 + (m % 2)
                for k in range(4):
                    T(lambda e, m=m, k=k, bk=bk: e.matmul(B[bk][:], lhsT=WGLU[:, k, m * 128:(m + 1) * 128],
                                                          rhs=YSs[:, k, :], start=(k == 0), stop=(k == 3)),
                      r=["WGLU", ("YY", 1)], w=[("B", bk)])
                A(lambda e, bk=bk, m=m: e.activation(out=fa[m % 2][:], in_=B[bk][:], func=AF.Sigmoid),
                  r=[("B", bk)], w=[("fa", m % 2)])
                V(lambda e, m=m: e.tensor_tensor(out=YG[:, m, :], in0=YSs[:, m, :], in1=fa[m % 2][:], op=OP.mult),
                  r=[("YY", 1), ("fa", m % 2)], w=[("YG", m)])
            for n in range(8):
                q = n % 2
                b0, b1, b2, b3 = (0 + 4 * q, 1 + 4 * q, 2 + 4 * q, 3 + 4 * q)
                for k in range(8):
                    T(lambda e, n=n, k=k, b0=b0: e.matmul(B[b0][:], lhsT=WG[:, k, n * 128:(n + 1) * 128],
                                                          rhs=XBF[:, k, :], start=(k == 0), stop=(k == 7)),
                      r=[("WG", k), ("XBF", k)], w=[("B", b0)])
                for k in range(8):
                    T(lambda e, n=n, k=k, b1=b1: e.matmul(B[b1][:],
                                                          lhsT=WG[:, k, 1024 + n * 128:1024 + (n + 1) * 128],
                                                          rhs=XBF[:, k, :], start=(k == 0), stop=(k == 7)),
                      r=[("WG", k), ("XBF", k)], w=[("B", b1)])
                for k in range(4):
                    T(lambda e, n=n, k=k, b2=b2: e.matmul(B[b2][:], lhsT=WPA[:, k, n * 128:(n + 1) * 128],
                                                          rhs=YAs[:, k, :], start=(k == 0), stop=(k == 3)),
                      r=["WPA", ("YY", 0)], w=[("B", b2)])
                for k in range(4):
                    T(lambda e, n=n, k=k, b3=b3: e.matmul(B[b3][:], lhsT=WPS[:, k, n * 128:(n + 1) * 128],
                                                          rhs=YG[:, k, :], start=(k == 0), stop=(k == 3)),
                      r=["WPS", ("YG", k)], w=[("B", b3)])
                V(lambda e, q=q, b0=b0: e.tensor_tensor(out=fa[q][:], in0=B[b0][:], in1=RSTD[:], op=OP.mult),
                  r=[("B", b0), "RSTD"], w=[("fa", q)])
                A(lambda e, q=q: e.activation(out=fa[q][:], in_=fa[q][:], func=AF.Sigmoid), r=[("fa", q)],
                  w=[("fa", q)])
                V(lambda e, q=q, b1=b1: e.tensor_tensor(out=fb[q][:], in0=B[b1][:], in1=RSTD[:], op=OP.mult),
                  r=[("B", b1), "RSTD"], w=[("fb", q)])
                A(lambda e, q=q: e.activation(out=fb[q][:], in_=fb[q][:], func=AF.Sigmoid), r=[("fb", q)],
                  w=[("fb", q)])
                V(lambda e, q=q, b2=b2: e.tensor_tensor(out=fa[q][:], in0=B[b2][:], in1=fa[q][:], op=OP.mult),
                  r=[("B", b2), ("fa", q)], w=[("fa", q)])
                V(lambda e, q=q, b3=b3: e.tensor_tensor(out=fb[q][:], in0=B[b3][:], in1=fb[q][:], op=OP.mult),
                  r=[("B", b3), ("fb", q)], w=[("fb", q)])
                PL(lambda e, n=n, q=q: e.tensor_tensor(out=MIX[:, n, :], in0=fa[q][:], in1=fb[q][:], op=OP.add),
                   r=[("fa", q), ("fb", q)], w=[("XSQ", n)])
            for n in range(8):
                bk = n % 4
                for k in range(8):
                    T(lambda e, n=n, k=k, bk=bk: e.matmul(B[bk][:], lhsT=WOUT[:, k, n * 128:(n + 1) * 128],
                                                          rhs=MIX[:, k, :], start=(k == 0), stop=(k == 7)),
                      r=["WOUT", ("XSQ", k)], w=[("B", bk)])
                V(lambda e, n=n, sl=sl, bk=bk: e.tensor_tensor(out=X[:, n, sl], in0=X[:, n, sl], in1=B[bk][:],
                                                               op=OP.add), r=[("X", n), ("B", bk)], w=[("X", n)])

        def load_expert(ex):
            p = ex % 2
            G(lambda e: e.dma_start(out=WEG[p], in_=weg[ex].rearrange("(k p) n -> p k n", p=128)), w=[WKEY[p][0]])
            G(lambda e: e.dma_start(out=WEU[p], in_=weu[ex].rearrange("(k p) n -> p k n", p=128)), w=[WKEY[p][1]])
            G(lambda e: e.dma_start(out=WED[p], in_=wed[ex].rearrange("(k p) n -> p k n", p=128)), w=[WKEY[p][2]])
        ldw(SELt, sel, "WPS")
        load_expert(0)
        load_expert(1)

        for b in range(4):
            sl = slice(b * 512, (b + 1) * 512)
            rmsstat(sl, 0)
            for tt in range(4):
                ts_ = slice(tt * 128, (tt + 1) * 128)
                gs_ = slice(b * 512 + tt * 128, b * 512 + (tt + 1) * 128)
                for n in range(8):
                    V(lambda e, n=n, gs_=gs_, ts_=ts_: e.scalar_tensor_tensor(
                        out=H2F[:, n, :], in0=X[:, n, gs_], scalar=G2[:, n:n + 1], in1=RSTD[:, ts_], op0=OP.mult,
                        op1=OP.mult), r=[("X", n), "G2", "RSTD"], w=[("H2F", n)])
                    PL(lambda e, n=n, gs_=gs_: e.tensor_copy(out=H2[:, n, gs_], in_=H2F[:, n, :]), r=[("H2F", n)],
                       w=[("WG", n)])
                for k in range(8):
                    T(lambda e, k=k, tt=tt: e.matmul(B[1 + tt][:, 0:36], lhsT=H2F[:, k, :], rhs=WR[:, k, :],
                                                     start=(k == 0), stop=(k == 7)), r=[("H2F", k), "WR"],
                      w=[("B", 1 + tt)])
            steps = []

            def chain(tt):
                RT = dict(r=[("RT", tt)], w=[("RT", tt)])
                LGt, MKt, CMt, CM2t, T8t = LG[:, tt, :], MK[:, tt, :], CM[:, tt, :], CM2[:, tt, :], T8[:, tt, :]
                g = {n: t[:, tt:tt + 1] for n, t in sc1.items()}
                eg, mg, nb_ = sc4["eg"][:, tt, :], sc4["mg"][:, tt, :], sc4["nb"][:, tt, :]
                ops = []
                ops.append(lambda: V(lambda e: e.tensor_tensor(out=LGt, in0=B[1 + tt][:, 0:36], in1=BR[:], op=OP.add),
                                     r=[("B", 1 + tt), "BR", ("RT", tt)], w=[("RT", tt)]))
                ops.append(lambda: V(lambda e: e.tensor_reduce(out=g["gmax"], in_=LGt[:, 0:4], axis=AX.X, op=OP.max),
                                     **RT))
                ops.append(lambda: V(lambda e: e.tensor_scalar(out=g["ngmax"], in0=g["gmax"], scalar1=-1.0,
                                                               scalar2=None, op0=OP.mult), **RT))
                ops.append(lambda: A(lambda e: e.activation(out=eg, in_=LGt[:, 0:4], func=AF.Exp, bias=g["ngmax"],
                                                            scale=1.0), **RT))
                ops.append(lambda: V(lambda e: e.tensor_reduce(out=g["gsum"], in_=eg, axis=AX.X, op=OP.add), **RT))
                ops.append(lambda: V(lambda e: e.reciprocal(out=g["gtop"], in_=g["gsum"]), **RT))
                ops.append(lambda: V(lambda e: e.tensor_scalar(out=mg, in0=LGt[:, 0:4], scalar1=g["gmax"],
                                                               scalar2=None, op0=OP.is_equal), **RT))
                ops.append(lambda: V(lambda e: e.tensor_scalar(out=nb_, in0=mg, scalar1=-1.0, scalar2=1e30,
                                                               op0=OP.add, op1=OP.mult), **RT))
                for gi in range(4):
                    ops.append(lambda gi=gi: V(lambda e: e.tensor_scalar(
                        out=MKt[:, gi * 8:(gi + 1) * 8], in0=LGt[:, 4 + gi * 8:4 + (gi + 1) * 8],
                        scalar1=mg[:, gi:gi + 1], scalar2=nb_[:, gi:gi + 1], op0=OP.mult, op1=OP.add), **RT))
                ops.append(lambda: V(lambda e: e.max(out=T8t, in_=MKt), **RT))
                ops.append(lambda: V(lambda e: e.tensor_tensor(out=g["d"], in0=T8t[:, 1:2], in1=T8t[:, 0:1],
                                                               op=OP.subtract), **RT))
                ops.append(lambda: A(lambda e: e.activation(out=g["s1"], in_=g["d"], func=AF.Exp), **RT))
                ops.append(lambda: V(lambda e: e.tensor_scalar(out=g["s1"], in0=g["s1"], scalar1=1.0, scalar2=None,
                                                               op0=OP.add), **RT))
                ops.append(lambda: V(lambda e: e.reciprocal(out=g["s1"], in_=g["s1"]), **RT))
                ops.append(lambda: V(lambda e: e.tensor_tensor(out=g["w1"], in0=g["s1"], in1=g["gtop"], op=OP.mult),
                                     **RT))
                ops.append(lambda: V(lambda e: e.tensor_tensor(out=g["w2"], in0=g["gtop"], in1=g["w1"],
                                                               op=OP.subtract), **RT))
                ops.append(lambda: V(lambda e: e.tensor_scalar(out=CMt, in0=MKt, scalar1=T8t[:, 0:1],
                                                               scalar2=g["w1"], op0=OP.is_equal, op1=OP.mult), **RT))
                ops.append(lambda: V(lambda e: e.tensor_scalar(out=CM2t, in0=MKt, scalar1=T8t[:, 1:2],
                                                               scalar2=g["w2"], op0=OP.is_equal, op1=OP.mult), **RT))
                ops.append(lambda: V(lambda e: e.tensor_tensor(out=CMt, in0=CMt, in1=CM2t, op=OP.add), **RT))
                ops.append(lambda: T(lambda e: e.transpose(B[5][0:32, tt * 128:(tt + 1) * 128], CMt, IDF[:]),
                                     r=[("RT", tt), "IDF"], w=[("B", 5)]))
                return ops
            chains = [chain(tt) for tt in range(4)]
            for i in range(len(chains[0])):
                for tt in range(4):
                    chains[tt][i]()
            V(lambda e, b=b: e.tensor_copy(out=CT[:, b * 512:(b + 1) * 512], in_=B[5][0:32, :]), r=[("B", 5)],
              w=[("CT", b)])

        units = [(ex, b) for ex in range(32) for b in range(4)]

        def up(ui):
            ex, b = units[ui]
            p = ex % 2
            hp = ui % 2
            sl = slice(b * 512, (b + 1) * 512)
            T(lambda e: e.matmul(B[0][:], lhsT=SELt[:, ex * 128:(ex + 1) * 128], rhs=CT[:, sl], start=True,
                                 stop=True), r=["WPS", ("CT", b)], w=[("B", 0)])
            A(lambda e: e.activation(out=fc[hp][:], in_=B[0][:], func=AF.Copy), r=[("B", 0)], w=[("fc", hp)])
            for f in range(2):
                bg, bu = 1 + 2 * f, 2 + 2 * f
                for k in range(8):
                    T(lambda e, f=f, k=k, bg=bg: e.matmul(B[bg][:], lhsT=WEG[p][:, k, f * 128:(f + 1) * 128],
                                                          rhs=H2[:, k, sl], start=(k == 0), stop=(k == 7)),
                      r=[WKEY[p][0], ("WG", k)], w=[("B", bg)])
                for k in range(8):
                    T(lambda e, f=f, k=k, bu=bu: e.matmul(B[bu][:], lhsT=WEU[p][:, k, f * 128:(f + 1) * 128],
                                                          rhs=H2[:, k, sl], start=(k == 0), stop=(k == 7)),
                      r=[WKEY[p][1], ("WG", k)], w=[("B", bu)])
                A(lambda e, f=f, bg=bg: e.activation(out=fa[f][:], in_=B[bg][:], func=AF.Silu), r=[("B", bg)],
                  w=[("fa", f)])
                V(lambda e, f=f, bu=bu: e.tensor_tensor(out=fb[f][:], in0=B[bu][:], in1=fc[hp][:], op=OP.mult),
                  r=[("B", bu), ("fc", hp)], w=[("fb", f)])
                PL(lambda e, f=f: e.tensor_tensor(out=HS[hp][:, f, :], in0=fa[f][:], in1=fb[f][:], op=OP.mult),
                   r=[("fa", f), ("fb", f)], w=[("HS", hp, f)])

        def down(ui):
            ex, b = units[ui]
            p = ex % 2
            hp = ui % 2
            sl = slice(b * 512, (b + 1) * 512)
            for n in range(8):
                bk = 5 + (n % 3)
                for f in range(2):
                    T(lambda e, n=n, f=f, bk=bk: e.matmul(B[bk][:], lhsT=WED[p][:, f, n * 128:(n + 1) * 128],
                                                          rhs=HS[hp][:, f, :], start=(f == 0), stop=(f == 1)),
                      r=[WKEY[p][2], ("HS", hp, f)], w=[("B", bk)])
                V(lambda e, n=n, bk=bk: e.tensor_tensor(out=X[:, n, sl], in0=X[:, n, sl], in1=B[bk][:], op=OP.add),
                  r=[("X", n), ("B", bk)], w=[("X", n)])
            if b == 3 and ex + 2 < 32:
                load_expert(ex + 2)
        for ui in range(len(units)):
            up(ui)
            if ui >= 1:
                down(ui - 1)
        down(len(units) - 1)
        for k in range(8):
            SY(lambda e, k=k: e.dma_start(out=outT[k * 128:(k + 1) * 128, :], in_=X[:, k, :]), r=[("X", k)],
               w=["outT"])
        P.emit()
    return nc


def _consts():
    onesb = np.ones((128, 128), np.float32)
    ident = np.eye(128, dtype=np.float32)
    iota1 = np.tile(np.arange(1, 513, dtype=np.float32)[None, :], (128, 1))
    p = np.arange(128)
    maskd = np.where(p[:, None] > p[None, :], -30000.0, 0.0).astype(np.float32)
    sel = np.zeros((32, 32, 128), np.float32)
    for e in range(32):
        sel[e, e, :] = 1.0
    return onesb, ident, iota1, maskd, sel.reshape(32, 32 * 128)


def stage_a_inputs(x, norm_mix_g, w_in, b_forget, q_norm_g, k_norm_g, ssm_A_re, ssm_A_im, ssm_log_dt, ssm_B_re,
                   ssm_B_im, ssm_C_re, ssm_C_im, ssm_D):
    onesb, ident, iota1, maskd, _ = _consts()
    xT = np.ascontiguousarray(x.reshape(NTOK, D).T)
    w = w_in[0]
    g1 = np.ascontiguousarray(norm_mix_g[0].reshape(8, 128).T)
    maps = []
    for c in range(NCORES):
        wq = np.ascontiguousarray(w[:, c * 64:(c + 1) * 64])
        wk = np.ascontiguousarray(np.concatenate([w[:, 512 + c * 64:512 + (c + 1) * 64],
                                                  w[:, 1536 + c:1537 + c]], axis=1))
        wv = w[:, 1024 + c * 64:1024 + (c + 1) * 64]
        wu = w[:, 1544 + c * 64:1544 + (c + 1) * 64]
        wvu = np.ascontiguousarray(np.concatenate([wv, wu], axis=1))
        nbf = np.full((65, 2), b_forget[0, c], np.float32)
        gqk = np.ascontiguousarray(np.stack([q_norm_g[0], k_norm_g[0]], axis=1)).astype(np.float32)
        gs = np.arange(4 * c, 4 * c + 4)
        are = np.zeros((128, 2), np.float32)
        aim = np.zeros((128, 2), np.float32)
        ldt = np.zeros((128, 2), np.float32)
        brp = np.zeros((64, 2, 128), np.float32)
        bip = np.zeros((64, 2, 128), np.float32)
        crp = np.zeros((128, 2, 128), np.float32)
        cip = np.zeros((128, 2, 128), np.float32)
        for gh in range(2):
            for gl in range(2):
                gloc = 2 * gh + gl
                g = gs[gloc]
                are[gl * 64:(gl + 1) * 64, gh] = ssm_A_re[0, g]
                aim[gl * 64:(gl + 1) * 64, gh] = ssm_A_im[0, g]
                ldt[gl * 64:(gl + 1) * 64, gh] = ssm_log_dt[0, g]
                brp[gloc * 16:(gloc + 1) * 16, gh, gl * 64:(gl + 1) * 64] = ssm_B_re[0, g].T
                bip[gloc * 16:(gloc + 1) * 16, gh, gl * 64:(gl + 1) * 64] = ssm_B_im[0, g].T
                crp[gl * 64:(gl + 1) * 64, gh, 64 + gloc * 16:64 + (gloc + 1) * 16] = ssm_C_re[0, g].T
                cip[gl * 64:(gl + 1) * 64, gh, 64 + gloc * 16:64 + (gloc + 1) * 16] = ssm_C_im[0, g].T
        dsk = np.ascontiguousarray(np.repeat(ssm_D[0, c * 64:(c + 1) * 64][:, None], 2, axis=1)).astype(np.float32)
        maps.append(dict(xT=xT, wq=wq, wk=wk, wvu=wvu, g1=g1, nbf=nbf, gqk=gqk, are=are, aim=aim, ldt=ldt,
                         brp=brp, bip=bip, crp=crp, cip=cip, dsk=dsk, onesb=onesb, identb=ident, iota1=iota1,
                         maskd=maskd))
    return maps


def stage_b_inputs(x, ya_full, ys_full, norm_mix_g, w_in, w_glu, w_proj_attn, w_proj_ssm, w_out, norm_ffn_g,
                   w_router_group, b_router_group, w_router_expert, b_router_expert, w_expert_gate, w_expert_up,
                   w_expert_down):
    onesb, ident, iota1, maskd, sel = _consts()
    xf = x.reshape(NTOK, D)
    g1 = np.ascontiguousarray(norm_mix_g[0].reshape(8, 128).T)
    g2 = np.ascontiguousarray(norm_ffn_g[0].reshape(8, 128).T)
    wgt = np.ascontiguousarray(w_in[0][:, 2056:4104])
    wr = np.ascontiguousarray(np.concatenate([w_router_group[0], w_router_expert[0]], axis=1))
    br = np.ascontiguousarray(np.tile(np.concatenate([b_router_group[0], b_router_expert[0]])[None, :], (128, 1)))
    maps = []
    for c in range(NCORES):
        tsl = slice(c * TB, (c + 1) * TB)
        maps.append(dict(xT=np.ascontiguousarray(xf[tsl].T),
                         yaT=None if ya_full is None else np.ascontiguousarray(ya_full[:, tsl]),
                         ysT=None if ys_full is None else np.ascontiguousarray(ys_full[:, tsl]), wgt=wgt, g1=g1, g2=g2, wglu=w_glu[0],
                         wpa=w_proj_attn[0], wps=w_proj_ssm[0], wout=w_out[0], wr=wr, br=br, weg=w_expert_gate[0],
                         weu=w_expert_up[0], wed=w_expert_down[0], onesb=onesb, identf=ident, sel=sel))
    return maps


def run_a(inputs):
    nc = build_a()
    keys = ["x", "norm_mix_g", "w_in", "b_forget", "q_norm_g", "k_norm_g", "ssm_A_re", "ssm_A_im", "ssm_log_dt",
            "ssm_B_re", "ssm_B_im", "ssm_C_re", "ssm_C_im", "ssm_D"]
    maps = stage_a_inputs(*[np.asarray(inputs[k], np.float32) for k in keys])
    res = run_bass_kernel_spmd(nc, maps, core_ids=list(range(NCORES)))
    ya = np.concatenate([np.asarray(res.results[c]["yaT"]) for c in range(NCORES)], axis=0)
    ys = np.concatenate([np.asarray(res.results[c]["ysT"]) for c in range(NCORES)], axis=0)
    return ya, ys


def run_b(inputs, ya, ys):
    nc = build_b()
    keys = ["norm_mix_g", "w_in", "w_glu", "w_proj_attn", "w_proj_ssm", "w_out", "norm_ffn_g", "w_router_group",
            "b_router_group", "w_router_expert", "b_router_expert", "w_expert_gate", "w_expert_up", "w_expert_down"]
    maps = stage_b_inputs(np.asarray(inputs["x"], np.float32), ya, ys,
                          *[np.asarray(inputs[k], np.float32) for k in keys])
    res = run_bass_kernel_spmd(nc, maps, core_ids=list(range(NCORES)))
    out = np.concatenate([np.asarray(res.results[c]["outT"]).T for c in range(NCORES)], axis=0)
    return out.reshape(2, S, D).astype(np.float32)


def build_fused():
    nc = bass.Bass("TRN2", target_bir_lowering=False)
    snd_t = [nc.dram_tensor("snd%d" % j, [1024, 256], F32) for j in range(8)]
    rcv_t = [nc.dram_tensor("rcv%d" % j, [8192, 256], F32) for j in range(8)]
    snd = [t.ap().rearrange("(p a) c -> p (a c)", a=8) for t in snd_t]
    rcv = [t.ap().rearrange("(i p a) c -> i p (a c)", i=8, a=8) for t in rcv_t]
    shared = {"sems": None, "base": {}}
    with contextlib.ExitStack() as sem_stack:
        build_a(nc, snd, sem_stack, shared)
        build_b(nc, rcv, sem_stack, shared, coll=(snd_t, rcv_t))
    return nc


def kernel_fused(**inputs):
    nc = build_fused()
    keys_a = ["x", "norm_mix_g", "w_in", "b_forget", "q_norm_g", "k_norm_g", "ssm_A_re", "ssm_A_im", "ssm_log_dt",
              "ssm_B_re", "ssm_B_im", "ssm_C_re", "ssm_C_im", "ssm_D"]
    maps_a = stage_a_inputs(*[np.asarray(inputs[k], np.float32) for k in keys_a])
    keys_b = ["norm_mix_g", "w_in", "w_glu", "w_proj_attn", "w_proj_ssm", "w_out", "norm_ffn_g", "w_router_group",
              "b_router_group", "w_router_expert", "b_router_expert", "w_expert_gate", "w_expert_up", "w_expert_down"]
    maps_b = stage_b_inputs(np.asarray(inputs["x"], np.float32), None, None,
                            *[np.asarray(inputs[k], np.float32) for k in keys_b])
    maps = []
    for c in range(NCORES):
        m = dict(maps_a[c])
        lsel = np.zeros((128, 8, 4, 128), np.float32)
        for k in range(64):
            lsel[k, c, 0, k] = 1.0
            lsel[k, c, 1, 64 + k] = 1.0
            lsel[64 + k, c, 2, k] = 1.0
            lsel[64 + k, c, 3, 64 + k] = 1.0
        m["b_lsel"] = lsel.reshape(128, 8 * 4 * 128)
        for k, v in maps_b[c].items():
            if k in ("yaT", "ysT"):
                continue
            m["b_" + k] = v
        maps.append(m)
    res = run_bass_kernel_spmd(nc, maps, core_ids=list(range(NCORES)))
    out = np.concatenate([np.asarray(res.results[c]["b_outT"]).T for c in range(NCORES)], axis=0)
    return out.reshape(2, S, D).astype(np.float32)


def kernel(**inputs):
    ya, ys = run_a(inputs)
    return run_b(inputs, ya, ys)
```

```python
import contextlib
import numpy as np
import ml_dtypes
import concourse.bass as bass
import concourse.mybir as mybir
from concourse.bass_utils import run_bass_kernel_spmd

F32 = mybir.dt.float32
BF16 = mybir.dt.bfloat16
AF = mybir.ActivationFunctionType
OP = mybir.AluOpType
AX = mybir.AxisListType

NCORES = 8
D = 1024
S = 8192
NTOK = 16384
TB = 2048
EPS = 1e-6
CH = 3000
NDS = 3
MAGIC = 12582912.0
INV2PI = 0.15915494309189535
C1 = 6.28125
C2 = 0.0019353071795864769
PI = 3.141592653589793

STREAMS = {'pe': ('tensor', False), 'dve': ('vector', False), 'act': ('scalar', False),
           'pool': ('gpsimd', False), 'gq': ('gpsimd', True), 'sq': ('sync', True), 'vq': ('scalar', True)}


def _norm(k):
    if isinstance(k, tuple):
        return k[0], k[1:]
    return k, None


class Prog:
    def __init__(self, nc):
        self.nc = nc
        self.ops = []

    def add(self, st, fn, r=(), w=()):
        self.ops.append((st, fn, tuple(r), tuple(w)))

    def emit(self):
        nc = self.nc
        ops = self.ops
        cnt = {}
        idx = []
        for (st, fn, r, w) in ops:
            idx.append(cnt.get(st, 0))
            cnt[st] = cnt.get(st, 0) + 1
        writers = {}
        readers = {}
        deps = []

        def conf(a, b):
            return a is None or b is None or a == b
        for i, (st, fn, r, w) in enumerate(ops):
            d = set()
            for k in r:
                name, sub = _norm(k)
                for (s2, o) in writers.get(name, ()):
                    if conf(sub, s2):
                        d.add(o)
            for k in w:
                name, sub = _norm(k)
                for (s2, o) in writers.get(name, ()):
                    if conf(sub, s2):
                        d.add(o)
                for (s2, o) in readers.get(name, ()):
                    if conf(sub, s2):
                        d.add(o)
            for k in w:
                name, sub = _norm(k)
                writers[name] = [(s2, o) for (s2, o) in writers.get(name, []) if not (sub is None or s2 == sub)]
                writers[name].append((sub, i))
                readers[name] = [(s2, o) for (s2, o) in readers.get(name, []) if not conf(sub, s2)]
            for k in r:
                name, sub = _norm(k)
                readers.setdefault(name, []).append((sub, i))
            d.discard(i)
            need = {}
            needd = set()
            for o in d:
                s2 = ops[o][0]
                if STREAMS[s2][1]:
                    needd.add((s2, idx[o]))
                elif need.get(s2, -1) < idx[o]:
                    need[s2] = idx[o]
            deps.append((need, needd))

        with contextlib.ExitStack() as st_:
            sems = {}
            for s in cnt:
                if STREAMS[s][1]:
                    sems[s] = [st_.enter_context(nc.semaphore("d_%s_%d" % (s, i))) for i in range(NDS)]
                else:
                    sems[s] = [st_.enter_context(nc.semaphore("s_%s_%d" % (s, i)))
                               for i in range(cnt[s] // CH + 1)]
            block = st_.enter_context(nc.Block())

            def section(ename, eng):
                waited = {}
                waitedd = {}

                def wait_c(s2, j):
                    if waited.get(s2, -1) >= j:
                        return
                    eng.wait_ge(sems[s2][j // CH], (j % CH) + 1)
                    waited[s2] = j

                def wait_d(s2, j):
                    key = (s2, j % NDS)
                    if waitedd.get(key, -1) >= j:
                        return
                    eng.wait_ge(sems[s2][j % NDS], (j // NDS + 1) * 16)
                    waitedd[key] = j
                for i, (st, fn, r, w) in enumerate(ops):
                    if STREAMS[st][0] != ename:
                        continue
                    need, needd = deps[i]
                    for s2, j in need.items():
                        if st == 'pe' and s2 == 'pe':
                            continue
                        wait_c(s2, j)
                    for (s2, j) in sorted(needd):
                        wait_d(s2, j)
                    j = idx[i]
                    if STREAMS[st][1]:
                        if j >= NDS:
                            wait_d(st, j - NDS)
                        inst = fn(eng)
                        inst.then_inc(sems[st][j % NDS], 16)
                    else:
                        inst = fn(eng)
                        inst.then_inc(sems[st][j // CH], 1)
                if ename == 'sync':
                    for s2 in cnt:
                        if STREAMS[s2][1]:
                            for j in range(max(0, cnt[s2] - NDS), cnt[s2]):
                                wait_d(s2, j)
                        else:
                            wait_c(s2, cnt[s2] - 1)

            @block.tensor
            def _(eng):
                section('tensor', eng)

            @block.vector
            def _(eng):
                section('vector', eng)

            @block.scalar
            def _(eng):
                section('scalar', eng)

            @block.gpsimd
            def _(eng):
                section('gpsimd', eng)

            @block.sync
            def _(eng):
                section('sync', eng)


def _helpers(P):
    def mk(st):
        def f(fn, r=(), w=()):
            P.add(st, fn, r, w)
        return f
    return mk('dve'), mk('act'), mk('pe'), mk('gq'), mk('sq'), mk('pool'), mk('vq')


def build_a():
    nc = bass.Bass("TRN2", target_bir_lowering=False)
    dt = nc.dram_tensor
    xT = dt("xT", [D, NTOK], F32, kind="ExternalInput").ap()
    wq = dt("wq", [D, 64], F32, kind="ExternalInput").ap()
    wk = dt("wk", [D, 65], F32, kind="ExternalInput").ap()
    wvu = dt("wvu", [D, 128], F32, kind="ExternalInput").ap()
    g1 = dt("g1", [128, 8], F32, kind="ExternalInput").ap()
    nbf = dt("nbf", [65, 2], F32, kind="ExternalInput").ap()
    gqk = dt("gqk", [64, 2], F32, kind="ExternalInput").ap()
    are = dt("are", [128, 2], F32, kind="ExternalInput").ap()
    aim = dt("aim", [128, 2], F32, kind="ExternalInput").ap()
    ldt = dt("ldt", [128, 2], F32, kind="ExternalInput").ap()
    brp = dt("brp", [64, 2, 128], F32, kind="ExternalInput").ap()
    bip = dt("bip", [64, 2, 128], F32, kind="ExternalInput").ap()
    crp = dt("crp", [128, 2, 128], F32, kind="ExternalInput").ap()
    cip = dt("cip", [128, 2, 128], F32, kind="ExternalInput").ap()
    dsk = dt("dsk", [64, 2], F32, kind="ExternalInput").ap()
    onesb = dt("onesb", [128, 128], F32, kind="ExternalInput").ap()
    identb = dt("identb", [128, 128], F32, kind="ExternalInput").ap()
    iota1 = dt("iota1", [128, 512], F32, kind="ExternalInput").ap()
    maskd = dt("maskd", [128, 128], F32, kind="ExternalInput").ap()
    yaT = dt("yaT", [64, NTOK], BF16, kind="ExternalOutput").ap()
    ysT = dt("ysT", [64, NTOK], BF16, kind="ExternalOutput").ap()

    P = Prog(nc)
    V, A, T, G, SY, PL, VQ = _helpers(P)
    with contextlib.ExitStack() as st:
        def sb(name, shape, dtype):
            return st.enter_context(nc.sbuf_tensor(name, shape, dtype))

        def pp(name, shape, dtype=F32):
            return st.enter_context(nc.psum_tensor(name, shape, dtype))
        WQ = sb("WQ", [128, 8, 64], BF16)
        WK = sb("WK", [128, 8, 65], BF16)
        WVU = sb("WVU", [128, 8, 128], BF16)
        G1 = sb("G1", [128, 8], F32)
        NBF = sb("NBF", [65, 2], F32)
        GQK = sb("GQK", [64, 2], F32)
        ARE = sb("ARE", [128, 2], F32)
        AIM = sb("AIM", [128, 2], F32)
        LDT = sb("LDT", [128, 2], F32)
        BRP = sb("BRP", [128, 2, 128], BF16)
        BIP = sb("BIP", [128, 2, 128], BF16)
        CRP = sb("CRP", [128, 2, 128], BF16)
        CIP = sb("CIP", [128, 2, 128], BF16)
        CRN = sb("CRN", [128, 2, 128], BF16)
        CIN = sb("CIN", [128, 2, 128], BF16)
        DSK = sb("DSK", [128, 2], F32)
        ONB = sb("ONB", [128, 128], BF16)
        ONF = sb("ONF", [128, 128], F32)
        IDB = sb("IDB", [128, 128], BF16)
        IOT = sb("IOT", [128, 512], F32)
        MSK = sb("MSK", [128, 128], BF16)
        sm = {n: sb("sm_" + n, [128, 2], F32) for n in
              ["dt", "th", "rho", "k", "r", "ar", "sn", "cs", "lr", "li", "nr", "den", "t1", "t2", "cr", "ci",
               "cL", "sL", "CR", "CI", "ta", "tb"]}
        TR = sb("TR", [128, 2, 512], F32)
        TI = sb("TI", [128, 2, 512], F32)
        TC = sb("TC", [128, 2, 512], F32)
        TS = sb("TS", [128, 2, 512], F32)
        RB = sb("RB", [128, 2, 512], F32)
        QA = sb("QA", [128, S], BF16)
        KA = sb("KA", [128, S], BF16)
        VTM = sb("VTM", [128, 64, 128], BF16)
        UT = sb("UT", [128, S], BF16)
        FR = [sb("FR%d" % i, [65, 512], F32) for i in range(2)]
        AR = sb("AR", [65, 512], F32)
        AH = sb("AH", [65, 512], BF16)
        AL = sb("AL", [65, 512], BF16)
        FCOL = sb("FCOL", [128, 64], F32)
        FQ0 = sb("FQ0", [128, 16], F32)
        CB = sb("CB", [128, 16, 64], F32)
        YA = [sb("YA%d" % i, [64, 512], BF16) for i in range(2)]
        YS = [sb("YS%d" % i, [128, 512], BF16) for i in range(2)]
        XSQ = sb("XSQ", [128, 8, 512], BF16)
        XBF = [sb("XBF%d" % i, [128, 8, 512], BF16) for i in range(2)]
        RSTD = [sb("RSTD%d" % i, [128, 512], F32) for i in range(2)]
        RS = RSTD
        XST = [sb("XST%d" % i, [128, 512], F32) for i in range(4)]
        QF = [[sb("QF%d_%d" % (i, j), [64, 512], F32) for j in range(2)] for i in range(2)]
        SQ1 = [sb("SQ1%d" % i, [64, 512], BF16) for i in range(2)]
        RQ2 = [sb("RQ2%d" % i, [64, 512], F32) for i in range(2)]
        RQ = RQ2
        VB = [sb("VB%d" % i, [64, 512], BF16) for i in range(2)]
        FF = [sb("FF%d" % i, [65, 512], F32) for i in range(2)]
        FE = sb("FE", [65, 512], F32)
        ONR = sb("ONR", [65, 512], F32)
        PT = [sb("PT%d" % i, [128, 512], BF16) for i in range(2)]
        OS = [sb("OS%d" % i, [65, 512], F32) for i in range(2)]
        f1 = sb("f1", [128, 512], F32)
        f2 = sb("f2", [128, 512], F32)
        f3 = sb("f3", [128, 512], F32)
        f4 = sb("f4", [128, 512], F32)
        PH, PK, PA2 = f1, f2, f3
        WRl = [sb("WRr%d" % i, [128, 512], F32) for i in range(2)]
        WIl = [sb("WIi%d" % i, [128, 512], F32) for i in range(2)]
        bb = [[sb("b%d_%d" % (i, j), [128, 512], BF16) for i in range(4)] for j in range(2)]
        B = [pp("B%d" % i, [128, 512]) for i in range(7)]
        PTB = pp("PTB", [128, 4, 64], BF16)

        def ldw(dst, src, key):
            G(lambda e: e.dma_start(out=dst, in_=src), w=[key])
        ldw(WQ[:], wq.rearrange("(k p) n -> p k n", p=128), "WQ")
        ldw(WK[:], wk.rearrange("(k p) n -> p k n", p=128), "WK")
        ldw(WVU[:], wvu.rearrange("(k p) n -> p k n", p=128), "WVU")
        for (dst, src, key) in [(G1[:], g1, "G1"), (NBF[:], nbf, "NBF"), (GQK[:], gqk, "GQK"), (ARE[:], are, "ARE"),
                                (AIM[:], aim, "AIM"), (LDT[:], ldt, "LDT"), (BRP[64:128], brp, "BRP"),
                                (BIP[64:128], bip, "BIP"), (CRP[:], crp, "CRP"), (CIP[:], cip, "CIP"),
                                (DSK[64:128], dsk, "DSK"), (ONB[:], onesb, "ONB"), (ONF[:], onesb, "ONF"),
                                (IDB[:], identb, "IDB"), (IOT[:], iota1, "IOT"), (MSK[:], maskd, "MSK")]:
            ldw(dst, src, key)

        def xload(gb):
            T0 = gb * 512
            p = gb % 2
            for k in range(8):
                si = (gb * 8 + k) % 4
                if k < 4:
                    G(lambda e, k=k, T0=T0, p=p: e.dma_start(out=XBF[p][:, k, :],
                                                              in_=xT[k * 128:(k + 1) * 128, T0:T0 + 512]),
                      w=[("XBF", p, k)])
                else:
                    si = k - 4
                    SY(lambda e, k=k, T0=T0, si=si: e.dma_start(out=XST[si][:],
                                                                in_=xT[k * 128:(k + 1) * 128, T0:T0 + 512]),
                       w=[("XST", si)])
                    A(lambda e, k=k, p=p, si=si: e.activation(out=XBF[p][:, k, :], in_=XST[si][:], func=AF.Copy),
                      r=[("XST", si)], w=[("XBF", p, k)])
        xload(0)
        for (Wt, key) in ((WQ, "WQ"), (WK, "WK"), (WVU, "WVU")):
            for k in range(8):
                V(lambda e, Wt=Wt, k=k: e.tensor_scalar(out=Wt[:, k, :], in0=Wt[:, k, :], scalar1=G1[:, k:k + 1],
                                                        scalar2=None, op0=OP.mult), r=["G1", key], w=[key])
        V(lambda e: e.tensor_scalar(out=GQK[:, 0:1], in0=GQK[:, 0:1], scalar1=0.125, scalar2=None, op0=OP.mult),
          r=["GQK"], w=["GQK"])
        V(lambda e: e.tensor_scalar(out=NBF[:], in0=NBF[:], scalar1=-1.0, scalar2=None, op0=OP.mult), r=["NBF"],
          w=["NBF"])
        V(lambda e: e.memset(ONR[:], 1.0), w=["ONR"])
        V(lambda e: e.memset(QA[:], 0.0), w=["QA"])
        V(lambda e: e.memset(KA[:], 0.0), w=["KA"])
        V(lambda e: e.memset(KA[64:66, :], 1.0), r=["KA"], w=["KA"])
        V(lambda e: e.memset(VTM[:], 1.0), w=["VTM"])
        V(lambda e: e.tensor_scalar(out=CRN[:], in0=CRP[:], scalar1=-1.0, scalar2=None, op0=OP.mult), r=["CRP"],
          w=["CRN"])
        V(lambda e: e.tensor_scalar(out=CIN[:], in0=CIP[:], scalar1=-1.0, scalar2=None, op0=OP.mult), r=["CIP"],
          w=["CIN"])

        def VS(fn):
            V(fn, r=["S5T", "ARE", "AIM", "LDT", "IOT"], w=["S5T"])

        def AS(fn):
            A(fn, r=["S5T", "LDT"], w=["S5T"])

        def rred(out, in_, k_t):
            VS(lambda e: e.tensor_scalar(out=k_t, in0=in_, scalar1=INV2PI, scalar2=MAGIC, op0=OP.mult, op1=OP.add))
            VS(lambda e: e.tensor_scalar(out=k_t, in0=k_t, scalar1=MAGIC, scalar2=None, op0=OP.subtract))
            VS(lambda e: e.scalar_tensor_tensor(out=out, in0=k_t, scalar=-C1, in1=in_, op0=OP.mult, op1=OP.add))
            VS(lambda e: e.scalar_tensor_tensor(out=out, in0=k_t, scalar=-C2, in1=out, op0=OP.mult, op1=OP.add))
            VS(lambda e: e.tensor_scalar(out=out, in0=out, scalar1=PI, scalar2=-PI, op0=OP.min, op1=OP.max))

        def sincos(sn, cs, r, tmp):
            AS(lambda e: e.activation(out=sn, in_=r, func=AF.Sin))
            VS(lambda e: e.tensor_scalar(out=tmp, in0=r, scalar1=-1.0, scalar2=None, op0=OP.mult))
            VS(lambda e: e.tensor_tensor(out=tmp, in0=tmp, in1=r, op=OP.max))
            VS(lambda e: e.tensor_scalar(out=tmp, in0=tmp, scalar1=-1.0, scalar2=PI / 2, op0=OP.mult, op1=OP.add))
            AS(lambda e: e.activation(out=cs, in_=tmp, func=AF.Sin))
        s_ = {k: v[:] for k, v in sm.items()}
        AS(lambda e: e.activation(out=s_["dt"], in_=LDT[:], func=AF.Exp))
        VS(lambda e: e.tensor_tensor(out=s_["th"], in0=AIM[:], in1=s_["dt"], op=OP.mult))
        VS(lambda e: e.tensor_tensor(out=s_["t1"], in0=ARE[:], in1=s_["dt"], op=OP.mult))
        AS(lambda e: e.activation(out=s_["rho"], in_=s_["t1"], func=AF.Exp))
        rred(s_["r"], s_["th"], s_["k"])
        sincos(s_["sn"], s_["cs"], s_["r"], s_["ar"])
        VS(lambda e: e.tensor_tensor(out=s_["lr"], in0=s_["rho"], in1=s_["cs"], op=OP.mult))
        VS(lambda e: e.tensor_tensor(out=s_["li"], in0=s_["rho"], in1=s_["sn"], op=OP.mult))
        VS(lambda e: e.tensor_scalar(out=s_["nr"], in0=s_["lr"], scalar1=-1.0, scalar2=None, op0=OP.add))
        VS(lambda e: e.tensor_tensor(out=s_["t1"], in0=ARE[:], in1=ARE[:], op=OP.mult))
        VS(lambda e: e.tensor_tensor(out=s_["t2"], in0=AIM[:], in1=AIM[:], op=OP.mult))
        VS(lambda e: e.tensor_tensor(out=s_["den"], in0=s_["t1"], in1=s_["t2"], op=OP.add))
        VS(lambda e: e.reciprocal(out=s_["den"], in_=s_["den"]))
        VS(lambda e: e.tensor_tensor(out=s_["t1"], in0=s_["nr"], in1=ARE[:], op=OP.mult))
        VS(lambda e: e.tensor_tensor(out=s_["t2"], in0=s_["li"], in1=AIM[:], op=OP.mult))
        VS(lambda e: e.tensor_tensor(out=s_["t1"], in0=s_["t1"], in1=s_["t2"], op=OP.add))
        VS(lambda e: e.tensor_tensor(out=s_["cr"], in0=s_["t1"], in1=s_["den"], op=OP.mult))
        VS(lambda e: e.tensor_tensor(out=s_["t1"], in0=s_["li"], in1=ARE[:], op=OP.mult))
        VS(lambda e: e.tensor_tensor(out=s_["t2"], in0=s_["nr"], in1=AIM[:], op=OP.mult))
        VS(lambda e: e.tensor_tensor(out=s_["t1"], in0=s_["t1"], in1=s_["t2"], op=OP.subtract))
        VS(lambda e: e.tensor_tensor(out=s_["ci"], in0=s_["t1"], in1=s_["den"], op=OP.mult))
        for gh in range(2):
            VS(lambda e, gh=gh: e.tensor_scalar(out=PH[:], in0=IOT[:], scalar1=sm["th"][:, gh:gh + 1], scalar2=None,
                                                op0=OP.mult))
            rred(PA2[:], PH[:], PK[:])
            sincos(TS[:, gh, :], TC[:, gh, :], PA2[:], PK[:])
            VS(lambda e, gh=gh: e.tensor_scalar(out=PH[:], in0=TS[:, gh, :], scalar1=sm["ci"][:, gh:gh + 1],
                                                scalar2=None, op0=OP.mult))
            VS(lambda e, gh=gh: e.scalar_tensor_tensor(out=TR[:, gh, :], in0=TC[:, gh, :],
                                                       scalar=sm["cr"][:, gh:gh + 1], in1=PH[:], op0=OP.mult,
                                                       op1=OP.add))
            VS(lambda e, gh=gh: e.tensor_scalar(out=PH[:], in0=TS[:, gh, :], scalar1=sm["cr"][:, gh:gh + 1],
                                                scalar2=None, op0=OP.mult))
            VS(lambda e, gh=gh: e.scalar_tensor_tensor(out=TI[:, gh, :], in0=TC[:, gh, :],
                                                       scalar=sm["ci"][:, gh:gh + 1], in1=PH[:], op0=OP.mult,
                                                       op1=OP.subtract))
            VS(lambda e, gh=gh: e.tensor_scalar(out=RB[:, gh, :], in0=IOT[:], scalar1=0.0,
                                                scalar2=sm["rho"][:, gh:gh + 1], op0=OP.mult, op1=OP.add))
            VS(lambda e, gh=gh: e.tensor_copy(out=sm["cL"][:, gh:gh + 1], in_=TC[:, gh, 511:512]))
            VS(lambda e, gh=gh: e.tensor_copy(out=sm["sL"][:, gh:gh + 1], in_=TS[:, gh, 511:512]))
        S5K = ["S5T"]
        V(lambda e: e.memset(f4[0:1, 0:2], 0.0), r=["S5T"], w=["f1", "f2", "f3", "f4"])

        def inproj_s1(s, b):
            gb = s * 16 + b
            p = gb % 2
            t0 = b * 512
            sl = slice(t0, t0 + 512)
            xk = ("XBF", p)
            if gb + 1 < 32:
                xload(gb + 1)
            for k in range(8):
                V(lambda e, k=k, p=p: e.tensor_tensor(out=XSQ[:, k, :], in0=XBF[p][:, k, :], in1=XBF[p][:, k, :],
                                                      op=OP.mult), r=[("XBF", p, k)], w=[("XSQ", k)])
            for k in range(8):
                T(lambda e, k=k, p=p: e.matmul(B[p][:], lhsT=ONB[:], rhs=XSQ[:, k, :], start=(k == 0), stop=(k == 7)),
                  r=[("XSQ", k), "ONB"], w=[("B", p)])
            A(lambda e, p=p: e.activation(out=RS[p][:], in_=B[p][:], func=AF.Ln, bias=EPS, scale=1.0 / D),
              r=[("B", p)], w=[("RSTD", p)])
            A(lambda e, p=p: e.activation(out=RSTD[p][:], in_=RS[p][:], func=AF.Exp, scale=-0.5), r=[("RSTD", p)],
              w=[("RSTD", p)])
            rk = ("RSTD", p)
            for qi, (Wt, wkey, M) in enumerate(((WQ, "WQ", 64), (WK, "WK", 65))):
                pb = 2 + qi
                for k in range(8):
                    T(lambda e, k=k, Wt=Wt, pb=pb, p=p, M=M: e.matmul(B[pb][0:M, :], lhsT=Wt[:, k, :],
                                                                       rhs=XBF[p][:, k, :], start=(k == 0),
                                                                       stop=(k == 7)),
                      r=[wkey, ("XBF", p, k)], w=[("B", pb)])
                V(lambda e, pb=pb, qi=qi, p=p: e.tensor_tensor(out=QF[qi][p][:], in0=B[pb][0:64, :],
                                                               in1=RSTD[p][0:64, :], op=OP.mult),
                  r=[("B", pb), rk], w=[("QF", qi, p)])
                if qi == 1:
                    V(lambda e, p=p: e.tensor_tensor(out=FF[p][64:65, :], in0=B[3][64:65, :],
                                                     in1=RSTD[p][64:65, :], op=OP.mult), r=[("B", 3), rk],
                      w=[("FF", p)])
            for k in range(8):
                T(lambda e, k=k, p=p: e.matmul(B[6][:], lhsT=WVU[:, k, :], rhs=XBF[p][:, k, :], start=(k == 0),
                                               stop=(k == 7)), r=["WVU", ("XBF", p, k)], w=[("B", 6)])
            V(lambda e, p=p: e.tensor_tensor(out=VB[p][:], in0=B[6][0:64, :], in1=RSTD[p][0:64, :], op=OP.mult),
              r=[("B", 6), rk], w=[("VB", p)])
            V(lambda e, sl=sl, p=p: e.tensor_tensor(out=UT[64:128, sl], in0=B[6][64:128, :], in1=RSTD[p][64:128, :],
                                                    op=OP.mult), r=[("B", 6), rk], w=[("UT", b)])

        def inproj_s2(s, b):
            gb = s * 16 + b
            p = gb % 2
            t0 = b * 512
            sl = slice(t0, t0 + 512)
            for qi, (dst, dkey, gcol) in enumerate(((QA, "QA", 0), (KA, "KA", 1))):
                A(lambda e, qi=qi, p=p: e.activation(out=SQ1[qi][:], in_=QF[qi][p][:], func=AF.Square),
                  r=[("QF", qi, p)], w=[("SQ1", qi)])
                T(lambda e, qi=qi: e.matmul(B[4 + qi][0:64, :], lhsT=ONB[0:64, 0:64], rhs=SQ1[qi][:], start=True,
                                            stop=True), r=[("SQ1", qi), "ONB"], w=[("B", 4 + qi)])
                A(lambda e, qi=qi: e.activation(out=RQ[qi][:], in_=B[4 + qi][0:64, :], func=AF.Ln, bias=EPS,
                                                scale=1.0 / 64), r=[("B", 4 + qi)], w=[("RQ2", qi)])
                A(lambda e, qi=qi: e.activation(out=RQ2[qi][:], in_=RQ[qi][:], func=AF.Exp, scale=-0.5),
                  r=[("RQ2", qi)], w=[("RQ2", qi)])
                V(lambda e, dst=dst, gcol=gcol, sl=sl, qi=qi, p=p: e.scalar_tensor_tensor(
                    out=dst[0:64, sl], in0=QF[qi][p][:], scalar=GQK[:, gcol:gcol + 1], in1=RQ2[qi][:], op0=OP.mult,
                    op1=OP.mult), r=[("QF", qi, p), ("RQ2", qi), "GQK"], w=[(dkey, b)])
            for tt in range(4):
                T(lambda e, tt=tt, p=p: e.transpose(PTB[:, tt, :], VB[p][:, tt * 128:(tt + 1) * 128],
                                                    IDB[0:64, 0:64]), r=[("VB", p), "IDB"], w=["PTB"])
            for tt in range(4):
                V(lambda e, tt=tt, b=b: e.tensor_copy(out=VTM[:, b * 4 + tt, 0:64], in_=PTB[:, tt, :]),
                  r=["PTB"], w=[("VTM", b * 4 + tt)])
            A(lambda e, p=p: e.activation(out=FE[64:65, :], in_=FF[p][64:65, :], func=AF.Exp, bias=NBF[64:65, 0:1],
                                          scale=-1.0), r=[("FF", p), "NBF"], w=["FE"])
            A(lambda e: e.activation(out=FE[64:65, :], in_=FE[64:65, :], func=AF.Ln, bias=1.0, scale=1.0),
              r=["FE"], w=["FE"])
            V(lambda e: e.tensor_scalar(out=FE[64:65, :], in0=FE[64:65, :], scalar1=-1.0, scalar2=None,
                                        op0=OP.mult), r=["FE"], w=["FE"])
            fp = b % 2
            if b == 0:
                V(lambda e, fp=fp: e.tensor_tensor_scan(out=FR[fp][64:65, :], data0=ONR[64:65, :],
                                                        data1=FE[64:65, :], initial=0.0, op0=OP.mult, op1=OP.add),
                  r=["FE", "ONR"], w=[("FR", fp)])
            else:
                V(lambda e, fp=fp: e.tensor_tensor_scan(out=FR[fp][64:65, :], data0=ONR[64:65, :],
                                                        data1=FE[64:65, :], initial=FR[1 - fp][64:65, 511:512],
                                                        op0=OP.mult, op1=OP.add),
                  r=["FE", "ONR", ("FR", 1 - fp)], w=[("FR", fp)])
            frk = ("FR", fp)
            V(lambda e, fp=fp: e.tensor_scalar(out=AR[64:65, :], in0=FR[fp][64:65, :], scalar1=FR[fp][64:65, 0:1],
                                               scalar2=None, op0=OP.subtract), r=[frk], w=["AR"])
            V(lambda e: e.tensor_copy(out=AH[64:65, :], in_=AR[64:65, :]), r=["AR"], w=["AH"])
            V(lambda e: e.tensor_tensor(out=AL[64:65, :], in0=AR[64:65, :], in1=AH[64:65, :], op=OP.subtract),
              r=["AR", "AH"], w=["AL"])
            VQ(lambda e, sl=sl: e.dma_start(out=QA[64:65, sl], in_=AH[64:65, :]), r=["AH"], w=[("QA", b)])
            VQ(lambda e, sl=sl: e.dma_start(out=QA[65:66, sl], in_=AL[64:65, :]), r=["AL"], w=[("QA", b)])
            for j in range(4):
                T(lambda e, j=j, fp=fp: e.matmul(B[5][:, 8 + j:9 + j], lhsT=FR[fp][64:65, j * 128:(j + 1) * 128],
                                                 rhs=ONF[64:65, 0:1], start=True, stop=True), r=[frk, "ONF"],
                  w=[("B", 5)])
            T(lambda e, fp=fp: e.matmul(B[5][:, 16:17], lhsT=ONF[64:65, :], rhs=FR[fp][64:65, 0:1], start=True,
                                        stop=True), r=[frk, "ONF"], w=[("B", 5)])
            V(lambda e, b=b: e.tensor_copy(out=FCOL[:, 4 * b:4 * b + 4], in_=B[5][:, 8:12]), r=[("B", 5)],
              w=["FCOL"])
            V(lambda e, b=b: e.tensor_copy(out=FQ0[:, b:b + 1], in_=B[5][:, 16:17]), r=[("B", 5)], w=["FQ0"])
            V(lambda e, b=b: e.tensor_scalar(out=CB[:, b, 0:4 * b + 4], in0=FCOL[:, 0:4 * b + 4], scalar1=-1.0,
                                             scalar2=FQ0[:, b:b + 1], op0=OP.mult, op1=OP.add),
              r=["FCOL", "FQ0"], w=[("CB", b)])

        def capture(fn, *args):
            saved = P.ops
            P.ops = []
            fn(*args)
            out = P.ops
            P.ops = saved
            return out

        def merge(la, lb):
            na, nb = len(la), len(lb)
            ia = ib = 0
            while ia < na or ib < nb:
                if ib >= nb or (ia < na and ia * nb <= ib * na):
                    P.ops.append(la[ia])
                    ia += 1
                else:
                    P.ops.append(lb[ib])
                    ib += 1
        def att_tiles():
            lst = []
            for qb in range(16):
                nk = 4 * qb + 4
                for j in range(nk):
                    lst.append((qb, j, nk))
            return lst

        def att_qk(s, i, qb, j, nk):
            sp = i % 2
            t0 = qb * 512
            dj = j - 4 * qb
            c0 = dj * 128 if dj > 0 else 0
            diag = dj >= 0
            T(lambda e: e.matmul(B[sp][:, c0:512], lhsT=KA[:, j * 128:(j + 1) * 128],
                                 rhs=QA[:, t0 + c0:t0 + 512], start=True, stop=(not diag)),
              r=[("KA", j // 4), ("QA", qb)], w=[("B", sp)])
            if diag:
                T(lambda e: e.matmul(B[sp][:, c0:c0 + 128], lhsT=IDB[:], rhs=MSK[:], start=False, stop=True),
                  r=["IDB", "MSK"], w=[("B", sp)])
            A(lambda e: e.activation(out=PT[sp][:, c0:512], in_=B[sp][:, c0:512], func=AF.Exp,
                                     bias=CB[:, qb, j:j + 1], scale=1.0), r=[("B", sp), ("CB", qb)], w=[("PT", sp)])

        def att_pv(s, i, qb, j, nk):
            sp = i % 2
            op_ = qb % 2
            t0 = qb * 512
            dj = j - 4 * qb
            c0 = dj * 128 if dj > 0 else 0
            T(lambda e: e.matmul(B[2 + op_][:, c0:512], lhsT=VTM[:, j, :], rhs=PT[sp][:, c0:512],
                                 start=(j == 0), stop=(j == nk - 1)), r=[("VTM", j), ("PT", sp)], w=[("B", 2 + op_)])
            if j == nk - 1:
                V(lambda e: e.tensor_copy(out=OS[op_][:], in_=B[2 + op_][0:65, :]), r=[("B", 2 + op_)],
                  w=[("OS", op_)])
                V(lambda e: e.reciprocal(out=OS[op_][64:65, :], in_=OS[op_][64:65, :]), r=[("OS", op_)],
                  w=[("OS", op_)])
                T(lambda e: e.matmul(B[4][0:64, :], lhsT=ONF[64:65, 0:64], rhs=OS[op_][64:65, :], start=True,
                                     stop=True), r=[("OS", op_), "ONF"], w=[("B", 4)])
                V(lambda e: e.tensor_tensor(out=YA[op_][:], in0=OS[op_][0:64, :], in1=B[4][0:64, :], op=OP.mult),
                  r=[("OS", op_), ("B", 4)], w=[("YA", op_)])
                SY(lambda e: e.dma_start(out=yaT[:, s * S + t0:s * S + t0 + 512], in_=YA[op_][:]), r=[("YA", op_)],
                   w=["yaT"])

        def s5_front(s, b, gh):
            u = b * 2 + gh
            bp = u % 2
            WR_, WI_ = WRl[bp], WIl[bp]
            t0 = b * 512
            sl = slice(t0, t0 + 512)
            if b == 0 and gh == 0:
                V(lambda e: e.memset(sm["CR"][:], 0.0), w=["CRI"])
                V(lambda e: e.memset(sm["CI"][:], 0.0), w=["CRI"])
            T(lambda e: e.matmul(B[5][:], lhsT=BRP[64:128, gh, :], rhs=UT[64:128, sl], start=True, stop=True),
              r=["BRP", ("UT", b)], w=[("B", 5)])
            T(lambda e: e.matmul(B[6][:], lhsT=BIP[64:128, gh, :], rhs=UT[64:128, sl], start=True, stop=True),
              r=["BIP", ("UT", b)], w=[("B", 6)])
            V(lambda e: e.tensor_tensor(out=f1[:], in0=B[5][:], in1=TR[:, gh, :], op=OP.mult), r=[("B", 5)] + S5K,
              w=["f1"])
            V(lambda e: e.tensor_tensor(out=f2[:], in0=B[6][:], in1=TI[:, gh, :], op=OP.mult), r=[("B", 6)] + S5K,
              w=["f2"])
            V(lambda e: e.tensor_tensor(out=f3[:], in0=B[5][:], in1=TI[:, gh, :], op=OP.mult), r=[("B", 5)] + S5K,
              w=["f3"])
            V(lambda e: e.tensor_tensor(out=f4[:], in0=B[6][:], in1=TR[:, gh, :], op=OP.mult), r=[("B", 6)] + S5K,
              w=["f4"])
            V(lambda e: e.tensor_tensor(out=f1[:], in0=f1[:], in1=f2[:], op=OP.subtract), r=["f1", "f2"], w=["f1"])
            V(lambda e: e.tensor_tensor(out=f3[:], in0=f3[:], in1=f4[:], op=OP.add), r=["f3", "f4"], w=["f3"])
            V(lambda e: e.tensor_tensor_scan(out=WR_[:], data0=RB[:, gh, :], data1=f1[:],
                                             initial=sm["CR"][:, gh:gh + 1], op0=OP.mult, op1=OP.add),
              r=["f1", "CRI"] + S5K, w=[("WR", bp)])
            V(lambda e: e.tensor_tensor_scan(out=WI_[:], data0=RB[:, gh, :], data1=f3[:],
                                             initial=sm["CI"][:, gh:gh + 1], op0=OP.mult, op1=OP.add),
              r=["f3", "CRI"] + S5K, w=[("WI", bp)])
            cr_, ci_ = sm["CR"][:, gh:gh + 1], sm["CI"][:, gh:gh + 1]
            cl_, sl_ = sm["cL"][:, gh:gh + 1], sm["sL"][:, gh:gh + 1]
            ta_, tb_ = sm["ta"][:, gh:gh + 1], sm["tb"][:, gh:gh + 1]
            kk = dict(r=[("WR", bp), ("WI", bp), "CRI"] + S5K, w=["CRI"])
            V(lambda e: e.tensor_tensor(out=ta_, in0=WR_[:, 511:512], in1=cl_, op=OP.mult), **kk)
            V(lambda e: e.tensor_tensor(out=tb_, in0=WI_[:, 511:512], in1=sl_, op=OP.mult), **kk)
            V(lambda e: e.tensor_tensor(out=cr_, in0=ta_, in1=tb_, op=OP.subtract), **kk)
            V(lambda e: e.tensor_tensor(out=ta_, in0=WR_[:, 511:512], in1=sl_, op=OP.mult), **kk)
            V(lambda e: e.tensor_tensor(out=tb_, in0=WI_[:, 511:512], in1=cl_, op=OP.mult), **kk)
            V(lambda e: e.tensor_tensor(out=ci_, in0=ta_, in1=tb_, op=OP.add), **kk)
            bt = bb[bp]
            for i, (src, key, tab) in enumerate(((WR_, "WR", TC), (WI_, "WI", TS), (WR_, "WR", TS), (WI_, "WI", TC))):
                PL(lambda e, i=i, src=src, tab=tab: e.tensor_tensor(out=bt[i][:], in0=src[:], in1=tab[:, gh, :],
                                                                    op=OP.mult), r=[(key, bp)] + S5K,
                   w=[("bb", bp, i)])

        def s5_cproj(s, b, gh):
            u = b * 2 + gh
            bp = u % 2
            bt = bb[bp]
            for i, (Wc, wkey) in enumerate(((CRP, "CRP"), (CRN, "CRN"), (CIN, "CIN"), (CIN, "CIN"))):
                T(lambda e, i=i, Wc=Wc: e.matmul(B[4][:], lhsT=Wc[:, gh, :], rhs=bt[i][:],
                                                 start=(gh == 0 and i == 0), stop=(gh == 1 and i == 3)),
                  r=[wkey, ("bb", bp, i)], w=[("B", 4)])

        def s5_out(s, b):
            t0 = b * 512
            sl = slice(t0, t0 + 512)
            yp = b % 2
            V(lambda e: e.scalar_tensor_tensor(out=YS[yp][64:128, :], in0=UT[64:128, sl], scalar=DSK[64:128, 0:1],
                                               in1=B[4][64:128, :], op0=OP.mult, op1=OP.add),
              r=[("UT", b), "DSK", ("B", 4)], w=[("YS", yp)])
            SY(lambda e: e.dma_start(out=ysT[:, s * S + t0:s * S + t0 + 512], in_=YS[yp][64:128, :]),
               r=[("YS", yp)], w=["ysT"])

        for s in range(2):
            P.ops.extend(capture(inproj_s1, s, 0))
            for b in range(16):
                la = capture(inproj_s2, s, b)
                lb = capture(inproj_s1, s, b + 1) if b + 1 < 16 else []
                merge(la, lb)
            tiles = att_tiles()
            units = [(b, gh) for b in range(16) for gh in range(2)]
            nt = len(tiles)
            nu = len(units)
            per = (nt + nu - 1) // nu
            ti = 0
            for k in range(nu + 3):
                if 0 <= k - 2 < nu and units[k - 2][1] == 1:
                    s5_out(s, units[k - 2][0])
                if k < nu:
                    s5_front(s, *units[k])
                hi = min(nt, (k + 1) * per) if k < nu - 1 else nt
                while ti < hi:
                    att_qk(s, ti, *tiles[ti])
                    if ti >= 1:
                        att_pv(s, ti - 1, *tiles[ti - 1])
                    ti += 1
                if ti == nt and k == nu - 1:
                    att_pv(s, nt - 1, *tiles[nt - 1])
                if 0 <= k - 1 < nu:
                    s5_cproj(s, *units[k - 1])
        P.emit()
    return nc


def build_b():
    nc = bass.Bass("TRN2", target_bir_lowering=False)
    dt = nc.dram_tensor
    xT = dt("xT", [D, TB], F32, kind="ExternalInput").ap()
    yaT = dt("yaT", [512, TB], BF16, kind="ExternalInput").ap()
    ysT = dt("ysT", [512, TB], BF16, kind="ExternalInput").ap()
    wgt = dt("wgt", [D, 2048], F32, kind="ExternalInput").ap()
    g1 = dt("g1", [128, 8], F32, kind="ExternalInput").ap()
    g2 = dt("g2", [128, 8], F32, kind="ExternalInput").ap()
    wglu = dt("wglu", [512, 512], F32, kind="ExternalInput").ap()
    wpa = dt("wpa", [512, D], F32, kind="ExternalInput").ap()
    wps = dt("wps", [512, D], F32, kind="ExternalInput").ap()
    wout = dt("wout", [D, D], F32, kind="ExternalInput").ap()
    wr = dt("wr", [D, 36], F32, kind="ExternalInput").ap()
    br = dt("br", [128, 36], F32, kind="ExternalInput").ap()
    weg = dt("weg", [32, D, 256], F32, kind="ExternalInput").ap()
    weu = dt("weu", [32, D, 256], F32, kind="ExternalInput").ap()
    wed = dt("wed", [32, 256, D], F32, kind="ExternalInput").ap()
    onesb = dt("onesb", [128, 128], F32, kind="ExternalInput").ap()
    identf = dt("identf", [128, 128], F32, kind="ExternalInput").ap()
    sel = dt("sel", [32, 32 * 128], F32, kind="ExternalInput").ap()
    outT = dt("outT", [D, TB], F32, kind="ExternalOutput").ap()

    P = Prog(nc)
    V, A, T, G, SY, PL, VQ = _helpers(P)
    with contextlib.ExitStack() as st:
        def sb(name, shape, dtype):
            return st.enter_context(nc.sbuf_tensor(name, shape, dtype))

        def pp(name, shape, dtype=F32):
            return st.enter_context(nc.psum_tensor(name, shape, dtype))
        X = sb("X", [128, 8, TB], F32)
        WG = sb("WG", [128, 8, 2048], BF16)
        H2 = WG
        YY = sb("YY", [128, 2, 4, 512], BF16)
        YAs = YY[:, 0]
        YSs = YY[:, 1]
        WGLU = sb("WGLU", [128, 4, 512], BF16)
        WPA = sb("WPA", [128, 4, D], BF16)
        WPS = sb("WPS", [128, 4, D], BF16)
        WOUT = sb("WOUT", [128, 8, D], BF16)
        WR = sb("WR", [128, 8, 36], F32)
        BR = sb("BR", [128, 36], F32)
        G1 = sb("G1", [128, 8], F32)
        G2 = sb("G2", [128, 8], F32)
        ONB = sb("ONB", [128, 128], BF16)
        IDF = sb("IDF", [128, 128], F32)
        XSQ = sb("XSQ", [128, 8, 512], BF16)
        XBF = sb("XBF", [128, 8, 512], BF16)
        MIX = XSQ
        SELt = WPS[0:32].rearrange("p a b -> p (a b)")
        WEG = [WOUT[:, :, 0:256], XBF[:, :, 0:256]]
        WEU = [WOUT[:, :, 256:512], XBF[:, :, 256:512]]
        WED = [WPA[:, 0:2, :], YY[:, 0].rearrange("p a b -> p (a b)").rearrange("p (f n) -> p f n", f=2)]
        WKEY = [["WOUT", "WOUT", "WPA"], ["XBF", "XBF", "YY"]]
        CT = sb("CT", [32, TB], BF16)
        H2F = sb("H2F", [128, 8, 128], F32)
        RS = sb("RS", [128, 512], F32)
        RSTD = sb("RSTD", [128, 512], F32)
        YG = sb("YG", [128, 4, 512], BF16)
        fa = [sb("fa%d" % i, [128, 512], F32) for i in range(2)]
        fb = [sb("fb%d" % i, [128, 512], F32) for i in range(2)]
        fc = [sb("fc%d" % i, [128, 512], BF16) for i in range(2)]
        HS = [sb("HS%d" % i, [128, 2, 512], BF16) for i in range(2)]
        LG = sb("LG", [128, 4, 36], F32)
        MK = sb("MK", [128, 4, 32], F32)
        CM = sb("CM", [128, 4, 32], F32)
        CM2 = sb("CM2", [128, 4, 32], F32)
        T8 = sb("T8", [128, 4, 8], F32)
        sc1 = {n: sb("sc_" + n, [128, 4], F32) for n in ["gmax", "ngmax", "gsum", "gtop", "d", "s1", "w1", "w2"]}
        sc4 = {n: sb("sc4_" + n, [128, 4, 4], F32) for n in ["mg", "nb", "eg"]}
        B = [pp("B%d" % i, [128, 512]) for i in range(8)]

        for k in range(8):
            SY(lambda e, k=k: e.dma_start(out=X[:, k, :], in_=xT[k * 128:(k + 1) * 128, :]), w=[("X", k)])

        def ldw(dst, src, key):
            G(lambda e: e.dma_start(out=dst, in_=src), w=[key])
        for (dst, src, key) in [(G1[:], g1, "G1"), (ONB[:], onesb, "ONB")]:
            ldw(dst, src, key)
        wgt_v = wgt.rearrange("(k p) n -> p k n", p=128)
        for n in range(8):
            for off in (0, 1024):
                c0 = off + n * 128
                G(lambda e, c0=c0: e.dma_start(out=WG[:, :, c0:c0 + 128], in_=wgt_v[:, :, c0:c0 + 128]),
                  w=[("WGc", c0)])
        ldw(WGLU[:], wglu.rearrange("(k p) n -> p k n", p=128), "WGLU")
        ldw(WPA[:], wpa.rearrange("(k p) n -> p k n", p=128), "WPA")
        ldw(WPS[:], wps.rearrange("(k p) n -> p k n", p=128), "WPS")
        ldw(WOUT[:], wout.rearrange("(k p) n -> p k n", p=128), "WOUT")
        ldw(WR[:], wr.rearrange("(k p) n -> p k n", p=128), "WR")
        for (dst, src, key) in [(BR[:], br, "BR"), (G2[:], g2, "G2"), (IDF[:], identf, "IDF")]:
            ldw(dst, src, key)
        for n in range(8):
            for off in (0, 1024):
                c0 = off + n * 128
                for k in range(8):
                    V(lambda e, k=k, c0=c0: e.tensor_scalar(out=WG[:, k, c0:c0 + 128], in0=WG[:, k, c0:c0 + 128],
                                                            scalar1=G1[:, k:k + 1], scalar2=None, op0=OP.mult),
                      r=["G1", ("WGc", c0)], w=[("WGc", c0)])

        def rmsstat(sl, bank):
            for k in range(8):
                A(lambda e, k=k: e.activation(out=XSQ[:, k, :], in_=X[:, k, sl], func=AF.Square), r=[("X", k)],
                  w=[("XSQ", k)])
            for k in range(8):
                T(lambda e, k=k: e.matmul(B[bank][:], lhsT=ONB[:], rhs=XSQ[:, k, :], start=(k == 0), stop=(k == 7)),
                  r=[("XSQ", k), "ONB"], w=[("B", bank)])
            A(lambda e: e.activation(out=RS[:], in_=B[bank][:], func=AF.Sqrt, bias=EPS, scale=1.0 / D),
              r=[("B", bank)], w=["RS"])
            V(lambda e: e.reciprocal(out=RSTD[:], in_=RS[:]), r=["RS"], w=["RSTD"])

        for b in range(4):
            sl = slice(b * 512, (b + 1) * 512)
            G(lambda e, sl=sl: e.dma_start(out=YAs, in_=yaT.rearrange("(k p) t -> p k t", p=128)[:, :, sl]),
              w=[("YY", 0)])
            G(lambda e, sl=sl: e.dma_start(out=YSs, in_=ysT.rearrange("(k p) t -> p k t", p=128)[:, :, sl]),
              w=[("YY", 1)])
            for k in range(8):
                PL(lambda e, k=k, sl=sl: e.tensor_copy(out=XBF[:, k, :], in_=X[:, k, sl]), r=[("X", k)],
                   w=[("XBF", k)])
            rmsstat(sl, 0)
            for m in range(4):
                q = m % 2
                V(lambda e, m=m, q=q: e.tensor_tensor(out=fa[q][:], in0=YSs[:, m, :], in1=YSs[:, m, :], op=OP.mult),
                  r=[("YY", 1)], w=[("fa", q)])
                V(lambda e, q=q: e.tensor_scalar(out=fa[q][:], in0=fa[q][:], scalar1=0.044715, scalar2=1.0,
                                                 op0=OP.mult, op1=OP.add), r=[("fa", q)], w=[("fa", q)])
                V(lambda e, m=m, q=q: e.tensor_tensor(out=fa[q][:], in0=fa[q][:], in1=YSs[:, m, :], op=OP.mult),
                  r=[("fa", q), ("YY", 1)], w=[("fa", q)])
                A(lambda e, q=q: e.activation(out=fb[q][:], in_=fa[q][:], func=AF.Sigmoid,
                                              scale=1.5957691216057308), r=[("fa", q)], w=[("fb", q)])
                V(lambda e, m=m, q=q: e.tensor_tensor(out=YSs[:, m, :], in0=YSs[:, m, :], in1=fb[q][:], op=OP.mult),
                  r=[("fb", q), ("YY", 1)], w=[("YY", 1)])
            for m in range(4):
                bk = 1 + (m % 2)
                for k in range(4):
                    T(lambda e, m=m, k=k, bk=bk: e.matmul(B[bk][:], lhsT=WGLU[:, k, m * 128:(m + 1) * 128],
                                                          rhs=YSs[:, k, :], start=(k == 0), stop=(k == 3)),
                      r=["WGLU", ("YY", 1)], w=[("B", bk)])
                A(lambda e, bk=bk, m=m: e.activation(out=fa[m % 2][:], in_=B[bk][:], func=AF.Sigmoid),
                  r=[("B", bk)], w=[("fa", m % 2)])
                V(lambda e, m=m: e.tensor_tensor(out=YG[:, m, :], in0=YSs[:, m, :], in1=fa[m % 2][:], op=OP.mult),
                  r=[("YY", 1), ("fa", m % 2)], w=[("YG", m)])
            for n in range(8):
                q = n % 2
                b0, b1, b2, b3 = (0 + 4 * q, 1 + 4 * q, 2 + 4 * q, 3 + 4 * q)
                for k in range(8):
                    T(lambda e, n=n, k=k, b0=b0: e.matmul(B[b0][:], lhsT=WG[:, k, n * 128:(n + 1) * 128],
                                                          rhs=XBF[:, k, :], start=(k == 0), stop=(k == 7)),
                      r=[("WGc", n * 128), ("XBF", k)], w=[("B", b0)])
                for k in range(8):
                    T(lambda e, n=n, k=k, b1=b1: e.matmul(B[b1][:],
                                                          lhsT=WG[:, k, 1024 + n * 128:1024 + (n + 1) * 128],
                                                          rhs=XBF[:, k, :], start=(k == 0), stop=(k == 7)),
                      r=[("WGc", 1024 + n * 128), ("XBF", k)], w=[("B", b1)])
                for k in range(4):
                    T(lambda e, n=n, k=k, b2=b2: e.matmul(B[b2][:], lhsT=WPA[:, k, n * 128:(n + 1) * 128],
                                                          rhs=YAs[:, k, :], start=(k == 0), stop=(k == 3)),
                      r=["WPA", ("YY", 0)], w=[("B", b2)])
                for k in range(4):
                    T(lambda e, n=n, k=k, b3=b3: e.matmul(B[b3][:], lhsT=WPS[:, k, n * 128:(n + 1) * 128],
                                                          rhs=YG[:, k, :], start=(k == 0), stop=(k == 3)),
                      r=["WPS", ("YG", k)], w=[("B", b3)])
                V(lambda e, q=q, b0=b0: e.tensor_tensor(out=fa[q][:], in0=B[b0][:], in1=RSTD[:], op=OP.mult),
                  r=[("B", b0), "RSTD"], w=[("fa", q)])
                A(lambda e, q=q: e.activation(out=fa[q][:], in_=fa[q][:], func=AF.Sigmoid), r=[("fa", q)],
                  w=[("fa", q)])
                V(lambda e, q=q, b1=b1: e.tensor_tensor(out=fb[q][:], in0=B[b1][:], in1=RSTD[:], op=OP.mult),
                  r=[("B", b1), "RSTD"], w=[("fb", q)])
                A(lambda e, q=q: e.activation(out=fb[q][:], in_=fb[q][:], func=AF.Sigmoid), r=[("fb", q)],
                  w=[("fb", q)])
                V(lambda e, q=q, b2=b2: e.tensor_tensor(out=fa[q][:], in0=B[b2][:], in1=fa[q][:], op=OP.mult),
                  r=[("B", b2), ("fa", q)], w=[("fa", q)])
                V(lambda e, q=q, b3=b3: e.tensor_tensor(out=fb[q][:], in0=B[b3][:], in1=fb[q][:], op=OP.mult),
                  r=[("B", b3), ("fb", q)], w=[("fb", q)])
                PL(lambda e, n=n, q=q: e.tensor_tensor(out=MIX[:, n, :], in0=fa[q][:], in1=fb[q][:], op=OP.add),
                   r=[("fa", q), ("fb", q)], w=[("XSQ", n)])
            for n in range(8):
                bk = n % 4
                for k in range(8):
                    T(lambda e, n=n, k=k, bk=bk: e.matmul(B[bk][:], lhsT=WOUT[:, k, n * 128:(n + 1) * 128],
                                                          rhs=MIX[:, k, :], start=(k == 0), stop=(k == 7)),
                      r=["WOUT", ("XSQ", k)], w=[("B", bk)])
                V(lambda e, n=n, sl=sl, bk=bk: e.tensor_tensor(out=X[:, n, sl], in0=X[:, n, sl], in1=B[bk][:],
                                                               op=OP.add), r=[("X", n), ("B", bk)], w=[("X", n)])

        def load_expert(ex):
            p = ex % 2
            G(lambda e: e.dma_start(out=WEG[p], in_=weg[ex].rearrange("(k p) n -> p k n", p=128)), w=[WKEY[p][0]])
            G(lambda e: e.dma_start(out=WEU[p], in_=weu[ex].rearrange("(k p) n -> p k n", p=128)), w=[WKEY[p][1]])
            G(lambda e: e.dma_start(out=WED[p], in_=wed[ex].rearrange("(k p) n -> p k n", p=128)), w=[WKEY[p][2]])
        ldw(SELt, sel, "WPS")
        load_expert(0)
        load_expert(1)

        for b in range(4):
            sl = slice(b * 512, (b + 1) * 512)
            rmsstat(sl, 0)
            for tt in range(4):
                ts_ = slice(tt * 128, (tt + 1) * 128)
                gs_ = slice(b * 512 + tt * 128, b * 512 + (tt + 1) * 128)
                for n in range(8):
                    V(lambda e, n=n, gs_=gs_, ts_=ts_: e.scalar_tensor_tensor(
                        out=H2F[:, n, :], in0=X[:, n, gs_], scalar=G2[:, n:n + 1], in1=RSTD[:, ts_], op0=OP.mult,
                        op1=OP.mult), r=[("X", n), "G2", "RSTD"], w=[("H2F", n)])
                    PL(lambda e, n=n, gs_=gs_: e.tensor_copy(out=H2[:, n, gs_], in_=H2F[:, n, :]), r=[("H2F", n)],
                       w=[("WG", n), "WGc"])
                for k in range(8):
                    T(lambda e, k=k, tt=tt: e.matmul(B[1 + tt][:, 0:36], lhsT=H2F[:, k, :], rhs=WR[:, k, :],
                                                     start=(k == 0), stop=(k == 7)), r=[("H2F", k), "WR"],
                      w=[("B", 1 + tt)])
            steps = []

            def chain(tt):
                RT = dict(r=[("RT", tt)], w=[("RT", tt)])
                LGt, MKt, CMt, CM2t, T8t = LG[:, tt, :], MK[:, tt, :], CM[:, tt, :], CM2[:, tt, :], T8[:, tt, :]
                g = {n: t[:, tt:tt + 1] for n, t in sc1.items()}
                eg, mg, nb_ = sc4["eg"][:, tt, :], sc4["mg"][:, tt, :], sc4["nb"][:, tt, :]
                ops = []
                ops.append(lambda: V(lambda e: e.tensor_tensor(out=LGt, in0=B[1 + tt][:, 0:36], in1=BR[:], op=OP.add),
                                     r=[("B", 1 + tt), "BR", ("RT", tt)], w=[("RT", tt)]))
                ops.append(lambda: V(lambda e: e.tensor_reduce(out=g["gmax"], in_=LGt[:, 0:4], axis=AX.X, op=OP.max),
                                     **RT))
                ops.append(lambda: V(lambda e: e.tensor_scalar(out=g["ngmax"], in0=g["gmax"], scalar1=-1.0,
                                                               scalar2=None, op0=OP.mult), **RT))
                ops.append(lambda: A(lambda e: e.activation(out=eg, in_=LGt[:, 0:4], func=AF.Exp, bias=g["ngmax"],
                                                            scale=1.0), **RT))
                ops.append(lambda: V(lambda e: e.tensor_reduce(out=g["gsum"], in_=eg, axis=AX.X, op=OP.add), **RT))
                ops.append(lambda: V(lambda e: e.reciprocal(out=g["gtop"], in_=g["gsum"]), **RT))
                ops.append(lambda: V(lambda e: e.tensor_scalar(out=mg, in0=LGt[:, 0:4], scalar1=g["gmax"],
                                                               scalar2=None, op0=OP.is_equal), **RT))
                ops.append(lambda: V(lambda e: e.tensor_scalar(out=nb_, in0=mg, scalar1=-1.0, scalar2=1e30,
                                                               op0=OP.add, op1=OP.mult), **RT))
                for gi in range(4):
                    ops.append(lambda gi=gi: V(lambda e: e.tensor_scalar(
                        out=MKt[:, gi * 8:(gi + 1) * 8], in0=LGt[:, 4 + gi * 8:4 + (gi + 1) * 8],
                        scalar1=mg[:, gi:gi + 1], scalar2=nb_[:, gi:gi + 1], op0=OP.mult, op1=OP.add), **RT))
                ops.append(lambda: V(lambda e: e.max(out=T8t, in_=MKt), **RT))
                ops.append(lambda: V(lambda e: e.tensor_tensor(out=g["d"], in0=T8t[:, 1:2], in1=T8t[:, 0:1],
                                                               op=OP.subtract), **RT))
                ops.append(lambda: A(lambda e: e.activation(out=g["s1"], in_=g["d"], func=AF.Exp), **RT))
                ops.append(lambda: V(lambda e: e.tensor_scalar(out=g["s1"], in0=g["s1"], scalar1=1.0, scalar2=None,
                                                               op0=OP.add), **RT))
                ops.append(lambda: V(lambda e: e.reciprocal(out=g["s1"], in_=g["s1"]), **RT))
                ops.append(lambda: V(lambda e: e.tensor_tensor(out=g["w1"], in0=g["s1"], in1=g["gtop"], op=OP.mult),
                                     **RT))
                ops.append(lambda: V(lambda e: e.tensor_tensor(out=g["w2"], in0=g["gtop"], in1=g["w1"],
                                                               op=OP.subtract), **RT))
                ops.append(lambda: V(lambda e: e.tensor_scalar(out=CMt, in0=MKt, scalar1=T8t[:, 0:1],
                                                               scalar2=g["w1"], op0=OP.is_equal, op1=OP.mult), **RT))
                ops.append(lambda: V(lambda e: e.tensor_scalar(out=CM2t, in0=MKt, scalar1=T8t[:, 1:2],
                                                               scalar2=g["w2"], op0=OP.is_equal, op1=OP.mult), **RT))
                ops.append(lambda: V(lambda e: e.tensor_tensor(out=CMt, in0=CMt, in1=CM2t, op=OP.add), **RT))
                ops.append(lambda: T(lambda e: e.transpose(B[5][0:32, tt * 128:(tt + 1) * 128], CMt, IDF[:]),
                                     r=[("RT", tt), "IDF"], w=[("B", 5)]))
                return ops
            chains = [chain(tt) for tt in range(4)]
            for i in range(len(chains[0])):
                for tt in range(4):
                    chains[tt][i]()
            V(lambda e, b=b: e.tensor_copy(out=CT[:, b * 512:(b + 1) * 512], in_=B[5][0:32, :]), r=[("B", 5)],
              w=[("CT", b)])

        units = [(ex, b) for ex in range(32) for b in range(4)]

        def up(ui):
            ex, b = units[ui]
            p = ex % 2
            hp = ui % 2
            sl = slice(b * 512, (b + 1) * 512)
            T(lambda e: e.matmul(B[0][:], lhsT=SELt[:, ex * 128:(ex + 1) * 128], rhs=CT[:, sl], start=True,
                                 stop=True), r=["WPS", ("CT", b)], w=[("B", 0)])
            A(lambda e: e.activation(out=fc[hp][:], in_=B[0][:], func=AF.Copy), r=[("B", 0)], w=[("fc", hp)])
            for f in range(2):
                bg, bu = 1 + 2 * f, 2 + 2 * f
                for k in range(8):
                    T(lambda e, f=f, k=k, bg=bg: e.matmul(B[bg][:], lhsT=WEG[p][:, k, f * 128:(f + 1) * 128],
                                                          rhs=H2[:, k, sl], start=(k == 0), stop=(k == 7)),
                      r=[WKEY[p][0], ("WG", k)], w=[("B", bg)])
                for k in range(8):
                    T(lambda e, f=f, k=k, bu=bu: e.matmul(B[bu][:], lhsT=WEU[p][:, k, f * 128:(f + 1) * 128],
                                                          rhs=H2[:, k, sl], start=(k == 0), stop=(k == 7)),
                      r=[WKEY[p][1], ("WG", k)], w=[("B", bu)])
                A(lambda e, f=f, bg=bg: e.activation(out=fa[f][:], in_=B[bg][:], func=AF.Silu), r=[("B", bg)],
                  w=[("fa", f)])
                V(lambda e, f=f, bu=bu: e.tensor_tensor(out=fb[f][:], in0=B[bu][:], in1=fc[hp][:], op=OP.mult),
                  r=[("B", bu), ("fc", hp)], w=[("fb", f)])
                PL(lambda e, f=f: e.tensor_tensor(out=HS[hp][:, f, :], in0=fa[f][:], in1=fb[f][:], op=OP.mult),
                   r=[("fa", f), ("fb", f)], w=[("HS", hp, f)])

        def down(ui):
            ex, b = units[ui]
            p = ex % 2
            hp = ui % 2
            sl = slice(b * 512, (b + 1) * 512)
            for n in range(8):
                bk = 5 + (n % 3)
                for f in range(2):
                    T(lambda e, n=n, f=f, bk=bk: e.matmul(B[bk][:], lhsT=WED[p][:, f, n * 128:(n + 1) * 128],
                                                          rhs=HS[hp][:, f, :], start=(f == 0), stop=(f == 1)),
                      r=[WKEY[p][2], ("HS", hp, f)], w=[("B", bk)])
                V(lambda e, n=n, bk=bk: e.tensor_tensor(out=X[:, n, sl], in0=X[:, n, sl], in1=B[bk][:], op=OP.add),
                  r=[("X", n), ("B", bk)], w=[("X", n)])
            if b == 3 and ex + 2 < 32:
                load_expert(ex + 2)
        for ui in range(len(units)):
            up(ui)
            if ui >= 1:
                down(ui - 1)
        down(len(units) - 1)
        for k in range(8):
            SY(lambda e, k=k: e.dma_start(out=outT[k * 128:(k + 1) * 128, :], in_=X[:, k, :]), r=[("X", k)],
               w=["outT"])
        P.emit()
    return nc


def _consts():
    onesb = np.ones((128, 128), np.float32)
    ident = np.eye(128, dtype=np.float32)
    iota1 = np.tile(np.arange(1, 513, dtype=np.float32)[None, :], (128, 1))
    p = np.arange(128)
    maskd = np.where(p[:, None] > p[None, :], -30000.0, 0.0).astype(np.float32)
    sel = np.zeros((32, 32, 128), np.float32)
    for e in range(32):
        sel[e, e, :] = 1.0
    return onesb, ident, iota1, maskd, sel.reshape(32, 32 * 128)


def stage_a_inputs(x, norm_mix_g, w_in, b_forget, q_norm_g, k_norm_g, ssm_A_re, ssm_A_im, ssm_log_dt, ssm_B_re,
                   ssm_B_im, ssm_C_re, ssm_C_im, ssm_D):
    onesb, ident, iota1, maskd, _ = _consts()
    xT = np.ascontiguousarray(x.reshape(NTOK, D).T)
    w = w_in[0]
    g1 = np.ascontiguousarray(norm_mix_g[0].reshape(8, 128).T)
    maps = []
    for c in range(NCORES):
        wq = np.ascontiguousarray(w[:, c * 64:(c + 1) * 64])
        wk = np.ascontiguousarray(np.concatenate([w[:, 512 + c * 64:512 + (c + 1) * 64],
                                                  w[:, 1536 + c:1537 + c]], axis=1))
        wv = w[:, 1024 + c * 64:1024 + (c + 1) * 64]
        wu = w[:, 1544 + c * 64:1544 + (c + 1) * 64]
        wvu = np.ascontiguousarray(np.concatenate([wv, wu], axis=1))
        nbf = np.full((65, 2), b_forget[0, c], np.float32)
        gqk = np.ascontiguousarray(np.stack([q_norm_g[0], k_norm_g[0]], axis=1)).astype(np.float32)
        gs = np.arange(4 * c, 4 * c + 4)
        are = np.zeros((128, 2), np.float32)
        aim = np.zeros((128, 2), np.float32)
        ldt = np.zeros((128, 2), np.float32)
        brp = np.zeros((64, 2, 128), np.float32)
        bip = np.zeros((64, 2, 128), np.float32)
        crp = np.zeros((128, 2, 128), np.float32)
        cip = np.zeros((128, 2, 128), np.float32)
        for gh in range(2):
            for gl in range(2):
                gloc = 2 * gh + gl
                g = gs[gloc]
                are[gl * 64:(gl + 1) * 64, gh] = ssm_A_re[0, g]
                aim[gl * 64:(gl + 1) * 64, gh] = ssm_A_im[0, g]
                ldt[gl * 64:(gl + 1) * 64, gh] = ssm_log_dt[0, g]
                brp[gloc * 16:(gloc + 1) * 16, gh, gl * 64:(gl + 1) * 64] = ssm_B_re[0, g].T
                bip[gloc * 16:(gloc + 1) * 16, gh, gl * 64:(gl + 1) * 64] = ssm_B_im[0, g].T
                crp[gl * 64:(gl + 1) * 64, gh, 64 + gloc * 16:64 + (gloc + 1) * 16] = ssm_C_re[0, g].T
                cip[gl * 64:(gl + 1) * 64, gh, 64 + gloc * 16:64 + (gloc + 1) * 16] = ssm_C_im[0, g].T
        dsk = np.ascontiguousarray(np.repeat(ssm_D[0, c * 64:(c + 1) * 64][:, None], 2, axis=1)).astype(np.float32)
        maps.append(dict(xT=xT, wq=wq, wk=wk, wvu=wvu, g1=g1, nbf=nbf, gqk=gqk, are=are, aim=aim, ldt=ldt,
                         brp=brp, bip=bip, crp=crp, cip=cip, dsk=dsk, onesb=onesb, identb=ident, iota1=iota1,
                         maskd=maskd))
    return maps


def stage_b_inputs(x, ya_full, ys_full, norm_mix_g, w_in, w_glu, w_proj_attn, w_proj_ssm, w_out, norm_ffn_g,
                   w_router_group, b_router_group, w_router_expert, b_router_expert, w_expert_gate, w_expert_up,
                   w_expert_down):
    onesb, ident, iota1, maskd, sel = _consts()
    xf = x.reshape(NTOK, D)
    g1 = np.ascontiguousarray(norm_mix_g[0].reshape(8, 128).T)
    g2 = np.ascontiguousarray(norm_ffn_g[0].reshape(8, 128).T)
    wgt = np.ascontiguousarray(w_in[0][:, 2056:4104])
    wr = np.ascontiguousarray(np.concatenate([w_router_group[0], w_router_expert[0]], axis=1))
    br = np.ascontiguousarray(np.tile(np.concatenate([b_router_group[0], b_router_expert[0]])[None, :], (128, 1)))
    maps = []
    for c in range(NCORES):
        tsl = slice(c * TB, (c + 1) * TB)
        maps.append(dict(xT=np.ascontiguousarray(xf[tsl].T), yaT=np.ascontiguousarray(ya_full[:, tsl]),
                         ysT=np.ascontiguousarray(ys_full[:, tsl]), wgt=wgt, g1=g1, g2=g2, wglu=w_glu[0],
                         wpa=w_proj_attn[0], wps=w_proj_ssm[0], wout=w_out[0], wr=wr, br=br, weg=w_expert_gate[0],
                         weu=w_expert_up[0], wed=w_expert_down[0], onesb=onesb, identf=ident, sel=sel))
    return maps


def run_a(inputs):
    nc = build_a()
    keys = ["x", "norm_mix_g", "w_in", "b_forget", "q_norm_g", "k_norm_g", "ssm_A_re", "ssm_A_im", "ssm_log_dt",
            "ssm_B_re", "ssm_B_im", "ssm_C_re", "ssm_C_im", "ssm_D"]
    maps = stage_a_inputs(*[np.asarray(inputs[k], np.float32) for k in keys])
    res = run_bass_kernel_spmd(nc, maps, core_ids=list(range(NCORES)))
    ya = np.concatenate([np.asarray(res.results[c]["yaT"]) for c in range(NCORES)], axis=0)
    ys = np.concatenate([np.asarray(res.results[c]["ysT"]) for c in range(NCORES)], axis=0)
    return ya, ys


def run_b(inputs, ya, ys):
    nc = build_b()
    keys = ["norm_mix_g", "w_in", "w_glu", "w_proj_attn", "w_proj_ssm", "w_out", "norm_ffn_g", "w_router_group",
            "b_router_group", "w_router_expert", "b_router_expert", "w_expert_gate", "w_expert_up", "w_expert_down"]
    maps = stage_b_inputs(np.asarray(inputs["x"], np.float32), ya, ys,
                          *[np.asarray(inputs[k], np.float32) for k in keys])
    res = run_bass_kernel_spmd(nc, maps, core_ids=list(range(NCORES)))
    out = np.concatenate([np.asarray(res.results[c]["outT"]).T for c in range(NCORES)], axis=0)
    return out.reshape(2, S, D).astype(np.float32)


def kernel(**inputs):
    ya, ys = run_a(inputs)
    return run_b(inputs, ya, ys)
```

```python
import contextlib
import numpy as np
import ml_dtypes
import concourse.bass as bass
import concourse.mybir as mybir
from concourse.bass_utils import run_bass_kernel_spmd

F32 = mybir.dt.float32
BF16 = mybir.dt.bfloat16
AF = mybir.ActivationFunctionType
OP = mybir.AluOpType
AX = mybir.AxisListType

NCORES = 8
D = 1024
S = 8192
NTOK = 16384
TB = 2048
EPS = 1e-6
CH = 3000
NDS = 3
MAGIC = 12582912.0
INV2PI = 0.15915494309189535
C1 = 6.28125
C2 = 0.0019353071795864769
PI = 3.141592653589793

STREAMS = {'pe': ('tensor', False), 'dve': ('vector', False), 'act': ('scalar', False),
           'pool': ('gpsimd', False), 'gq': ('gpsimd', True), 'sq': ('sync', True), 'vq': ('scalar', True)}


def _norm(k):
    if isinstance(k, tuple):
        return k[0], k[1:]
    return k, None


class Prog:
    def __init__(self, nc):
        self.nc = nc
        self.ops = []

    def add(self, st, fn, r=(), w=()):
        self.ops.append((st, fn, tuple(r), tuple(w)))

    def emit(self):
        nc = self.nc
        ops = self.ops
        cnt = {}
        idx = []
        for (st, fn, r, w) in ops:
            idx.append(cnt.get(st, 0))
            cnt[st] = cnt.get(st, 0) + 1
        writers = {}
        readers = {}
        deps = []

        def conf(a, b):
            return a is None or b is None or a == b
        for i, (st, fn, r, w) in enumerate(ops):
            d = set()
            for k in r:
                name, sub = _norm(k)
                for (s2, o) in writers.get(name, ()):
                    if conf(sub, s2):
                        d.add(o)
            for k in w:
                name, sub = _norm(k)
                for (s2, o) in writers.get(name, ()):
                    if conf(sub, s2):
                        d.add(o)
                for (s2, o) in readers.get(name, ()):
                    if conf(sub, s2):
                        d.add(o)
            for k in w:
                name, sub = _norm(k)
                writers[name] = [(s2, o) for (s2, o) in writers.get(name, []) if not (sub is None or s2 == sub)]
                writers[name].append((sub, i))
                readers[name] = [(s2, o) for (s2, o) in readers.get(name, []) if not conf(sub, s2)]
            for k in r:
                name, sub = _norm(k)
                readers.setdefault(name, []).append((sub, i))
            d.discard(i)
            need = {}
            needd = set()
            for o in d:
                s2 = ops[o][0]
                if STREAMS[s2][1]:
                    needd.add((s2, idx[o]))
                elif need.get(s2, -1) < idx[o]:
                    need[s2] = idx[o]
            deps.append((need, needd))

        with contextlib.ExitStack() as st_:
            sems = {}
            for s in cnt:
                if STREAMS[s][1]:
                    sems[s] = [st_.enter_context(nc.semaphore("d_%s_%d" % (s, i))) for i in range(NDS)]
                else:
                    sems[s] = [st_.enter_context(nc.semaphore("s_%s_%d" % (s, i)))
                               for i in range(cnt[s] // CH + 1)]
            block = st_.enter_context(nc.Block())

            def section(ename, eng):
                waited = {}
                waitedd = {}

                def wait_c(s2, j):
                    if waited.get(s2, -1) >= j:
                        return
                    eng.wait_ge(sems[s2][j // CH], (j % CH) + 1)
                    waited[s2] = j

                def wait_d(s2, j):
                    key = (s2, j % NDS)
                    if waitedd.get(key, -1) >= j:
                        return
                    eng.wait_ge(sems[s2][j % NDS], (j // NDS + 1) * 16)
                    waitedd[key] = j
                for i, (st, fn, r, w) in enumerate(ops):
                    if STREAMS[st][0] != ename:
                        continue
                    need, needd = deps[i]
                    for s2, j in need.items():
                        if st == 'pe' and s2 == 'pe':
                            continue
                        wait_c(s2, j)
                    for (s2, j) in sorted(needd):
                        wait_d(s2, j)
                    j = idx[i]
                    if STREAMS[st][1]:
                        if j >= NDS:
                            wait_d(st, j - NDS)
                        inst = fn(eng)
                        inst.then_inc(sems[st][j % NDS], 16)
                    else:
                        inst = fn(eng)
                        inst.then_inc(sems[st][j // CH], 1)
                if ename == 'sync':
                    for s2 in cnt:
                        if STREAMS[s2][1]:
                            for j in range(max(0, cnt[s2] - NDS), cnt[s2]):
                                wait_d(s2, j)
                        else:
                            wait_c(s2, cnt[s2] - 1)

            @block.tensor
            def _(eng):
                section('tensor', eng)

            @block.vector
            def _(eng):
                section('vector', eng)

            @block.scalar
            def _(eng):
                section('scalar', eng)

            @block.gpsimd
            def _(eng):
                section('gpsimd', eng)

            @block.sync
            def _(eng):
                section('sync', eng)


def _helpers(P):
    def mk(st):
        def f(fn, r=(), w=()):
            P.add(st, fn, r, w)
        return f
    return mk('dve'), mk('act'), mk('pe'), mk('gq'), mk('sq'), mk('pool'), mk('vq')


def build_a():
    nc = bass.Bass("TRN2", target_bir_lowering=False)
    dt = nc.dram_tensor
    xT = dt("xT", [D, NTOK], F32, kind="ExternalInput").ap()
    wq = dt("wq", [D, 64], F32, kind="ExternalInput").ap()
    wk = dt("wk", [D, 65], F32, kind="ExternalInput").ap()
    wvu = dt("wvu", [D, 128], F32, kind="ExternalInput").ap()
    g1 = dt("g1", [128, 8], F32, kind="ExternalInput").ap()
    nbf = dt("nbf", [65, 2], F32, kind="ExternalInput").ap()
    gqk = dt("gqk", [64, 2], F32, kind="ExternalInput").ap()
    are = dt("are", [128, 2], F32, kind="ExternalInput").ap()
    aim = dt("aim", [128, 2], F32, kind="ExternalInput").ap()
    ldt = dt("ldt", [128, 2], F32, kind="ExternalInput").ap()
    brp = dt("brp", [64, 2, 128], F32, kind="ExternalInput").ap()
    bip = dt("bip", [64, 2, 128], F32, kind="ExternalInput").ap()
    crp = dt("crp", [128, 2, 128], F32, kind="ExternalInput").ap()
    cip = dt("cip", [128, 2, 128], F32, kind="ExternalInput").ap()
    dsk = dt("dsk", [64, 2], F32, kind="ExternalInput").ap()
    onesb = dt("onesb", [128, 128], F32, kind="ExternalInput").ap()
    identb = dt("identb", [128, 128], F32, kind="ExternalInput").ap()
    iota1 = dt("iota1", [128, 512], F32, kind="ExternalInput").ap()
    maskd = dt("maskd", [128, 128], F32, kind="ExternalInput").ap()
    yaT = dt("yaT", [64, NTOK], BF16, kind="ExternalOutput").ap()
    ysT = dt("ysT", [64, NTOK], BF16, kind="ExternalOutput").ap()

    P = Prog(nc)
    V, A, T, G, SY, PL, VQ = _helpers(P)
    with contextlib.ExitStack() as st:
        def sb(name, shape, dtype):
            return st.enter_context(nc.sbuf_tensor(name, shape, dtype))

        def pp(name, shape, dtype=F32):
            return st.enter_context(nc.psum_tensor(name, shape, dtype))
        WQ = sb("WQ", [128, 8, 64], BF16)
        WK = sb("WK", [128, 8, 65], BF16)
        WVU = sb("WVU", [128, 8, 128], BF16)
        G1 = sb("G1", [128, 8], F32)
        NBF = sb("NBF", [65, 2], F32)
        GQK = sb("GQK", [64, 2], F32)
        ARE = sb("ARE", [128, 2], F32)
        AIM = sb("AIM", [128, 2], F32)
        LDT = sb("LDT", [128, 2], F32)
        BRP = sb("BRP", [128, 2, 128], BF16)
        BIP = sb("BIP", [128, 2, 128], BF16)
        CRP = sb("CRP", [128, 2, 128], BF16)
        CIP = sb("CIP", [128, 2, 128], BF16)
        CRN = sb("CRN", [128, 2, 128], BF16)
        CIN = sb("CIN", [128, 2, 128], BF16)
        DSK = sb("DSK", [128, 2], F32)
        ONB = sb("ONB", [128, 128], BF16)
        ONF = sb("ONF", [128, 128], F32)
        IDB = sb("IDB", [128, 128], BF16)
        IOT = sb("IOT", [128, 512], F32)
        MSK = sb("MSK", [128, 128], BF16)
        sm = {n: sb("sm_" + n, [128, 2], F32) for n in
              ["dt", "th", "rho", "k", "r", "ar", "sn", "cs", "lr", "li", "nr", "den", "t1", "t2", "cr", "ci",
               "cL", "sL", "CR", "CI", "ta", "tb"]}
        TR = sb("TR", [128, 2, 512], F32)
        TI = sb("TI", [128, 2, 512], F32)
        TC = sb("TC", [128, 2, 512], F32)
        TS = sb("TS", [128, 2, 512], F32)
        RB = sb("RB", [128, 2, 512], F32)
        QA = sb("QA", [128, S], BF16)
        KA = sb("KA", [128, S], BF16)
        VTM = sb("VTM", [128, 64, 128], BF16)
        UT = sb("UT", [128, S], BF16)
        FR = [sb("FR%d" % i, [65, 512], F32) for i in range(2)]
        AR = sb("AR", [65, 512], F32)
        AH = sb("AH", [65, 512], BF16)
        AL = sb("AL", [65, 512], BF16)
        FCOL = sb("FCOL", [128, 64], F32)
        FQ0 = sb("FQ0", [128, 16], F32)
        CB = sb("CB", [128, 16, 64], F32)
        YA = [sb("YA%d" % i, [64, 512], BF16) for i in range(2)]
        YS = [sb("YS%d" % i, [128, 512], BF16) for i in range(2)]
        XSQ = sb("XSQ", [128, 8, 512], BF16)
        XBF = [sb("XBF%d" % i, [128, 8, 512], BF16) for i in range(2)]
        RSTD = [sb("RSTD%d" % i, [128, 512], F32) for i in range(2)]
        RS = RSTD
        XST = [sb("XST%d" % i, [128, 512], F32) for i in range(4)]
        QF = [[sb("QF%d_%d" % (i, j), [64, 512], F32) for j in range(2)] for i in range(2)]
        SQ1 = [sb("SQ1%d" % i, [64, 512], BF16) for i in range(2)]
        RQ2 = [sb("RQ2%d" % i, [64, 512], F32) for i in range(2)]
        RQ = RQ2
        VB = [sb("VB%d" % i, [64, 512], BF16) for i in range(2)]
        FF = [sb("FF%d" % i, [65, 512], F32) for i in range(2)]
        FE = sb("FE", [65, 512], F32)
        ONR = sb("ONR", [65, 512], F32)
        PT = [sb("PT%d" % i, [128, 512], BF16) for i in range(2)]
        OS = [sb("OS%d" % i, [65, 512], F32) for i in range(2)]
        f1 = sb("f1", [128, 512], F32)
        f2 = sb("f2", [128, 512], F32)
        f3 = sb("f3", [128, 512], F32)
        f4 = sb("f4", [128, 512], F32)
        PH, PK, PA2 = f1, f2, f3
        WRl = [sb("WRr%d" % i, [128, 512], F32) for i in range(2)]
        WIl = [sb("WIi%d" % i, [128, 512], F32) for i in range(2)]
        bb = [[sb("b%d_%d" % (i, j), [128, 512], BF16) for i in range(4)] for j in range(2)]
        B = [pp("B%d" % i, [128, 512]) for i in range(7)]
        PTB = pp("PTB", [128, 4, 64], BF16)

        def ldw(dst, src, key):
            G(lambda e: e.dma_start(out=dst, in_=src), w=[key])
        ldw(WQ[:], wq.rearrange("(k p) n -> p k n", p=128), "WQ")
        ldw(WK[:], wk.rearrange("(k p) n -> p k n", p=128), "WK")
        ldw(WVU[:], wvu.rearrange("(k p) n -> p k n", p=128), "WVU")
        for (dst, src, key) in [(G1[:], g1, "G1"), (NBF[:], nbf, "NBF"), (GQK[:], gqk, "GQK"), (ARE[:], are, "ARE"),
                                (AIM[:], aim, "AIM"), (LDT[:], ldt, "LDT"), (BRP[64:128], brp, "BRP"),
                                (BIP[64:128], bip, "BIP"), (CRP[:], crp, "CRP"), (CIP[:], cip, "CIP"),
                                (DSK[64:128], dsk, "DSK"), (ONB[:], onesb, "ONB"), (ONF[:], onesb, "ONF"),
                                (IDB[:], identb, "IDB"), (IOT[:], iota1, "IOT"), (MSK[:], maskd, "MSK")]:
            ldw(dst, src, key)

        def xload(gb):
            T0 = gb * 512
            p = gb % 2
            for k in range(8):
                si = (gb * 8 + k) % 4
                if k < 4:
                    G(lambda e, k=k, T0=T0, p=p: e.dma_start(out=XBF[p][:, k, :],
                                                              in_=xT[k * 128:(k + 1) * 128, T0:T0 + 512]),
                      w=[("XBF", p, k)])
                else:
                    si = k - 4
                    SY(lambda e, k=k, T0=T0, si=si: e.dma_start(out=XST[si][:],
                                                                in_=xT[k * 128:(k + 1) * 128, T0:T0 + 512]),
                       w=[("XST", si)])
                    A(lambda e, k=k, p=p, si=si: e.activation(out=XBF[p][:, k, :], in_=XST[si][:], func=AF.Copy),
                      r=[("XST", si)], w=[("XBF", p, k)])
        xload(0)
        for (Wt, key) in ((WQ, "WQ"), (WK, "WK"), (WVU, "WVU")):
            for k in range(8):
                V(lambda e, Wt=Wt, k=k: e.tensor_scalar(out=Wt[:, k, :], in0=Wt[:, k, :], scalar1=G1[:, k:k + 1],
                                                        scalar2=None, op0=OP.mult), r=["G1", key], w=[key])
        V(lambda e: e.tensor_scalar(out=GQK[:, 0:1], in0=GQK[:, 0:1], scalar1=0.125, scalar2=None, op0=OP.mult),
          r=["GQK"], w=["GQK"])
        V(lambda e: e.tensor_scalar(out=NBF[:], in0=NBF[:], scalar1=-1.0, scalar2=None, op0=OP.mult), r=["NBF"],
          w=["NBF"])
        V(lambda e: e.memset(ONR[:], 1.0), w=["ONR"])
        V(lambda e: e.memset(QA[:], 0.0), w=["QA"])
        V(lambda e: e.memset(KA[:], 0.0), w=["KA"])
        V(lambda e: e.memset(KA[64:66, :], 1.0), r=["KA"], w=["KA"])
        V(lambda e: e.memset(VTM[:], 1.0), w=["VTM"])
        V(lambda e: e.tensor_scalar(out=CRN[:], in0=CRP[:], scalar1=-1.0, scalar2=None, op0=OP.mult), r=["CRP"],
          w=["CRN"])
        V(lambda e: e.tensor_scalar(out=CIN[:], in0=CIP[:], scalar1=-1.0, scalar2=None, op0=OP.mult), r=["CIP"],
          w=["CIN"])

        def VS(fn):
            V(fn, r=["S5T", "ARE", "AIM", "LDT", "IOT"], w=["S5T"])

        def AS(fn):
            A(fn, r=["S5T", "LDT"], w=["S5T"])

        def rred(out, in_, k_t):
            VS(lambda e: e.tensor_scalar(out=k_t, in0=in_, scalar1=INV2PI, scalar2=MAGIC, op0=OP.mult, op1=OP.add))
            VS(lambda e: e.tensor_scalar(out=k_t, in0=k_t, scalar1=MAGIC, scalar2=None, op0=OP.subtract))
            VS(lambda e: e.scalar_tensor_tensor(out=out, in0=k_t, scalar=-C1, in1=in_, op0=OP.mult, op1=OP.add))
            VS(lambda e: e.scalar_tensor_tensor(out=out, in0=k_t, scalar=-C2, in1=out, op0=OP.mult, op1=OP.add))
            VS(lambda e: e.tensor_scalar(out=out, in0=out, scalar1=PI, scalar2=-PI, op0=OP.min, op1=OP.max))

        def sincos(sn, cs, r, tmp):
            AS(lambda e: e.activation(out=sn, in_=r, func=AF.Sin))
            VS(lambda e: e.tensor_scalar(out=tmp, in0=r, scalar1=-1.0, scalar2=None, op0=OP.mult))
            VS(lambda e: e.tensor_tensor(out=tmp, in0=tmp, in1=r, op=OP.max))
            VS(lambda e: e.tensor_scalar(out=tmp, in0=tmp, scalar1=-1.0, scalar2=PI / 2, op0=OP.mult, op1=OP.add))
            AS(lambda e: e.activation(out=cs, in_=tmp, func=AF.Sin))
        s_ = {k: v[:] for k, v in sm.items()}
        AS(lambda e: e.activation(out=s_["dt"], in_=LDT[:], func=AF.Exp))
        VS(lambda e: e.tensor_tensor(out=s_["th"], in0=AIM[:], in1=s_["dt"], op=OP.mult))
        VS(lambda e: e.tensor_tensor(out=s_["t1"], in0=ARE[:], in1=s_["dt"], op=OP.mult))
        AS(lambda e: e.activation(out=s_["rho"], in_=s_["t1"], func=AF.Exp))
        rred(s_["r"], s_["th"], s_["k"])
        sincos(s_["sn"], s_["cs"], s_["r"], s_["ar"])
        VS(lambda e: e.tensor_tensor(out=s_["lr"], in0=s_["rho"], in1=s_["cs"], op=OP.mult))
        VS(lambda e: e.tensor_tensor(out=s_["li"], in0=s_["rho"], in1=s_["sn"], op=OP.mult))
        VS(lambda e: e.tensor_scalar(out=s_["nr"], in0=s_["lr"], scalar1=-1.0, scalar2=None, op0=OP.add))
        VS(lambda e: e.tensor_tensor(out=s_["t1"], in0=ARE[:], in1=ARE[:], op=OP.mult))
        VS(lambda e: e.tensor_tensor(out=s_["t2"], in0=AIM[:], in1=AIM[:], op=OP.mult))
        VS(lambda e: e.tensor_tensor(out=s_["den"], in0=s_["t1"], in1=s_["t2"], op=OP.add))
        VS(lambda e: e.reciprocal(out=s_["den"], in_=s_["den"]))
        VS(lambda e: e.tensor_tensor(out=s_["t1"], in0=s_["nr"], in1=ARE[:], op=OP.mult))
        VS(lambda e: e.tensor_tensor(out=s_["t2"], in0=s_["li"], in1=AIM[:], op=OP.mult))
        VS(lambda e: e.tensor_tensor(out=s_["t1"], in0=s_["t1"], in1=s_["t2"], op=OP.add))
        VS(lambda e: e.tensor_tensor(out=s_["cr"], in0=s_["t1"], in1=s_["den"], op=OP.mult))
        VS(lambda e: e.tensor_tensor(out=s_["t1"], in0=s_["li"], in1=ARE[:], op=OP.mult))
        VS(lambda e: e.tensor_tensor(out=s_["t2"], in0=s_["nr"], in1=AIM[:], op=OP.mult))
        VS(lambda e: e.tensor_tensor(out=s_["t1"], in0=s_["t1"], in1=s_["t2"], op=OP.subtract))
        VS(lambda e: e.tensor_tensor(out=s_["ci"], in0=s_["t1"], in1=s_["den"], op=OP.mult))
        for gh in range(2):
            VS(lambda e, gh=gh: e.tensor_scalar(out=PH[:], in0=IOT[:], scalar1=sm["th"][:, gh:gh + 1], scalar2=None,
                                                op0=OP.mult))
            rred(PA2[:], PH[:], PK[:])
            sincos(TS[:, gh, :], TC[:, gh, :], PA2[:], PK[:])
            VS(lambda e, gh=gh: e.tensor_scalar(out=PH[:], in0=TS[:, gh, :], scalar1=sm["ci"][:, gh:gh + 1],
                                                scalar2=None, op0=OP.mult))
            VS(lambda e, gh=gh: e.scalar_tensor_tensor(out=TR[:, gh, :], in0=TC[:, gh, :],
                                                       scalar=sm["cr"][:, gh:gh + 1], in1=PH[:], op0=OP.mult,
                                                       op1=OP.add))
            VS(lambda e, gh=gh: e.tensor_scalar(out=PH[:], in0=TS[:, gh, :], scalar1=sm["cr"][:, gh:gh + 1],
                                                scalar2=None, op0=OP.mult))
            VS(lambda e, gh=gh: e.scalar_tensor_tensor(out=TI[:, gh, :], in0=TC[:, gh, :],
                                                       scalar=sm["ci"][:, gh:gh + 1], in1=PH[:], op0=OP.mult,
                                                       op1=OP.subtract))
            VS(lambda e, gh=gh: e.tensor_scalar(out=RB[:, gh, :], in0=IOT[:], scalar1=0.0,
                                                scalar2=sm["rho"][:, gh:gh + 1], op0=OP.mult, op1=OP.add))
            VS(lambda e, gh=gh: e.tensor_copy(out=sm["cL"][:, gh:gh + 1], in_=TC[:, gh, 511:512]))
            VS(lambda e, gh=gh: e.tensor_copy(out=sm["sL"][:, gh:gh + 1], in_=TS[:, gh, 511:512]))
        S5K = ["S5T"]
        V(lambda e: e.memset(f4[0:1, 0:2], 0.0), r=["S5T"], w=["f1", "f2", "f3", "f4"])

        def inproj_s1(s, b):
            gb = s * 16 + b
            p = gb % 2
            t0 = b * 512
            sl = slice(t0, t0 + 512)
            xk = ("XBF", p)
            if gb + 1 < 32:
                xload(gb + 1)
            for k in range(8):
                V(lambda e, k=k, p=p: e.tensor_tensor(out=XSQ[:, k, :], in0=XBF[p][:, k, :], in1=XBF[p][:, k, :],
                                                      op=OP.mult), r=[("XBF", p, k)], w=[("XSQ", k)])
            for k in range(8):
                T(lambda e, k=k, p=p: e.matmul(B[p][:], lhsT=ONB[:], rhs=XSQ[:, k, :], start=(k == 0), stop=(k == 7)),
                  r=[("XSQ", k), "ONB"], w=[("B", p)])
            A(lambda e, p=p: e.activation(out=RS[p][:], in_=B[p][:], func=AF.Ln, bias=EPS, scale=1.0 / D),
              r=[("B", p)], w=[("RSTD", p)])
            A(lambda e, p=p: e.activation(out=RSTD[p][:], in_=RS[p][:], func=AF.Exp, scale=-0.5), r=[("RSTD", p)],
              w=[("RSTD", p)])
            rk = ("RSTD", p)
            for qi, (Wt, wkey, M) in enumerate(((WQ, "WQ", 64), (WK, "WK", 65))):
                pb = 2 + qi
                for k in range(8):
                    T(lambda e, k=k, Wt=Wt, pb=pb, p=p, M=M: e.matmul(B[pb][0:M, :], lhsT=Wt[:, k, :],
                                                                       rhs=XBF[p][:, k, :], start=(k == 0),
                                                                       stop=(k == 7)),
                      r=[wkey, ("XBF", p, k)], w=[("B", pb)])
                V(lambda e, pb=pb, qi=qi, p=p: e.tensor_tensor(out=QF[qi][p][:], in0=B[pb][0:64, :],
                                                               in1=RSTD[p][0:64, :], op=OP.mult),
                  r=[("B", pb), rk], w=[("QF", qi, p)])
                if qi == 1:
                    V(lambda e, p=p: e.tensor_tensor(out=FF[p][64:65, :], in0=B[3][64:65, :],
                                                     in1=RSTD[p][64:65, :], op=OP.mult), r=[("B", 3), rk],
                      w=[("FF", p)])
            for k in range(8):
                T(lambda e, k=k, p=p: e.matmul(B[6][:], lhsT=WVU[:, k, :], rhs=XBF[p][:, k, :], start=(k == 0),
                                               stop=(k == 7)), r=["WVU", ("XBF", p, k)], w=[("B", 6)])
            V(lambda e, p=p: e.tensor_tensor(out=VB[p][:], in0=B[6][0:64, :], in1=RSTD[p][0:64, :], op=OP.mult),
              r=[("B", 6), rk], w=[("VB", p)])
            V(lambda e, sl=sl, p=p: e.tensor_tensor(out=UT[64:128, sl], in0=B[6][64:128, :], in1=RSTD[p][64:128, :],
                                                    op=OP.mult), r=[("B", 6), rk], w=[("UT", b)])

        def inproj_s2(s, b):
            gb = s * 16 + b
            p = gb % 2
            t0 = b * 512
            sl = slice(t0, t0 + 512)
            for qi, (dst, dkey, gcol) in enumerate(((QA, "QA", 0), (KA, "KA", 1))):
                A(lambda e, qi=qi, p=p: e.activation(out=SQ1[qi][:], in_=QF[qi][p][:], func=AF.Square),
                  r=[("QF", qi, p)], w=[("SQ1", qi)])
                T(lambda e, qi=qi: e.matmul(B[4 + qi][0:64, :], lhsT=ONB[0:64, 0:64], rhs=SQ1[qi][:], start=True,
                                            stop=True), r=[("SQ1", qi), "ONB"], w=[("B", 4 + qi)])
                A(lambda e, qi=qi: e.activation(out=RQ[qi][:], in_=B[4 + qi][0:64, :], func=AF.Ln, bias=EPS,
                                                scale=1.0 / 64), r=[("B", 4 + qi)], w=[("RQ2", qi)])
                A(lambda e, qi=qi: e.activation(out=RQ2[qi][:], in_=RQ[qi][:], func=AF.Exp, scale=-0.5),
                  r=[("RQ2", qi)], w=[("RQ2", qi)])
                V(lambda e, dst=dst, gcol=gcol, sl=sl, qi=qi, p=p: e.scalar_tensor_tensor(
                    out=dst[0:64, sl], in0=QF[qi][p][:], scalar=GQK[:, gcol:gcol + 1], in1=RQ2[qi][:], op0=OP.mult,
                    op1=OP.mult), r=[("QF", qi, p), ("RQ2", qi), "GQK"], w=[(dkey, b)])
            for tt in range(4):
                T(lambda e, tt=tt, p=p: e.transpose(PTB[:, tt, :], VB[p][:, tt * 128:(tt + 1) * 128],
                                                    IDB[0:64, 0:64]), r=[("VB", p), "IDB"], w=["PTB"])
            for tt in range(4):
                V(lambda e, tt=tt, b=b: e.tensor_copy(out=VTM[:, b * 4 + tt, 0:64], in_=PTB[:, tt, :]),
                  r=["PTB"], w=[("VTM", b * 4 + tt)])
            A(lambda e, p=p: e.activation(out=FE[64:65, :], in_=FF[p][64:65, :], func=AF.Exp, bias=NBF[64:65, 0:1],
                                          scale=-1.0), r=[("FF", p), "NBF"], w=["FE"])
            A(lambda e: e.activation(out=FE[64:65, :], in_=FE[64:65, :], func=AF.Ln, bias=1.0, scale=1.0),
              r=["FE"], w=["FE"])
            V(lambda e: e.tensor_scalar(out=FE[64:65, :], in0=FE[64:65, :], scalar1=-1.0, scalar2=None,
                                        op0=OP.mult), r=["FE"], w=["FE"])
            fp = b % 2
            if b == 0:
                V(lambda e, fp=fp: e.tensor_tensor_scan(out=FR[fp][64:65, :], data0=ONR[64:65, :],
                                                        data1=FE[64:65, :], initial=0.0, op0=OP.mult, op1=OP.add),
                  r=["FE", "ONR"], w=[("FR", fp)])
            else:
                V(lambda e, fp=fp: e.tensor_tensor_scan(out=FR[fp][64:65, :], data0=ONR[64:65, :],
                                                        data1=FE[64:65, :], initial=FR[1 - fp][64:65, 511:512],
                                                        op0=OP.mult, op1=OP.add),
                  r=["FE", "ONR", ("FR", 1 - fp)], w=[("FR", fp)])
            frk = ("FR", fp)
            V(lambda e, fp=fp: e.tensor_scalar(out=AR[64:65, :], in0=FR[fp][64:65, :], scalar1=FR[fp][64:65, 0:1],
                                               scalar2=None, op0=OP.subtract), r=[frk], w=["AR"])
            V(lambda e: e.tensor_copy(out=AH[64:65, :], in_=AR[64:65, :]), r=["AR"], w=["AH"])
            V(lambda e: e.tensor_tensor(out=AL[64:65, :], in0=AR[64:65, :], in1=AH[64:65, :], op=OP.subtract),
              r=["AR", "AH"], w=["AL"])
            VQ(lambda e, sl=sl: e.dma_start(out=QA[64:65, sl], in_=AH[64:65, :]), r=["AH"], w=[("QA", b)])
            VQ(lambda e, sl=sl: e.dma_start(out=QA[65:66, sl], in_=AL[64:65, :]), r=["AL"], w=[("QA", b)])
            for j in range(4):
                T(lambda e, j=j, fp=fp: e.matmul(B[5][:, 8 + j:9 + j], lhsT=FR[fp][64:65, j * 128:(j + 1) * 128],
                                                 rhs=ONF[64:65, 0:1], start=True, stop=True), r=[frk, "ONF"],
                  w=[("B", 5)])
            T(lambda e, fp=fp: e.matmul(B[5][:, 16:17], lhsT=ONF[64:65, :], rhs=FR[fp][64:65, 0:1], start=True,
                                        stop=True), r=[frk, "ONF"], w=[("B", 5)])
            V(lambda e, b=b: e.tensor_copy(out=FCOL[:, 4 * b:4 * b + 4], in_=B[5][:, 8:12]), r=[("B", 5)],
              w=["FCOL"])
            V(lambda e, b=b: e.tensor_copy(out=FQ0[:, b:b + 1], in_=B[5][:, 16:17]), r=[("B", 5)], w=["FQ0"])
            V(lambda e, b=b: e.tensor_scalar(out=CB[:, b, 0:4 * b + 4], in0=FCOL[:, 0:4 * b + 4], scalar1=-1.0,
                                             scalar2=FQ0[:, b:b + 1], op0=OP.mult, op1=OP.add),
              r=["FCOL", "FQ0"], w=[("CB", b)])

        def capture(fn, *args):
            saved = P.ops
            P.ops = []
            fn(*args)
            out = P.ops
            P.ops = saved
            return out

        def merge(la, lb):
            na, nb = len(la), len(lb)
            ia = ib = 0
            while ia < na or ib < nb:
                if ib >= nb or (ia < na and ia * nb <= ib * na):
                    P.ops.append(la[ia])
                    ia += 1
                else:
                    P.ops.append(lb[ib])
                    ib += 1
        def att_tiles():
            lst = []
            for qb in range(16):
                nk = 4 * qb + 4
                for j in range(nk):
                    lst.append((qb, j, nk))
            return lst

        def att_qk(s, i, qb, j, nk):
            sp = i % 2
            t0 = qb * 512
            dj = j - 4 * qb
            c0 = dj * 128 if dj > 0 else 0
            diag = dj >= 0
            T(lambda e: e.matmul(B[sp][:, c0:512], lhsT=KA[:, j * 128:(j + 1) * 128],
                                 rhs=QA[:, t0 + c0:t0 + 512], start=True, stop=(not diag)),
              r=[("KA", j // 4), ("QA", qb)], w=[("B", sp)])
            if diag:
                T(lambda e: e.matmul(B[sp][:, c0:c0 + 128], lhsT=IDB[:], rhs=MSK[:], start=False, stop=True),
                  r=["IDB", "MSK"], w=[("B", sp)])
            A(lambda e: e.activation(out=PT[sp][:, c0:512], in_=B[sp][:, c0:512], func=AF.Exp,
                                     bias=CB[:, qb, j:j + 1], scale=1.0), r=[("B", sp), ("CB", qb)], w=[("PT", sp)])

        def att_pv(s, i, qb, j, nk):
            sp = i % 2
            op_ = qb % 2
            t0 = qb * 512
            dj = j - 4 * qb
            c0 = dj * 128 if dj > 0 else 0
            T(lambda e: e.matmul(B[2 + op_][:, c0:512], lhsT=VTM[:, j, :], rhs=PT[sp][:, c0:512],
                                 start=(j == 0), stop=(j == nk - 1)), r=[("VTM", j), ("PT", sp)], w=[("B", 2 + op_)])
            if j == nk - 1:
                V(lambda e: e.tensor_copy(out=OS[op_][:], in_=B[2 + op_][0:65, :]), r=[("B", 2 + op_)],
                  w=[("OS", op_)])
                V(lambda e: e.reciprocal(out=OS[op_][64:65, :], in_=OS[op_][64:65, :]), r=[("OS", op_)],
                  w=[("OS", op_)])
                T(lambda e: e.matmul(B[4][0:64, :], lhsT=ONF[64:65, 0:64], rhs=OS[op_][64:65, :], start=True,
                                     stop=True), r=[("OS", op_), "ONF"], w=[("B", 4)])
                V(lambda e: e.tensor_tensor(out=YA[op_][:], in0=OS[op_][0:64, :], in1=B[4][0:64, :], op=OP.mult),
                  r=[("OS", op_), ("B", 4)], w=[("YA", op_)])
                SY(lambda e: e.dma_start(out=yaT[:, s * S + t0:s * S + t0 + 512], in_=YA[op_][:]), r=[("YA", op_)],
                   w=["yaT"])

        def s5_front(s, b, gh):
            u = b * 2 + gh
            bp = u % 2
            WR_, WI_ = WRl[bp], WIl[bp]
            t0 = b * 512
            sl = slice(t0, t0 + 512)
            if b == 0 and gh == 0:
                V(lambda e: e.memset(sm["CR"][:], 0.0), w=["CRI"])
                V(lambda e: e.memset(sm["CI"][:], 0.0), w=["CRI"])
            T(lambda e: e.matmul(B[5][:], lhsT=BRP[64:128, gh, :], rhs=UT[64:128, sl], start=True, stop=True),
              r=["BRP", ("UT", b)], w=[("B", 5)])
            T(lambda e: e.matmul(B[6][:], lhsT=BIP[64:128, gh, :], rhs=UT[64:128, sl], start=True, stop=True),
              r=["BIP", ("UT", b)], w=[("B", 6)])
            V(lambda e: e.tensor_tensor(out=f1[:], in0=B[5][:], in1=TR[:, gh, :], op=OP.mult), r=[("B", 5)] + S5K,
              w=["f1"])
            V(lambda e: e.tensor_tensor(out=f2[:], in0=B[6][:], in1=TI[:, gh, :], op=OP.mult), r=[("B", 6)] + S5K,
              w=["f2"])
            V(lambda e: e.tensor_tensor(out=f3[:], in0=B[5][:], in1=TI[:, gh, :], op=OP.mult), r=[("B", 5)] + S5K,
              w=["f3"])
            V(lambda e: e.tensor_tensor(out=f4[:], in0=B[6][:], in1=TR[:, gh, :], op=OP.mult), r=[("B", 6)] + S5K,
              w=["f4"])
            V(lambda e: e.tensor_tensor(out=f1[:], in0=f1[:], in1=f2[:], op=OP.subtract), r=["f1", "f2"], w=["f1"])
            V(lambda e: e.tensor_tensor(out=f3[:], in0=f3[:], in1=f4[:], op=OP.add), r=["f3", "f4"], w=["f3"])
            V(lambda e: e.tensor_tensor_scan(out=WR_[:], data0=RB[:, gh, :], data1=f1[:],
                                             initial=sm["CR"][:, gh:gh + 1], op0=OP.mult, op1=OP.add),
              r=["f1", "CRI"] + S5K, w=[("WR", bp)])
            V(lambda e: e.tensor_tensor_scan(out=WI_[:], data0=RB[:, gh, :], data1=f3[:],
                                             initial=sm["CI"][:, gh:gh + 1], op0=OP.mult, op1=OP.add),
              r=["f3", "CRI"] + S5K, w=[("WI", bp)])
            cr_, ci_ = sm["CR"][:, gh:gh + 1], sm["CI"][:, gh:gh + 1]
            cl_, sl_ = sm["cL"][:, gh:gh + 1], sm["sL"][:, gh:gh + 1]
            ta_, tb_ = sm["ta"][:, gh:gh + 1], sm["tb"][:, gh:gh + 1]
            kk = dict(r=[("WR", bp), ("WI", bp), "CRI"] + S5K, w=["CRI"])
            V(lambda e: e.tensor_tensor(out=ta_, in0=WR_[:, 511:512], in1=cl_, op=OP.mult), **kk)
            V(lambda e: e.tensor_tensor(out=tb_, in0=WI_[:, 511:512], in1=sl_, op=OP.mult), **kk)
            V(lambda e: e.tensor_tensor(out=cr_, in0=ta_, in1=tb_, op=OP.subtract), **kk)
            V(lambda e: e.tensor_tensor(out=ta_, in0=WR_[:, 511:512], in1=sl_, op=OP.mult), **kk)
            V(lambda e: e.tensor_tensor(out=tb_, in0=WI_[:, 511:512], in1=cl_, op=OP.mult), **kk)
            V(lambda e: e.tensor_tensor(out=ci_, in0=ta_, in1=tb_, op=OP.add), **kk)
            bt = bb[bp]
            for i, (src, key, tab) in enumerate(((WR_, "WR", TC), (WI_, "WI", TS), (WR_, "WR", TS), (WI_, "WI", TC))):
                PL(lambda e, i=i, src=src, tab=tab: e.tensor_tensor(out=bt[i][:], in0=src[:], in1=tab[:, gh, :],
                                                                    op=OP.mult), r=[(key, bp)] + S5K,
                   w=[("bb", bp, i)])

        def s5_cproj(s, b, gh):
            u = b * 2 + gh
            bp = u % 2
            bt = bb[bp]
            for i, (Wc, wkey) in enumerate(((CRP, "CRP"), (CRN, "CRN"), (CIN, "CIN"), (CIN, "CIN"))):
                T(lambda e, i=i, Wc=Wc: e.matmul(B[4][64:128, :], lhsT=Wc[:, gh, 64:128], rhs=bt[i][:],
                                                 start=(gh == 0 and i == 0), stop=(gh == 1 and i == 3)),
                  r=[wkey, ("bb", bp, i)], w=[("B", 4)])

        def s5_out(s, b):
            t0 = b * 512
            sl = slice(t0, t0 + 512)
            yp = b % 2
            V(lambda e: e.scalar_tensor_tensor(out=YS[yp][64:128, :], in0=UT[64:128, sl], scalar=DSK[64:128, 0:1],
                                               in1=B[4][64:128, :], op0=OP.mult, op1=OP.add),
              r=[("UT", b), "DSK", ("B", 4)], w=[("YS", yp)])
            SY(lambda e: e.dma_start(out=ysT[:, s * S + t0:s * S + t0 + 512], in_=YS[yp][64:128, :]),
               r=[("YS", yp)], w=["ysT"])

        for s in range(2):
            P.ops.extend(capture(inproj_s1, s, 0))
            for b in range(16):
                la = capture(inproj_s2, s, b)
                lb = capture(inproj_s1, s, b + 1) if b + 1 < 16 else []
                merge(la, lb)
            tiles = att_tiles()
            units = [(b, gh) for b in range(16) for gh in range(2)]
            nt = len(tiles)
            nu = len(units)
            per = (nt + nu - 1) // nu
            ti = 0
            for k in range(nu + 3):
                if 0 <= k - 2 < nu and units[k - 2][1] == 1:
                    s5_out(s, units[k - 2][0])
                if k < nu:
                    s5_front(s, *units[k])
                hi = min(nt, (k + 1) * per) if k < nu - 1 else nt
                while ti < hi:
                    att_qk(s, ti, *tiles[ti])
                    if ti >= 1:
                        att_pv(s, ti - 1, *tiles[ti - 1])
                    ti += 1
                if ti == nt and k == nu - 1:
                    att_pv(s, nt - 1, *tiles[nt - 1])
                if 0 <= k - 1 < nu:
                    s5_cproj(s, *units[k - 1])
        P.emit()
    return nc


def build_b():
    nc = bass.Bass("TRN2", target_bir_lowering=False)
    dt = nc.dram_tensor
    xT = dt("xT", [D, TB], F32, kind="ExternalInput").ap()
    yaT = dt("yaT", [512, TB], BF16, kind="ExternalInput").ap()
    ysT = dt("ysT", [512, TB], BF16, kind="ExternalInput").ap()
    wgt = dt("wgt", [D, 2048], F32, kind="ExternalInput").ap()
    g1 = dt("g1", [128, 8], F32, kind="ExternalInput").ap()
    g2 = dt("g2", [128, 8], F32, kind="ExternalInput").ap()
    wglu = dt("wglu", [512, 512], F32, kind="ExternalInput").ap()
    wpa = dt("wpa", [512, D], F32, kind="ExternalInput").ap()
    wps = dt("wps", [512, D], F32, kind="ExternalInput").ap()
    wout = dt("wout", [D, D], F32, kind="ExternalInput").ap()
    wr = dt("wr", [D, 36], F32, kind="ExternalInput").ap()
    br = dt("br", [128, 36], F32, kind="ExternalInput").ap()
    weg = dt("weg", [32, D, 256], F32, kind="ExternalInput").ap()
    weu = dt("weu", [32, D, 256], F32, kind="ExternalInput").ap()
    wed = dt("wed", [32, 256, D], F32, kind="ExternalInput").ap()
    onesb = dt("onesb", [128, 128], F32, kind="ExternalInput").ap()
    identf = dt("identf", [128, 128], F32, kind="ExternalInput").ap()
    sel = dt("sel", [32, 32 * 128], F32, kind="ExternalInput").ap()
    outT = dt("outT", [D, TB], F32, kind="ExternalOutput").ap()

    P = Prog(nc)
    V, A, T, G, SY, PL, VQ = _helpers(P)
    with contextlib.ExitStack() as st:
        def sb(name, shape, dtype):
            return st.enter_context(nc.sbuf_tensor(name, shape, dtype))

        def pp(name, shape, dtype=F32):
            return st.enter_context(nc.psum_tensor(name, shape, dtype))
        X = sb("X", [128, 8, TB], F32)
        WG = sb("WG", [128, 8, 2048], BF16)
        H2 = WG
        YY = sb("YY", [128, 2, 4, 512], BF16)
        YAs = YY[:, 0]
        YSs = YY[:, 1]
        WGLU = sb("WGLU", [128, 4, 512], BF16)
        WPA = sb("WPA", [128, 4, D], BF16)
        WPS = sb("WPS", [128, 4, D], BF16)
        WOUT = sb("WOUT", [128, 8, D], BF16)
        WR = sb("WR", [128, 8, 36], F32)
        BR = sb("BR", [128, 36], F32)
        G1 = sb("G1", [128, 8], F32)
        G2 = sb("G2", [128, 8], F32)
        ONB = sb("ONB", [128, 128], BF16)
        IDF = sb("IDF", [128, 128], F32)
        XSQ = sb("XSQ", [128, 8, 512], BF16)
        XBF = sb("XBF", [128, 8, 512], BF16)
        MIX = XSQ
        SELt = WPS[0:32].rearrange("p a b -> p (a b)")
        WEG = [WOUT[:, :, 0:256], XBF[:, :, 0:256]]
        WEU = [WOUT[:, :, 256:512], XBF[:, :, 256:512]]
        WED = [WPA[:, 0:2, :], YY[:, 0].rearrange("p a b -> p (a b)").rearrange("p (f n) -> p f n", f=2)]
        WKEY = [["WOUT", "WOUT", "WPA"], ["XBF", "XBF", "YY"]]
        CT = sb("CT", [32, TB], BF16)
        H2F = sb("H2F", [128, 8, 128], F32)
        RS = sb("RS", [128, 512], F32)
        RSTD = sb("RSTD", [128, 512], F32)
        YG = sb("YG", [128, 4, 512], BF16)
        fa = [sb("fa%d" % i, [128, 512], F32) for i in range(2)]
        fb = [sb("fb%d" % i, [128, 512], F32) for i in range(2)]
        fc = [sb("fc%d" % i, [128, 512], BF16) for i in range(2)]
        HS = [sb("HS%d" % i, [128, 2, 512], BF16) for i in range(2)]
        LG = sb("LG", [128, 4, 36], F32)
        MK = sb("MK", [128, 4, 32], F32)
        CM = sb("CM", [128, 4, 32], F32)
        CM2 = sb("CM2", [128, 4, 32], F32)
        T8 = sb("T8", [128, 4, 8], F32)
        sc1 = {n: sb("sc_" + n, [128, 4], F32) for n in ["gmax", "ngmax", "gsum", "gtop", "d", "s1", "w1", "w2"]}
        sc4 = {n: sb("sc4_" + n, [128, 4, 4], F32) for n in ["mg", "nb", "eg"]}
        B = [pp("B%d" % i, [128, 512]) for i in range(8)]

        for k in range(8):
            SY(lambda e, k=k: e.dma_start(out=X[:, k, :], in_=xT[k * 128:(k + 1) * 128, :]), w=[("X", k)])

        def ldw(dst, src, key):
            G(lambda e: e.dma_start(out=dst, in_=src), w=[key])
        for (dst, src, key) in [(G1[:], g1, "G1"), (ONB[:], onesb, "ONB")]:
            ldw(dst, src, key)
        for k in range(8):
            ldw(WG[:, k, :], wgt[k * 128:(k + 1) * 128, :], ("WG", k))
        ldw(WGLU[:], wglu.rearrange("(k p) n -> p k n", p=128), "WGLU")
        ldw(WPA[:], wpa.rearrange("(k p) n -> p k n", p=128), "WPA")
        ldw(WPS[:], wps.rearrange("(k p) n -> p k n", p=128), "WPS")
        ldw(WOUT[:], wout.rearrange("(k p) n -> p k n", p=128), "WOUT")
        ldw(WR[:], wr.rearrange("(k p) n -> p k n", p=128), "WR")
        for (dst, src, key) in [(BR[:], br, "BR"), (G2[:], g2, "G2"), (IDF[:], identf, "IDF")]:
            ldw(dst, src, key)
        for k in range(8):
            V(lambda e, k=k: e.tensor_scalar(out=WG[:, k, :], in0=WG[:, k, :], scalar1=G1[:, k:k + 1], scalar2=None,
                                             op0=OP.mult), r=["G1", ("WG", k)], w=[("WG", k)])

        def rmsstat(sl, bank):
            for k in range(8):
                A(lambda e, k=k: e.activation(out=XSQ[:, k, :], in_=X[:, k, sl], func=AF.Square), r=[("X", k)],
                  w=[("XSQ", k)])
            for k in range(8):
                T(lambda e, k=k: e.matmul(B[bank][:], lhsT=ONB[:], rhs=XSQ[:, k, :], start=(k == 0), stop=(k == 7)),
                  r=[("XSQ", k), "ONB"], w=[("B", bank)])
            A(lambda e: e.activation(out=RS[:], in_=B[bank][:], func=AF.Sqrt, bias=EPS, scale=1.0 / D),
              r=[("B", bank)], w=["RS"])
            V(lambda e: e.reciprocal(out=RSTD[:], in_=RS[:]), r=["RS"], w=["RSTD"])

        for b in range(4):
            sl = slice(b * 512, (b + 1) * 512)
            G(lambda e, sl=sl: e.dma_start(out=YAs, in_=yaT.rearrange("(k p) t -> p k t", p=128)[:, :, sl]),
              w=[("YY", 0)])
            G(lambda e, sl=sl: e.dma_start(out=YSs, in_=ysT.rearrange("(k p) t -> p k t", p=128)[:, :, sl]),
              w=[("YY", 1)])
            for k in range(8):
                PL(lambda e, k=k, sl=sl: e.tensor_copy(out=XBF[:, k, :], in_=X[:, k, sl]), r=[("X", k)],
                   w=[("XBF", k)])
            rmsstat(sl, 0)
            for m in range(4):
                q = m % 2
                V(lambda e, m=m, q=q: e.tensor_tensor(out=fa[q][:], in0=YSs[:, m, :], in1=YSs[:, m, :], op=OP.mult),
                  r=[("YY", 1)], w=[("fa", q)])
                V(lambda e, q=q: e.tensor_scalar(out=fa[q][:], in0=fa[q][:], scalar1=0.044715, scalar2=1.0,
                                                 op0=OP.mult, op1=OP.add), r=[("fa", q)], w=[("fa", q)])
                V(lambda e, m=m, q=q: e.tensor_tensor(out=fa[q][:], in0=fa[q][:], in1=YSs[:, m, :], op=OP.mult),
                  r=[("fa", q), ("YY", 1)], w=[("fa", q)])
                A(lambda e, q=q: e.activation(out=fb[q][:], in_=fa[q][:], func=AF.Sigmoid,
                                              scale=1.5957691216057308), r=[("fa", q)], w=[("fb", q)])
                V(lambda e, m=m, q=q: e.tensor_tensor(out=YSs[:, m, :], in0=YSs[:, m, :], in1=fb[q][:], op=OP.mult),
                  r=[("fb", q), ("YY", 1)], w=[("YY", 1)])
            for m in range(4):
                bk = 1 + (m % 2)
                for k in range(4):
                    T(lambda e, m=m, k=k, bk=bk: e.matmul(B[bk][:], lhsT=WGLU[:, k, m * 128:(m + 1) * 128],
                                                          rhs=YSs[:, k, :], start=(k == 0), stop=(k == 3)),
                      r=["WGLU", ("YY", 1)], w=[("B", bk)])
                A(lambda e, bk=bk, m=m: e.activation(out=fa[m % 2][:], in_=B[bk][:], func=AF.Sigmoid),
                  r=[("B", bk)], w=[("fa", m % 2)])
                V(lambda e, m=m: e.tensor_tensor(out=YG[:, m, :], in0=YSs[:, m, :], in1=fa[m % 2][:], op=OP.mult),
                  r=[("YY", 1), ("fa", m % 2)], w=[("YG", m)])
            for n in range(8):
                q = n % 2
                b0, b1, b2, b3 = (0 + 4 * q, 1 + 4 * q, 2 + 4 * q, 3 + 4 * q)
                for k in range(8):
                    T(lambda e, n=n, k=k, b0=b0: e.matmul(B[b0][:], lhsT=WG[:, k, n * 128:(n + 1) * 128],
                                                          rhs=XBF[:, k, :], start=(k == 0), stop=(k == 7)),
                      r=[("WG", k), ("XBF", k)], w=[("B", b0)])
                for k in range(8):
                    T(lambda e, n=n, k=k, b1=b1: e.matmul(B[b1][:],
                                                          lhsT=WG[:, k, 1024 + n * 128:1024 + (n + 1) * 128],
                                                          rhs=XBF[:, k, :], start=(k == 0), stop=(k == 7)),
                      r=[("WG", k), ("XBF", k)], w=[("B", b1)])
                for k in range(4):
                    T(lambda e, n=n, k=k, b2=b2: e.matmul(B[b2][:], lhsT=WPA[:, k, n * 128:(n + 1) * 128],
                                                          rhs=YAs[:, k, :], start=(k == 0), stop=(k == 3)),
                      r=["WPA", ("YY", 0)], w=[("B", b2)])
                for k in range(4):
                    T(lambda e, n=n, k=k, b3=b3: e.matmul(B[b3][:], lhsT=WPS[:, k, n * 128:(n + 1) * 128],
                                                          rhs=YG[:, k, :], start=(k == 0), stop=(k == 3)),
                      r=["WPS", ("YG", k)], w=[("B", b3)])
                V(lambda e, q=q, b0=b0: e.tensor_tensor(out=fa[q][:], in0=B[b0][:], in1=RSTD[:], op=OP.mult),
                  r=[("B", b0), "RSTD"], w=[("fa", q)])
                A(lambda e, q=q: e.activation(out=fa[q][:], in_=fa[q][:], func=AF.Sigmoid), r=[("fa", q)],
                  w=[("fa", q)])
                V(lambda e, q=q, b1=b1: e.tensor_tensor(out=fb[q][:], in0=B[b1][:], in1=RSTD[:], op=OP.mult),
                  r=[("B", b1), "RSTD"], w=[("fb", q)])
                A(lambda e, q=q: e.activation(out=fb[q][:], in_=fb[q][:], func=AF.Sigmoid), r=[("fb", q)],
                  w=[("fb", q)])
                V(lambda e, q=q, b2=b2: e.tensor_tensor(out=fa[q][:], in0=B[b2][:], in1=fa[q][:], op=OP.mult),
                  r=[("B", b2), ("fa", q)], w=[("fa", q)])
                V(lambda e, q=q, b3=b3: e.tensor_tensor(out=fb[q][:], in0=B[b3][:], in1=fb[q][:], op=OP.mult),
                  r=[("B", b3), ("fb", q)], w=[("fb", q)])
                PL(lambda e, n=n, q=q: e.tensor_tensor(out=MIX[:, n, :], in0=fa[q][:], in1=fb[q][:], op=OP.add),
                   r=[("fa", q), ("fb", q)], w=[("XSQ", n)])
            for n in range(8):
                bk = n % 4
                for k in range(8):
                    T(lambda e, n=n, k=k, bk=bk: e.matmul(B[bk][:], lhsT=WOUT[:, k, n * 128:(n + 1) * 128],
                                                          rhs=MIX[:, k, :], start=(k == 0), stop=(k == 7)),
                      r=["WOUT", ("XSQ", k)], w=[("B", bk)])
                V(lambda e, n=n, sl=sl, bk=bk: e.tensor_tensor(out=X[:, n, sl], in0=X[:, n, sl], in1=B[bk][:],
                                                               op=OP.add), r=[("X", n), ("B", bk)], w=[("X", n)])

        def load_expert(ex):
            p = ex % 2
            G(lambda e: e.dma_start(out=WEG[p], in_=weg[ex].rearrange("(k p) n -> p k n", p=128)), w=[WKEY[p][0]])
            G(lambda e: e.dma_start(out=WEU[p], in_=weu[ex].rearrange("(k p) n -> p k n", p=128)), w=[WKEY[p][1]])
            G(lambda e: e.dma_start(out=WED[p], in_=wed[ex].rearrange("(k p) n -> p k n", p=128)), w=[WKEY[p][2]])
        ldw(SELt, sel, "WPS")
        load_expert(0)
        load_expert(1)

        for b in range(4):
            sl = slice(b * 512, (b + 1) * 512)
            rmsstat(sl, 0)
            for tt in range(4):
                ts_ = slice(tt * 128, (tt + 1) * 128)
                gs_ = slice(b * 512 + tt * 128, b * 512 + (tt + 1) * 128)
                for n in range(8):
                    V(lambda e, n=n, gs_=gs_, ts_=ts_: e.scalar_tensor_tensor(
                        out=H2F[:, n, :], in0=X[:, n, gs_], scalar=G2[:, n:n + 1], in1=RSTD[:, ts_], op0=OP.mult,
                        op1=OP.mult), r=[("X", n), "G2", "RSTD"], w=[("H2F", n)])
                    PL(lambda e, n=n, gs_=gs_: e.tensor_copy(out=H2[:, n, gs_], in_=H2F[:, n, :]), r=[("H2F", n)],
                       w=[("WG", n)])
                for k in range(8):
                    T(lambda e, k=k, tt=tt: e.matmul(B[1 + tt][:, 0:36], lhsT=H2F[:, k, :], rhs=WR[:, k, :],
                                                     start=(k == 0), stop=(k == 7)), r=[("H2F", k), "WR"],
                      w=[("B", 1 + tt)])
            steps = []

            def chain(tt):
                RT = dict(r=[("RT", tt)], w=[("RT", tt)])
                LGt, MKt, CMt, CM2t, T8t = LG[:, tt, :], MK[:, tt, :], CM[:, tt, :], CM2[:, tt, :], T8[:, tt, :]
                g = {n: t[:, tt:tt + 1] for n, t in sc1.items()}
                eg, mg, nb_ = sc4["eg"][:, tt, :], sc4["mg"][:, tt, :], sc4["nb"][:, tt, :]
                ops = []
                ops.append(lambda: V(lambda e: e.tensor_tensor(out=LGt, in0=B[1 + tt][:, 0:36], in1=BR[:], op=OP.add),
                                     r=[("B", 1 + tt), "BR", ("RT", tt)], w=[("RT", tt)]))
                ops.append(lambda: V(lambda e: e.tensor_reduce(out=g["gmax"], in_=LGt[:, 0:4], axis=AX.X, op=OP.max),
                                     **RT))
                ops.append(lambda: V(lambda e: e.tensor_scalar(out=g["ngmax"], in0=g["gmax"], scalar1=-1.0,
                                                               scalar2=None, op0=OP.mult), **RT))
                ops.append(lambda: A(lambda e: e.activation(out=eg, in_=LGt[:, 0:4], func=AF.Exp, bias=g["ngmax"],
                                                            scale=1.0), **RT))
                ops.append(lambda: V(lambda e: e.tensor_reduce(out=g["gsum"], in_=eg, axis=AX.X, op=OP.add), **RT))
                ops.append(lambda: V(lambda e: e.reciprocal(out=g["gtop"], in_=g["gsum"]), **RT))
                ops.append(lambda: V(lambda e: e.tensor_scalar(out=mg, in0=LGt[:, 0:4], scalar1=g["gmax"],
                                                               scalar2=None, op0=OP.is_equal), **RT))
                ops.append(lambda: V(lambda e: e.tensor_scalar(out=nb_, in0=mg, scalar1=-1.0, scalar2=1e30,
                                                               op0=OP.add, op1=OP.mult), **RT))
                for gi in range(4):
                    ops.append(lambda gi=gi: V(lambda e: e.tensor_scalar(
                        out=MKt[:, gi * 8:(gi + 1) * 8], in0=LGt[:, 4 + gi * 8:4 + (gi + 1) * 8],
                        scalar1=mg[:, gi:gi + 1], scalar2=nb_[:, gi:gi + 1], op0=OP.mult, op1=OP.add), **RT))
                ops.append(lambda: V(lambda e: e.max(out=T8t, in_=MKt), **RT))
                ops.append(lambda: V(lambda e: e.tensor_tensor(out=g["d"], in0=T8t[:, 1:2], in1=T8t[:, 0:1],
                                                               op=OP.subtract), **RT))
                ops.append(lambda: A(lambda e: e.activation(out=g["s1"], in_=g["d"], func=AF.Exp), **RT))
                ops.append(lambda: V(lambda e: e.tensor_scalar(out=g["s1"], in0=g["s1"], scalar1=1.0, scalar2=None,
                                                               op0=OP.add), **RT))
                ops.append(lambda: V(lambda e: e.reciprocal(out=g["s1"], in_=g["s1"]), **RT))
                ops.append(lambda: V(lambda e: e.tensor_tensor(out=g["w1"], in0=g["s1"], in1=g["gtop"], op=OP.mult),
                                     **RT))
                ops.append(lambda: V(lambda e: e.tensor_tensor(out=g["w2"], in0=g["gtop"], in1=g["w1"],
                                                               op=OP.subtract), **RT))
                ops.append(lambda: V(lambda e: e.tensor_scalar(out=CMt, in0=MKt, scalar1=T8t[:, 0:1],
                                                               scalar2=g["w1"], op0=OP.is_equal, op1=OP.mult), **RT))
                ops.append(lambda: V(lambda e: e.tensor_scalar(out=CM2t, in0=MKt, scalar1=T8t[:, 1:2],
                                                               scalar2=g["w2"], op0=OP.is_equal, op1=OP.mult), **RT))
                ops.append(lambda: V(lambda e: e.tensor_tensor(out=CMt, in0=CMt, in1=CM2t, op=OP.add), **RT))
                ops.append(lambda: T(lambda e: e.transpose(B[5][0:32, tt * 128:(tt + 1) * 128], CMt, IDF[:]),
                                     r=[("RT", tt), "IDF"], w=[("B", 5)]))
                return ops
            chains = [chain(tt) for tt in range(4)]
            for i in range(len(chains[0])):
                for tt in range(4):
                    chains[tt][i]()
            V(lambda e, b=b: e.tensor_copy(out=CT[:, b * 512:(b + 1) * 512], in_=B[5][0:32, :]), r=[("B", 5)],
              w=[("CT", b)])

        units = [(ex, b) for ex in range(32) for b in range(4)]

        def up(ui):
            ex, b = units[ui]
            p = ex % 2
            hp = ui % 2
            sl = slice(b * 512, (b + 1) * 512)
            T(lambda e: e.matmul(B[0][:], lhsT=SELt[:, ex * 128:(ex + 1) * 128], rhs=CT[:, sl], start=True,
                                 stop=True), r=["WPS", ("CT", b)], w=[("B", 0)])
            A(lambda e: e.activation(out=fc[hp][:], in_=B[0][:], func=AF.Copy), r=[("B", 0)], w=[("fc", hp)])
            for f in range(2):
                bg, bu = 1 + 2 * f, 2 + 2 * f
                for k in range(8):
                    T(lambda e, f=f, k=k, bg=bg: e.matmul(B[bg][:], lhsT=WEG[p][:, k, f * 128:(f + 1) * 128],
                                                          rhs=H2[:, k, sl], start=(k == 0), stop=(k == 7)),
                      r=[WKEY[p][0], ("WG", k)], w=[("B", bg)])
                for k in range(8):
                    T(lambda e, f=f, k=k, bu=bu: e.matmul(B[bu][:], lhsT=WEU[p][:, k, f * 128:(f + 1) * 128],
                                                          rhs=H2[:, k, sl], start=(k == 0), stop=(k == 7)),
                      r=[WKEY[p][1], ("WG", k)], w=[("B", bu)])
                A(lambda e, f=f, bg=bg: e.activation(out=fa[f][:], in_=B[bg][:], func=AF.Silu), r=[("B", bg)],
                  w=[("fa", f)])
                V(lambda e, f=f, bu=bu: e.tensor_tensor(out=fb[f][:], in0=B[bu][:], in1=fc[hp][:], op=OP.mult),
                  r=[("B", bu), ("fc", hp)], w=[("fb", f)])
                PL(lambda e, f=f: e.tensor_tensor(out=HS[hp][:, f, :], in0=fa[f][:], in1=fb[f][:], op=OP.mult),
                   r=[("fa", f), ("fb", f)], w=[("HS", hp, f)])

        def down(ui):
            ex, b = units[ui]
            p = ex % 2
            hp = ui % 2
            sl = slice(b * 512, (b + 1) * 512)
            for n in range(8):
                bk = 5 + (n % 3)
                for f in range(2):
                    T(lambda e, n=n, f=f, bk=bk: e.matmul(B[bk][:], lhsT=WED[p][:, f, n * 128:(n + 1) * 128],
                                                          rhs=HS[hp][:, f, :], start=(f == 0), stop=(f == 1)),
                      r=[WKEY[p][2], ("HS", hp, f)], w=[("B", bk)])
                V(lambda e, n=n, bk=bk: e.tensor_tensor(out=X[:, n, sl], in0=X[:, n, sl], in1=B[bk][:], op=OP.add),
                  r=[("X", n), ("B", bk)], w=[("X", n)])
            if b == 3 and ex + 2 < 32:
                load_expert(ex + 2)
        for ui in range(len(units)):
            up(ui)
            if ui >= 1:
                down(ui - 1)
        down(len(units) - 1)
        for k in range(8):
            SY(lambda e, k=k: e.dma_start(out=outT[k * 128:(k + 1) * 128, :], in_=X[:, k, :]), r=[("X", k)],
               w=["outT"])
        P.emit()
    return nc


def _consts():
    onesb = np.ones((128, 128), np.float32)
    ident = np.eye(128, dtype=np.float32)
    iota1 = np.tile(np.arange(1, 513, dtype=np.float32)[None, :], (128, 1))
    p = np.arange(128)
    maskd = np.where(p[:, None] > p[None, :], -30000.0, 0.0).astype(np.float32)
    sel = np.zeros((32, 32, 128), np.float32)
    for e in range(32):
        sel[e, e, :] = 1.0
    return onesb, ident, iota1, maskd, sel.reshape(32, 32 * 128)


def stage_a_inputs(x, norm_mix_g, w_in, b_forget, q_norm_g, k_norm_g, ssm_A_re, ssm_A_im, ssm_log_dt, ssm_B_re,
                   ssm_B_im, ssm_C_re, ssm_C_im, ssm_D):
    onesb, ident, iota1, maskd, _ = _consts()
    xT = np.ascontiguousarray(x.reshape(NTOK, D).T)
    w = w_in[0]
    g1 = np.ascontiguousarray(norm_mix_g[0].reshape(8, 128).T)
    maps = []
    for c in range(NCORES):
        wq = np.ascontiguousarray(w[:, c * 64:(c + 1) * 64])
        wk = np.ascontiguousarray(np.concatenate([w[:, 512 + c * 64:512 + (c + 1) * 64],
                                                  w[:, 1536 + c:1537 + c]], axis=1))
        wv = w[:, 1024 + c * 64:1024 + (c + 1) * 64]
        wu = w[:, 1544 + c * 64:1544 + (c + 1) * 64]
        wvu = np.ascontiguousarray(np.concatenate([wv, wu], axis=1))
        nbf = np.full((65, 2), b_forget[0, c], np.float32)
        gqk = np.ascontiguousarray(np.stack([q_norm_g[0], k_norm_g[0]], axis=1)).astype(np.float32)
        gs = np.arange(4 * c, 4 * c + 4)
        are = np.zeros((128, 2), np.float32)
        aim = np.zeros((128, 2), np.float32)
        ldt = np.zeros((128, 2), np.float32)
        brp = np.zeros((64, 2, 128), np.float32)
        bip = np.zeros((64, 2, 128), np.float32)
        crp = np.zeros((128, 2, 128), np.float32)
        cip = np.zeros((128, 2, 128), np.float32)
        for gh in range(2):
            for gl in range(2):
                gloc = 2 * gh + gl
                g = gs[gloc]
                are[gl * 64:(gl + 1) * 64, gh] = ssm_A_re[0, g]
                aim[gl * 64:(gl + 1) * 64, gh] = ssm_A_im[0, g]
                ldt[gl * 64:(gl + 1) * 64, gh] = ssm_log_dt[0, g]
                brp[gloc * 16:(gloc + 1) * 16, gh, gl * 64:(gl + 1) * 64] = ssm_B_re[0, g].T
                bip[gloc * 16:(gloc + 1) * 16, gh, gl * 64:(gl + 1) * 64] = ssm_B_im[0, g].T
                crp[gl * 64:(gl + 1) * 64, gh, 64 + gloc * 16:64 + (gloc + 1) * 16] = ssm_C_re[0, g].T
                cip[gl * 64:(gl + 1) * 64, gh, 64 + gloc * 16:64 + (gloc + 1) * 16] = ssm_C_im[0, g].T
        dsk = np.ascontiguousarray(np.repeat(ssm_D[0, c * 64:(c + 1) * 64][:, None], 2, axis=1)).astype(np.float32)
        maps.append(dict(xT=xT, wq=wq, wk=wk, wvu=wvu, g1=g1, nbf=nbf, gqk=gqk, are=are, aim=aim, ldt=ldt,
                         brp=brp, bip=bip, crp=crp, cip=cip, dsk=dsk, onesb=onesb, identb=ident, iota1=iota1,
                         maskd=maskd))
    return maps


def stage_b_inputs(x, ya_full, ys_full, norm_mix_g, w_in, w_glu, w_proj_attn, w_proj_ssm, w_out, norm_ffn_g,
                   w_router_group, b_router_group, w_router_expert, b_router_expert, w_expert_gate, w_expert_up,
                   w_expert_down):
    onesb, ident, iota1, maskd, sel = _consts()
    xf = x.reshape(NTOK, D)
    g1 = np.ascontiguousarray(norm_mix_g[0].reshape(8, 128).T)
    g2 = np.ascontiguousarray(norm_ffn_g[0].reshape(8, 128).T)
    wgt = np.ascontiguousarray(w_in[0][:, 2056:4104])
    wr = np.ascontiguousarray(np.concatenate([w_router_group[0], w_router_expert[0]], axis=1))
    br = np.ascontiguousarray(np.tile(np.concatenate([b_router_group[0], b_router_expert[0]])[None, :], (128, 1)))
    maps = []
    for c in range(NCORES):
        tsl = slice(c * TB, (c + 1) * TB)
        maps.append(dict(xT=np.ascontiguousarray(xf[tsl].T), yaT=np.ascontiguousarray(ya_full[:, tsl]),
                         ysT=np.ascontiguousarray(ys_full[:, tsl]), wgt=wgt, g1=g1, g2=g2, wglu=w_glu[0],
                         wpa=w_proj_attn[0], wps=w_proj_ssm[0], wout=w_out[0], wr=wr, br=br, weg=w_expert_gate[0],
                         weu=w_expert_up[0], wed=w_expert_down[0], onesb=onesb, identf=ident, sel=sel))
    return maps


def run_a(inputs):
    nc = build_a()
    keys = ["x", "norm_mix_g", "w_in", "b_forget", "q_norm_g", "k_norm_g", "ssm_A_re", "ssm_A_im", "ssm_log_dt",
            "ssm_B_re", "ssm_B_im", "ssm_C_re", "ssm_C_im", "ssm_D"]
    maps = stage_a_inputs(*[np.asarray(inputs[k], np.float32) for k in keys])
    res = run_bass_kernel_spmd(nc, maps, core_ids=list(range(NCORES)))
    ya = np.concatenate([np.asarray(res.results[c]["yaT"]) for c in range(NCORES)], axis=0)
    ys = np.concatenate([np.asarray(res.results[c]["ysT"]) for c in range(NCORES)], axis=0)
    return ya, ys


def run_b(inputs, ya, ys):
    nc = build_b()
    keys = ["norm_mix_g", "w_in", "w_glu", "w_proj_attn", "w_proj_ssm", "w_out", "norm_ffn_g", "w_router_group",
            "b_router_group", "w_router_expert", "b_router_expert", "w_expert_gate", "w_expert_up", "w_expert_down"]
    maps = stage_b_inputs(np.asarray(inputs["x"], np.float32), ya, ys,
                          *[np.asarray(inputs[k], np.float32) for k in keys])
    res = run_bass_kernel_spmd(nc, maps, core_ids=list(range(NCORES)))
    out = np.concatenate([np.asarray(res.results[c]["outT"]).T for c in range(NCORES)], axis=0)
    return out.reshape(2, S, D).astype(np.float32)


def kernel(**inputs):
    ya, ys = run_a(inputs)
    return run_b(inputs, ya, ys)
```
